# Optimizing a Trainium2 kernel written in Bass

```python
import math
import jax, jax.numpy as jnp
from jax import lax
import numpy as np

D_MODEL = 1024
BATCH = 16
SEQ = 2048
DEPTH = 1

N_DIFF_HEADS = 8
DIFF_HEAD_DIM = 64
ATTN_WIDTH = N_DIFF_HEADS * 2 * DIFF_HEAD_DIM
Q_BLOCK = 128
SGU_WIDTH = 1024
N_SGU_GROUPS = 8
SGU_GROUP_DIM = SGU_WIDTH // N_SGU_GROUPS
SGU_CHUNK = 128
N_GROUPS = 4
EXPERTS_PER_GROUP = 8
N_EXPERTS = N_GROUPS * EXPERTS_PER_GROUP
TOP_K_IN_GROUP = 2
D_EXPERT = 256
IN_SIZES = [ATTN_WIDTH, ATTN_WIDTH, ATTN_WIDTH, SGU_WIDTH, SGU_WIDTH, D_MODEL, D_MODEL]
IN_WIDTH = sum(IN_SIZES)
IN_SPLITS = [int(v) for v in np.cumsum(IN_SIZES)[:-1]]
EPS = 1e-6
ALIBI_SLOPES = np.array([2.0 ** (-8.0 * (h + 1) / N_DIFF_HEADS) for h in range(N_DIFF_HEADS)], dtype=np.float32)

kernel_name = "hybrid_diffattn_sgu_hmoe_encoder"


def lambda_init_fn(layer):
    return 0.8 - 0.6 * math.exp(-0.3 * layer)


def rmsnorm(x, g):
    xf = x.astype(jnp.float32)
    y = xf * lax.rsqrt(jnp.mean(xf * xf, axis=-1, keepdims=True) + EPS)
    return y.astype(x.dtype) * g


def layernorm(x, g, b):
    xf = x.astype(jnp.float32)
    mu = jnp.mean(xf, axis=-1, keepdims=True)
    var = jnp.mean(jnp.square(xf - mu), axis=-1, keepdims=True)
    return ((xf - mu) * lax.rsqrt(var + EPS)).astype(x.dtype) * g + b


def modulate(h, shift, scale):
    return h * (1.0 + scale[:, None, :]) + shift[:, None, :]


def diff_attention(q, k, v, lam, sub_g, lam_init):
    B, S = q.shape[0], q.shape[1]
    scale = DIFF_HEAD_DIM ** -0.5
    slopes = jnp.asarray(ALIBI_SLOPES)
    kpos = jnp.arange(S)

    def block(i):
        start = i * Q_BLOCK
        qb = lax.dynamic_slice_in_dim(q, start, Q_BLOCK, axis=1)
        qpos = start + jnp.arange(Q_BLOCK)
        dist = jnp.abs(qpos[:, None] - kpos[None, :]).astype(jnp.float32)
        bias = -slopes[:, None, None, None] * dist[None, None]
        s = jnp.einsum('bqhmd,bkhmd->bhmqk', qb, k).astype(jnp.float32) * scale + bias
        p = jax.nn.softmax(s, axis=-1)
        a = p[:, :, 0] - lam * p[:, :, 1]
        return jnp.einsum('bhqk,bkhe->bqhe', a.astype(v.dtype), v)

    out = lax.map(block, jnp.arange(S // Q_BLOCK))
    out = jnp.moveaxis(out, 0, 1).reshape(B, S, N_DIFF_HEADS, 2 * DIFF_HEAD_DIM)
    out = rmsnorm(out, sub_g) * (1.0 - lam_init)
    return out.reshape(B, S, ATTN_WIDTH)


def spatial_gating(u, s, ln_g, ln_b, w_s, b_s):
    B, S = u.shape[0], u.shape[1]
    v = layernorm(s, ln_g, ln_b)
    v = v.reshape(B, S // SGU_CHUNK, SGU_CHUNK, N_SGU_GROUPS, SGU_GROUP_DIM)
    mixed = jnp.einsum('gts,bnsgc->bntgc', w_s, v) + b_s.T[:, :, None]
    return u * mixed.reshape(B, S, SGU_WIDTH)


def hier_moe(h, w_rg, b_rg, w_re, b_re, w_gu, w_down):
    B, S, D = h.shape
    t = h.reshape(-1, D)
    T = t.shape[0]
    gp = jax.nn.softmax((t @ w_rg + b_rg).astype(jnp.float32), axis=-1)
    gval, gidx = lax.top_k(gp, 1)
    el = (t @ w_re + b_re).astype(jnp.float32).reshape(T, N_GROUPS, EXPERTS_PER_GROUP)
    el_sel = jnp.take_along_axis(el, gidx[:, :, None], axis=1)[:, 0]
    ep = jax.nn.softmax(el_sel, axis=-1)
    ev, eidx = lax.top_k(ep, TOP_K_IN_GROUP)
    ew = ev / jnp.sum(ev, axis=-1, keepdims=True) * gval
    gl_idx = gidx * EXPERTS_PER_GROUP + eidx
    combine = jnp.einsum('tk,tke->te', ew, jax.nn.one_hot(gl_idx, N_EXPERTS, dtype=jnp.float32))
    y = jnp.zeros((T, D), jnp.float32)
    for e in range(N_EXPERTS):
        gu = t @ w_gu[e]
        g, u = jnp.split(gu, 2, axis=-1)
        y = y + combine[:, e:e + 1] * ((jax.nn.silu(g) * u) @ w_down[e]).astype(jnp.float32)
    return y.astype(h.dtype).reshape(B, S, D)


def setup_inputs(seed: int = 0) -> dict:
    key = jax.random.key(seed)
    ks = jax.random.split(key, 32)
    f32 = jnp.float32
    L, D = DEPTH, D_MODEL
    nrm = lambda k, shape, s: jax.random.normal(k, shape, f32) * s
    return {
        "x": nrm(ks[0], (BATCH, SEQ, D), 1.0),
        "c": nrm(ks[1], (BATCH, D), 1.0),
        "w_ada": nrm(ks[2], (L, D, 6 * D), 0.5 * D ** -0.5),
        "b_ada": nrm(ks[3], (L, 6 * D), 0.02),
        "norm1_g": 1.0 + nrm(ks[4], (L, D), 0.02),
        "w_in": nrm(ks[5], (L, D, IN_WIDTH), D ** -0.5),
        "lambda_q1": nrm(ks[6], (L, DIFF_HEAD_DIM), 0.1),
        "lambda_k1": nrm(ks[7], (L, DIFF_HEAD_DIM), 0.1),
        "lambda_q2": nrm(ks[8], (L, DIFF_HEAD_DIM), 0.1),
        "lambda_k2": nrm(ks[9], (L, DIFF_HEAD_DIM), 0.1),
        "subln_g": 1.0 + nrm(ks[10], (L, 2 * DIFF_HEAD_DIM), 0.02),
        "w_attn_proj": nrm(ks[11], (L, ATTN_WIDTH, D), ATTN_WIDTH ** -0.5),
        "sgu_ln_g": 1.0 + nrm(ks[12], (L, SGU_WIDTH), 0.02),
        "sgu_ln_b": nrm(ks[13], (L, SGU_WIDTH), 0.02),
        "sgu_w_s": nrm(ks[14], (L, N_SGU_GROUPS, SGU_CHUNK, SGU_CHUNK), 0.5 * SGU_CHUNK ** -0.5),
        "sgu_b_s": 1.0 + nrm(ks[15], (L, N_SGU_GROUPS, SGU_CHUNK), 0.1),
        "w_sgu_proj": nrm(ks[16], (L, SGU_WIDTH, D), SGU_WIDTH ** -0.5),
        "w_out": nrm(ks[17], (L, D, D), D ** -0.5),
        "norm2_g": 1.0 + nrm(ks[18], (L, D), 0.02),
        "w_router_group": nrm(ks[19], (L, D, N_GROUPS), D ** -0.5),
        "b_router_group": nrm(ks[20], (L, N_GROUPS), 0.01),
        "w_router_expert": nrm(ks[21], (L, D, N_EXPERTS), D ** -0.5),
        "b_router_expert": nrm(ks[22], (L, N_EXPERTS), 0.01),
        "w_expert_gate_up": nrm(ks[23], (L, N_EXPERTS, D, 2 * D_EXPERT), D ** -0.5),
        "w_expert_down": nrm(ks[24], (L, N_EXPERTS, D_EXPERT, D), D_EXPERT ** -0.5),
        "final_g": 1.0 + nrm(ks[25], (D,), 0.02),
    }


def reference(x, c, w_ada, b_ada, norm1_g, w_in, lambda_q1, lambda_k1, lambda_q2, lambda_k2,
              subln_g, w_attn_proj, sgu_ln_g, sgu_ln_b, sgu_w_s, sgu_b_s, w_sgu_proj, w_out,
              norm2_g, w_router_group, b_router_group, w_router_expert, b_router_expert,
              w_expert_gate_up, w_expert_down, final_g):
    B, S = x.shape[0], x.shape[1]
    for l in range(DEPTH):
        mod = jax.nn.silu(c) @ w_ada[l] + b_ada[l]
        shift1, scale1, gate1, shift2, scale2, gate2 = jnp.split(mod, 6, axis=-1)

        h = modulate(rmsnorm(x, norm1_g[l]), shift1, scale1)
        q, k, v, u, s, ga, gb = jnp.split(h @ w_in[l], IN_SPLITS, axis=-1)
        q = q.reshape(B, S, N_DIFF_HEADS, 2, DIFF_HEAD_DIM)
        k = k.reshape(B, S, N_DIFF_HEADS, 2, DIFF_HEAD_DIM)
        v = v.reshape(B, S, N_DIFF_HEADS, 2 * DIFF_HEAD_DIM)
        lam_init = lambda_init_fn(l)
        lam = (jnp.exp(jnp.sum(lambda_q1[l] * lambda_k1[l]).astype(jnp.float32))
               - jnp.exp(jnp.sum(lambda_q2[l] * lambda_k2[l]).astype(jnp.float32)) + lam_init)
        y_attn = diff_attention(q, k, v, lam, subln_g[l], lam_init) @ w_attn_proj[l]
        y_sgu = spatial_gating(jax.nn.gelu(u), jax.nn.gelu(s), sgu_ln_g[l], sgu_ln_b[l],
                               sgu_w_s[l], sgu_b_s[l]) @ w_sgu_proj[l]
        y = jax.nn.sigmoid(ga) * y_attn + jax.nn.sigmoid(gb) * y_sgu
        x = x + gate1[:, None, :] * (y @ w_out[l])

        h2 = modulate(rmsnorm(x, norm2_g[l]), shift2, scale2)
        x = x + gate2[:, None, :] * hier_moe(h2, w_router_group[l], b_router_group[l],
                                             w_router_expert[l], b_router_expert[l],
                                             w_expert_gate_up[l], w_expert_down[l])
    return rmsnorm(x, final_g)
```

```python
import math
from contextlib import ExitStack
import numpy as np
import concourse.bass as bass
import concourse.mybir as mybir
from concourse.bass_utils import run_bass_kernel_spmd

F32 = mybir.dt.float32
BF16 = mybir.dt.bfloat16
AF = mybir.ActivationFunctionType
ALU = mybir.AluOpType
AX = mybir.AxisListType
EPS = 1e-6


class Buf:
    __slots__ = ("name", "w", "r", "dsem", "dcnt")

    def __init__(self, name):
        self.name = name
        self.w = None
        self.r = {}
        self.dsem = None
        self.dcnt = 0


class Eng:
    def __init__(self, K, name, sem, self_raw=True):
        self.K = K
        self.name = name
        self.sem = sem
        self.cnt = 0
        self.known = {}
        self.self_raw = self_raw
        self.prog = []

    def wait(self, ev):
        if ev is None:
            return
        sem, val = ev
        if self.known.get(id(sem), 0) >= val:
            return
        self.prog.append(("w", sem, val))
        self.known[id(sem)] = val

    def _deps(self, reads, writes):
        for b in reads:
            if b.w is not None:
                if b.w[0] is self.sem and not self.self_raw:
                    continue
                self.wait(b.w)
        for b in writes:
            if b.w is not None and (b.w[0] is not self.sem or self.self_raw):
                self.wait(b.w)
            for sem, v in b.r.values():
                if sem is not self.sem or self.self_raw:
                    self.wait((sem, v))

    def op(self, fn, reads=(), writes=(), sig=True):
        self._deps(reads, writes)
        if sig:
            self.cnt += 1
            self.prog.append(("o", fn, self.sem, 1))
            ev = (self.sem, self.cnt)
        else:
            self.prog.append(("o", fn, None, 0))
            ev = (self.sem, self.cnt + 1)
        for b in reads:
            b.r[id(self.sem)] = ev
        for b in writes:
            b.w = ev
            b.r = {}
        return ev

    def dma(self, fn, anchor, reads=(), writes=()):
        self._deps(reads, writes)
        if anchor.dsem is None:
            anchor.dsem = self.K.new_sem("d_" + anchor.name)
            self.K.dma_bufs.append(anchor)
        anchor.dcnt += 16
        self.prog.append(("o", fn, anchor.dsem, 16))
        ev = (anchor.dsem, anchor.dcnt)
        for b in reads:
            b.r[id(anchor.dsem)] = ev
        for b in writes:
            b.w = ev
            b.r = {}
        return ev

    def replay(self, eng):
        for it in self.prog:
            if it[0] == "w":
                eng.wait_ge(it[1], it[2])
            else:
                ins = it[1](eng)
                if it[2] is not None:
                    ins.then_inc(it[2], it[3])


class Kern:
    def __init__(self, nc, stack):
        self.nc = nc
        self.stack = stack
        self.dma_bufs = []
        self.pe = Eng(self, "pe", self.new_sem("s_pe"), self_raw=False)
        self.act = Eng(self, "act", self.new_sem("s_act"))
        self.dve = Eng(self, "dve", self.new_sem("s_dve"))
        self.pool = Eng(self, "pool", self.new_sem("s_pool"))
        self.sp = Eng(self, "sp", self.new_sem("s_sp"))
        self.engs = [self.pe, self.act, self.dve, self.pool, self.sp]

    def new_sem(self, name):
        return self.stack.enter_context(self.nc.semaphore(name))

    def sb(self, name, shape, dt):
        return self.stack.enter_context(self.nc.sbuf_tensor(name, shape, dt))

    def ps(self, name, shape, dt):
        return self.stack.enter_context(self.nc.psum_tensor(name, shape, dt))

    def barrier(self, engs=None):
        evs = [(e.sem, e.cnt) for e in self.engs if e.cnt > 0]
        evs += [(b.dsem, b.dcnt) for b in self.dma_bufs]
        for e in (engs or self.engs):
            for ev in evs:
                if ev[0] is not e.sem:
                    e.wait(ev)

    def emit(self):
        with self.nc.Block() as block:
            block.tensor(lambda e: self.pe.replay(e))
            block.scalar(lambda e: self.act.replay(e))
            block.vector(lambda e: self.dve.replay(e))
            block.gpsimd(lambda e: self.pool.replay(e))
            block.sync(lambda e: self.sp.replay(e))


class Cfg:
    def __init__(self, D=1024, S=2048, NB=2, NH=8, NG=4, EPG=8, DE=256, depth_l=0):
        self.D, self.S, self.NB, self.NH, self.NG, self.EPG, self.DE = D, S, NB, NH, NG, EPG, DE
        self.DC = D // 128
        self.TC = S // 128
        self.AW = NH * 128
        self.G = D // 128
        self.NE = NG * EPG
        self.FC = DE // 128
        self.INW = 3 * self.AW + 4 * D
        self.QT = min(512, S)
        self.lam_init = 0.8 - 0.6 * math.exp(-0.3 * depth_l)
        self.slopes = [2.0 ** (-8.0 * (h + 1) / NH) for h in range(NH)]


def build(cfg, stop_after=None):
    DB = min(512, cfg.D)
    D, S, NB, NH, NG, EPG, DE = cfg.D, cfg.S, cfg.NB, cfg.NH, cfg.NG, cfg.EPG, cfg.DE
    DC, TC, AW, G, NE, FC, INW, QT = cfg.DC, cfg.TC, cfg.AW, cfg.G, cfg.NE, cfg.FC, cfg.INW, cfg.QT
    NR = NG + NE
    nc = bass.Bass("TRN2", target_bir_lowering=False)
    _bufs = {}

    def GB(name):
        if name not in _bufs:
            _bufs[name] = Buf(name)
        return _bufs[name]

    def din(name, shape):
        return nc.dram_tensor(name, list(shape), F32, kind="ExternalInput").ap()

    x_d = din("x", [NB, S, D])
    c_d = din("c", [NB, D])
    w_ada = din("w_ada", [D, 6 * D])
    b_ada = din("b_ada", [1, 6 * D])
    norm1_g = din("norm1_g", [1, D])
    w_in = din("w_in", [D, INW])
    lam_d = din("lam4", [4, 64])
    subln_g = din("subln_g", [1, 128])
    w_ap = din("w_attn_proj", [AW, D])
    ln_g = din("sgu_ln_g", [1, D])
    ln_b = din("sgu_ln_b", [1, D])
    w_s_d = din("sgu_w_s", [G, 128, 128])
    b_s_d = din("sgu_b_s", [1, G * 128])
    w_sp = din("w_sgu_proj", [D, D])
    w_out = din("w_out", [D, D])
    norm2_g = din("norm2_g", [1, D])
    w_rt = din("w_router", [D, NR])
    b_rt = din("b_router", [1, NR])
    w_gu = din("w_expert_gate_up", [NE, D, 2 * DE])
    w_dn = din("w_expert_down", [NE, DE, D])
    final_g = din("final_g", [1, D])
    y_d = nc.dram_tensor("y", [NB, S, D], F32, kind="ExternalOutput").ap()

    with ExitStack() as st:
        K = Kern(nc, st)
        pe, act, dve, pool, sp = K.pe, K.act, K.dve, K.pool, K.sp

        banks = [K.ps("bank%d" % i, [128, 512], F32) for i in range(8)]
        bbuf = [GB("bank%d" % i) for i in range(8)]

        ident = K.sb("ident", [128, 128], F32); b_ident = GB("ident")
        identb = K.sb("identb", [128, 128], BF16); b_identb = GB("identb")
        bcA = K.sb("bcA", [128, D], F32); b_bcA = GB("bcA")
        bcB = K.sb("bcB", [128, D], F32); b_bcB = GB("bcB")
        fgbc = K.sb("fgbc", [128, D], F32); b_fgbc = GB("fgbc")
        subg = K.sb("subg", [128, 128], F32); b_subg = GB("subg")
        lam_t = K.sb("lam_t", [128, 8], F32); b_lam = GB("lam")
        brt = K.sb("brt", [128, NR], F32); b_brt = GB("brt")
        wrt = K.sb("wrt", [128, DC, NR], BF16); b_wrt = GB("wrt")
        mod_scr = nc.dram_tensor("mod_scr", [NB, 6 * D], F32, kind="Internal").ap(); b_modscr = GB("mod_scr")
        cw_scr = nc.dram_tensor("cw_scr", [NE, S], F32, kind="Internal").ap(); b_cwscr = GB("cw_scr")

        HW_ = DC * S // 2
        VW = TC * NH * 130 // 2 + 2
        o_hT = 0
        o_ya = o_hT + HW_
        o_z = o_ya + HW_
        ZW = max(HW_, VW)
        o_y = o_z + ZW
        YW = max(HW_, 2 * S + 4096)
        o_w = o_y + YW
        WW = 12 * 1024
        ARENA = o_w + WW
        arena = K.sb("arena", [128, ARENA], F32)

        def view(off, words, dt, pattern=None, **kw):
            a = arena[:, off:off + words]
            if dt is BF16:
                a = a.bitcast(BF16)
            if pattern:
                a = a.rearrange(pattern, **kw)
            return a

        hT = view(o_hT, HW_, BF16, "p (c s) -> p c s", c=DC); b_hT = GB("hT")
        yaT = view(o_ya, HW_, BF16, "p (c s) -> p c s", c=NH) if NH == DC else None
        assert NH == DC
        b_yaT = GB("yaT")
        zT = view(o_z, HW_, BF16, "p (c s) -> p c s", c=G); b_zT = GB("zT")
        vaug = view(o_z, (TC * NH * 130) // 2, BF16, "p (t h e) -> p t h e", t=TC, h=NH); b_vaug = GB("vaug")
        yT = view(o_y, HW_, BF16, "p (c s) -> p c s", c=DC); b_yT = GB("yT")
        x1 = view(o_ya, TC * D, F32, "p (t d) -> p t d", t=TC); b_x1 = [GB("x1_%d" % i) for i in range(TC)]
        assert TC * D <= HW_ + ZW

        def mm(out, lhsT, rhs, start, stop, reads, writes, sig=None):
            if sig is None:
                sig = stop
            pe.op(lambda e: e.matmul(out, lhsT=lhsT, rhs=rhs, start=start, stop=stop), reads, writes, sig=sig)

        def tr(out, in_, idt, reads, writes, sig=True):
            pe.op(lambda e: e.transpose(out, in_, idt), reads, writes, sig=sig)

        def A(out, in_, func, reads, writes, **kw):
            act.op(lambda e: e.activation(out=out, in_=in_, func=func, **kw), reads, writes)

        def TT(eng, out, in0, in1, op, reads, writes):
            eng.op(lambda e: e.tensor_tensor(out=out, in0=in0, in1=in1, op=op), reads, writes)

        def TS(eng, out, in0, s1, s2, op0, op1, reads, writes, **kw):
            if op1 is None:
                eng.op(lambda e: e.tensor_scalar(out=out, in0=in0, scalar1=s1, scalar2=None, op0=op0, **kw), reads, writes)
            else:
                eng.op(lambda e: e.tensor_scalar(out=out, in0=in0, scalar1=s1, scalar2=s2, op0=op0, op1=op1, **kw), reads, writes)

        def STT(eng, out, in0, scalar, in1, op0, op1, reads, writes):
            eng.op(lambda e: e.scalar_tensor_tensor(out=out, in0=in0, scalar=scalar, in1=in1, op0=op0, op1=op1), reads, writes)

        def CP(eng, out, in_, reads, writes):
            if eng is act:
                act.op(lambda e: e.copy(out=out, in_=in_), reads, writes)
            else:
                eng.op(lambda e: e.tensor_copy(out=out, in_=in_), reads, writes)

        def LD(q, out, in_, anchor, reads=(), writes=None):
            q.dma(lambda e: e.dma_start(out=out, in_=in_), anchor, reads, [anchor] if writes is None else writes)

        dbg_outs = {}

        def dump(name, ap_, shape, dt, rb):
            if not getattr(cfg, "debug", False):
                return
            d = nc.dram_tensor("dbg_" + name, list(shape), dt, kind="ExternalOutput").ap()
            bb = GB("dbg_" + name)
            sp.dma(lambda e: e.dma_start(out=d, in_=ap_), bb, rb, [bb])
            dbg_outs[name] = d

        def rsqrt_col(dst, src, scale, tmp, rb, wb):
            A(tmp, src, AF.Sqrt, rb, wb, bias=epsc[:src.shape[0], 0:1], scale=scale)
            dve.op(lambda e: e.reciprocal(out=dst, in_=tmp), wb, wb)

        epsc = K.sb("epsc", [128, 1], F32); b_eps = GB("epsc")
        pool.op(lambda e: e.memset(epsc[:], EPS), (), [b_eps])
        pool.op(lambda e: e.memset(ident[:], 1.0), (), [b_ident])
        pool.op(lambda e: e.affine_select(out=ident[:], in_=ident[:], pattern=[[-1, 128]], compare_op=ALU.is_equal,
                                          fill=0.0, base=0, channel_multiplier=1), [b_ident], [b_ident])
        CP(dve, identb[:], ident[:], [b_ident], [b_identb])
        LD(sp, fgbc[:], final_g.broadcast_to([128, D]), b_fgbc)
        LD(sp, subg[:], subln_g.broadcast_to([128, 128]), b_subg)
        LD(sp, brt[:], b_rt.broadcast_to([128, NR]), b_brt)
        TS(dve, subg[:], subg[:], 1.0 - cfg.lam_init, None, ALU.mult, None, [b_subg], [b_subg])
        LD(pool, wrt[:], w_rt.rearrange("(c p) n -> p c n", p=128), b_wrt)
        lamw = K.sb("lamw", [128, 4, 64], F32); b_lamw = GB("lamw")
        LD(sp, lamw[:].rearrange("p a b -> p (a b)"), lam_d.rearrange("a b -> (a b)").rearrange("(o n) -> o n", o=1).broadcast_to([128, 256]), b_lamw)
        TT(dve, lamw[:, 0, :], lamw[:, 0, :], lamw[:, 1, :], ALU.mult, [b_lamw], [b_lamw])
        TT(dve, lamw[:, 2, :], lamw[:, 2, :], lamw[:, 3, :], ALU.mult, [b_lamw], [b_lamw])
        dve.op(lambda e: e.tensor_reduce(out=lam_t[:, 0:1], in_=lamw[:, 0, :], axis=AX.X, op=ALU.add), [b_lamw], [b_lam])
        dve.op(lambda e: e.tensor_reduce(out=lam_t[:, 1:2], in_=lamw[:, 2, :], axis=AX.X, op=ALU.add), [b_lamw], [b_lam])
        A(lam_t[:, 2:4], lam_t[:, 0:2], AF.Exp, [b_lam], [b_lam])
        TT(dve, lam_t[:, 4:5], lam_t[:, 2:3], lam_t[:, 3:4], ALU.subtract, [b_lam], [b_lam])
        TS(dve, lam_t[:, 5:6], lam_t[:, 4:5], cfg.lam_init, -1.0, ALU.add, ALU.mult, [b_lam], [b_lam])

        def wview(off_words, words, dt, pattern=None, rows=None, **kw):
            assert off_words + words <= WW, (off_words, words, WW)
            a_ = arena[:, o_w + off_words:o_w + off_words + words] if rows is None else arena[0:rows, o_w + off_words:o_w + off_words + words]
            if dt is BF16:
                a_ = a_.bitcast(BF16)
            if pattern:
                a_ = a_.rearrange(pattern, **kw)
            return a_

        def yview(off_words, words, dt, pattern=None, **kw):
            assert off_words + words <= YW, (off_words, words, YW)
            return view(o_y + off_words, words, dt, pattern, **kw)

        modv = view(o_y, 6 * D, F32)[0:NB, :]; b_modv = GB("modv")
        c_sb = view(o_y + 6 * D, D, F32)[0:NB, :]; b_c = GB("c_sb")
        sgc = view(o_y + 7 * D, D, F32)[0:NB, :]
        g2row = view(o_hT, 2 * D, F32, "p (a d) -> p a d", a=2)[0:NB]; b_g2row = GB("g2row")
        siluT = wview(0, DC * NB // 2 + 1, BF16)[:, 0:DC * NB].rearrange("p (c b) -> p c b", c=DC); b_siluT = GB("siluT")
        LD(sp, c_sb, c_d, b_c)
        A(sgc, c_sb, AF.Sigmoid, [b_c], [b_c])
        TT(dve, c_sb, c_sb, sgc, ALU.mult, [b_c], [b_c])
        pt = banks[0]
        for dc in range(DC):
            tr(pt[:, dc * NB:(dc + 1) * NB], c_sb[:, dc * 128:(dc + 1) * 128], ident[0:NB, 0:NB], [b_c, b_ident], [bbuf[0]])
        CP(dve, siluT.rearrange("p c b -> p (c b)"), pt[:, 0:DC * NB], [bbuf[0]], [b_siluT])
        LD(sp, modv, b_ada.broadcast_to([NB, 6 * D]), b_modv)
        NBLK = 6 * D // 512
        wa = [wview(64 + i * (DC * 256), DC * 256, BF16, "p (c n) -> p c n", c=DC) for i in range(2)]
        b_wa = [GB("wa0"), GB("wa1")]
        for blk in range(NBLK):
            i = blk % 2
            LD(pool, wa[i], w_ada[:, blk * 512:(blk + 1) * 512].rearrange("(c p) n -> p c n", p=128), b_wa[i])
            bk = 1 + (blk % 2)
            for dc in range(DC):
                mm(banks[bk][0:NB, :], siluT[:, dc, :], wa[i][:, dc, :], dc == 0, dc == DC - 1, [b_siluT, b_wa[i]], [bbuf[bk]])
            TT(dve, modv[:, blk * 512:(blk + 1) * 512], modv[:, blk * 512:(blk + 1) * 512], banks[bk][0:NB, :], ALU.add,
               [b_modv, bbuf[bk]], [b_modv])
        LD(sp, g2row[:, 0, :], norm1_g.broadcast_to([NB, D]), b_g2row)
        LD(sp, g2row[:, 1, :], norm2_g.broadcast_to([NB, D]), b_g2row)
        for (gi, off) in ((0, 1), (1, 4)):
            STT(dve, modv[:, off * D:(off + 1) * D], modv[:, off * D:(off + 1) * D], 1.0, g2row[:, gi, :], ALU.add, ALU.mult,
                [b_modv, b_g2row], [b_modv])
        sp.dma(lambda e: e.dma_start(out=mod_scr, in_=modv), b_modv, [b_modv], [b_modscr])
        dump("modv", modv, [NB, 6 * D], F32, [b_modv])
        K.barrier()

        def bcast_mod(b, idx, dst, b_dst):
            sp.dma(lambda e: e.dma_start(out=dst[:], in_=mod_scr[b:b + 1, idx * D:(idx + 1) * D].broadcast_to([128, D])),
                   b_dst, [b_modscr], [b_dst])

        def norm_to_hT(b, src_fn, b_src_fn, tag):
            xt = [wview(i * D, D, F32) for i in range(2)]; b_xt = [GB(tag + "xt0"), GB(tag + "xt1")]
            junk = wview(2 * D, D, F32); b_junk = GB(tag + "junk")
            hb = [wview(3 * D + i * (D // 2), D // 2, BF16) for i in range(2)]; b_hb = [GB(tag + "hb0"), GB(tag + "hb1")]
            st_ = wview(4 * D, 8, F32); b_st = GB(tag + "st")
            for tc in range(TC):
                i = tc % 2
                src, b_src = src_fn(tc, xt[i], b_xt[i])
                A(junk, src, AF.Square, [b_src], [b_junk, b_st], accum_out=st_[:, 0:1])
                rsqrt_col(st_[:, 2:3], st_[:, 0:1], 1.0 / D, st_[:, 1:2], [b_st, b_eps], [b_st])
                STT(dve, junk, src, st_[:, 2:3], bcA[:], ALU.mult, ALU.mult, [b_src, b_st, b_bcA], [b_junk])
                TT(pool, hb[i], junk, bcB[:], ALU.add, [b_junk, b_bcB], [b_hb[i]])
                bk = tc % 2
                ptb = banks[bk][:].bitcast(BF16)
                for dc in range(DC):
                    tr(ptb[:, dc * 128:(dc + 1) * 128], hb[i][:, dc * 128:(dc + 1) * 128], identb[:], [b_hb[i], b_identb], [bbuf[bk]],
                       sig=(dc == DC - 1))
                CP(act, hT[:, :, tc * 128:(tc + 1) * 128], ptb[:, 0:DC * 128].rearrange("p (c t) -> p c t", c=DC), [bbuf[bk]], [b_hT])

        for b in range(NB):
            bcast_mod(b, 1, bcA, b_bcA)
            bcast_mod(b, 0, bcB, b_bcB)

            def src_x(tc, xt_i, b_xt_i):
                LD(sp, xt_i, x_d[b, tc * 128:(tc + 1) * 128, :], b_xt_i)
                return xt_i, b_xt_i
            norm_to_hT(b, src_x, None, "n1")
            if b == 0:
                dump("hT", hT, [128, DC, S], BF16, [b_hT])
            K.barrier()

            wv = wview(0, DC * AW // 2, BF16, "p (c n) -> p c n", c=DC); b_wv = GB("wv")
            LD(pool, wv, w_in[:, 2 * AW:3 * AW].rearrange("(c p) n -> p c n", p=128), b_wv)
            pool.op(lambda e: e.memset(vaug[:, :, :, 128:130], 1.0), (), [b_vaug])
            VB = min(512, AW)
            HPB = VB // 128
            for tc in range(TC):
                for hb_ in range(AW // VB):
                    bk = (tc * (AW // VB) + hb_) % 4
                    for dc in range(DC):
                        mm(banks[bk][:, 0:VB], hT[:, dc, tc * 128:(tc + 1) * 128], wv[:, dc, hb_ * VB:(hb_ + 1) * VB], dc == 0, dc == DC - 1,
                           [b_hT, b_wv], [bbuf[bk]])
                    eng = act if (hb_ % 2 == 0) else dve
                    CP(eng, vaug[:, tc, hb_ * HPB:(hb_ + 1) * HPB, 0:128], banks[bk][:, 0:VB].rearrange("p (h e) -> p h e", h=HPB), [bbuf[bk]], [b_vaug])
            if b == 0:
                dump("vaug", vaug, [128, TC, NH, 130], BF16, [b_vaug])
            K.barrier()

            Ttab = yview(0, 2 * S, F32); b_T = GB("Ttab")
            pool.op(lambda e: e.iota(Ttab, pattern=[[1, 2 * S]], base=-S, channel_multiplier=-1,
                                     allow_small_or_imprecise_dtypes=True), (), [b_T])
            Ttab2 = yview(2 * S, 2 * S, F32)
            pool.op(lambda e: e.iota(Ttab2, pattern=[[-1, 2 * S]], base=S, channel_multiplier=1,
                                     allow_small_or_imprecise_dtypes=True), (), [b_T])
            TT(dve, Ttab, Ttab, Ttab2, ALU.max, [b_T], [b_T])
            wqk = [wview(i * (DC * 128), DC * 128, BF16, "p (c n) -> p c n", c=DC) for i in range(2)]
            b_wqk = [GB("wqk0"), GB("wqk1")]
            qo = 2 * DC * 128
            qT = [wview(qo + i * (S // 2), S // 2, BF16) for i in range(2)]; b_qT = [GB("qT0"), GB("qT1")]
            ko = qo + S
            kT = [wview(ko + i * (S // 2), S // 2, BF16) for i in range(2)]; b_kT = [GB("kT0"), GB("kT1")]
            to = ko + S
            NTB = 4
            tmpb = [wview(to + i * QT, QT, F32) for i in range(NTB)]; b_tmp = [GB("tmp%d" % i) for i in range(NTB)]
            po = to + NTB * QT
            pTb = [wview(po + i * (QT // 2), QT // 2, BF16) for i in range(NTB)]; b_pT = [GB("pT%d" % i) for i in range(NTB)]
            so = po + NTB * (QT // 2)
            NQC = QT // 128
            nab = (2 * NQC + 2) // 3
            accs = [wview(so, nab * 387, F32, "p (k c) -> p k c", k=nab)] * 2
            b_accs = [GB("accs0")] * 2
            so += nab * 387
            a_h = [yview(2 * S + i * (TC * 128), TC * 128, F32, "p (t e) -> p t e", t=TC) for i in range(2)]
            b_ah = [GB("a_h0"), GB("a_h1")]
            ssq = [wview(so + i * 3 * TC, TC, F32) for i in range(2)]
            c2e = [wview(so + i * 3 * TC + TC, TC, F32) for i in range(2)]
            rstd = [wview(so + i * 3 * TC + 2 * TC, TC, F32) for i in range(2)]
            b_st3 = [GB("st3_0"), GB("st3_1")]
            so += 6 * TC
            sm = wview(so, 8, F32); b_sm = GB("sm")
            a_j = [wview(so + 8 + 256 + i * 128, 128, F32) for i in range(2)]; b_aj = [GB("a_j0"), GB("a_j1")]
            a_k = wview(so + 8 + 512, 128, F32); b_ak = GB("a_k")
            yh2 = [wview(so + 8 + 128 + i * 64, 64, BF16) for i in range(2)]; b_yh2 = [GB("yh0"), GB("yh1")]
            assert so + 8 + 640 <= WW, so
            SB_ = [3, 4, 5, 6]

            def emit_proj(h):
                i = h % 2
                LD(pool, wqk[i][:, :, 0:128], w_in[:, h * 128:(h + 1) * 128].rearrange("(c p) n -> p c n", p=128), b_wqk[i])
                LD(pool, wqk[i][:, :, 128:256], w_in[:, AW + h * 128:AW + (h + 1) * 128].rearrange("(c p) n -> p c n", p=128), b_wqk[i])
                for t5 in range(S // QT):
                    for (which, dstT, b_dst) in ((0, qT[i], b_qT[i]), (1, kT[i], b_kT[i])):
                        for dc in range(DC):
                            mm(banks[7][:, 0:QT], wqk[i][:, dc, which * 128:(which + 1) * 128], hT[:, dc, t5 * QT:(t5 + 1) * QT],
                               dc == 0, dc == DC - 1, [b_wqk[i], b_hT], [bbuf[7]])
                        CP(act if which == 0 else dve, dstT[:, t5 * QT:(t5 + 1) * QT], banks[7][:, 0:QT], [bbuf[7]], [b_dst])

            def acc(m, qc):
                a_ = m * NQC + qc
                bk = a_ // 3
                return banks[bk][:, (a_ % 3) * 129:(a_ % 3) * 129 + 129], bbuf[bk], (a_ % 3 == 0)

            def emit_score_pair(h, p, qt, kc):
                i = h % 2
                for m in range(2):
                    sbk = SB_[(2 * p + m) % 4]
                    mm(banks[sbk][:, 0:QT], kT[i][m * 64:(m + 1) * 64, kc * 128:(kc + 1) * 128],
                       qT[i][m * 64:(m + 1) * 64, qt * QT:(qt + 1) * QT], True, True, [b_kT[i], b_qT[i]], [bbuf[sbk]])
                off = qt * QT - kc * 128 + S
                for m in range(2):
                    sbk = SB_[(2 * p + m) % 4]
                    ti = (2 * p + m) % NTB
                    STT(dve, tmpb[ti], Ttab[:, off:off + QT], -8.0 * cfg.slopes[h], banks[sbk][:, 0:QT], ALU.mult, ALU.add,
                        [b_T, bbuf[sbk]], [b_tmp[ti]])
                    A(pTb[ti], tmpb[ti], AF.Exp, [b_tmp[ti]], [b_pT[ti]], scale=0.125)

            def emit_av_pair(h, p, qt, kc):
                for m in range(2):
                    ti = (2 * p + m) % NTB
                    for qc in range(NQC):
                        a_ap, a_b, first = acc(m, qc)
                        pe.op((lambda a_ap=a_ap, ti=ti, qc=qc, kc=kc, h=h, first=first: (lambda e: e.matmul(
                            a_ap, lhsT=pTb[ti][:, qc * 128:(qc + 1) * 128], rhs=vaug[:, kc, h, 0:129],
                            start=(kc == 0 and first), stop=(kc == TC - 1), skip_group_check=True)))(),
                            [b_pT[ti], b_vaug], [a_b], sig=(qc == NQC - 1))
                if kc == TC - 1:
                    emit_post(h, qt)

            def emit_post(h, qt):
                i = h % 2
                par = qt % 2
                for bk in range(nab):
                    ncol = min(3, 2 * NQC - 3 * bk) * 129
                    CP(dve, accs[par][:, bk, 0:ncol], banks[bk][:, 0:ncol], [bbuf[bk]], [b_accs[par]])
                for qc in range(NQC):
                    slot = qt * NQC + qc
                    a0_, a1_ = qc, NQC + qc
                    c0 = (a0_ % 3) * 129; c1 = (a1_ % 3) * 129
                    o0 = accs[par][:, a0_ // 3, c0:c0 + 128]; l0 = accs[par][:, a0_ // 3, c0 + 128:c0 + 129]
                    o1 = accs[par][:, a1_ // 3, c1:c1 + 128]; l1 = accs[par][:, a1_ // 3, c1 + 128:c1 + 129]
                    at = a_h[i][:, slot, :]
                    TS(pool, at, o0, l1, 0.0, ALU.mult, ALU.add, [b_accs[par]], [b_ah[i]])
                    TT(pool, sm[:, 0:1], l0, lam_t[:, 5:6], ALU.mult, [b_accs[par], b_lam], [b_sm])
                    TS(pool, a_k, o1, sm[:, 0:1], 0.0, ALU.mult, ALU.add, [b_accs[par], b_sm], [b_ak])
                    TT(pool, at, at, a_k, ALU.add, [b_ak, b_ah[i]], [b_ah[i]])
                    TS(pool, sm[:, 1:2], l0, l1, 0.0, ALU.mult, ALU.add, [b_accs[par]], [b_sm])
                    TS(pool, c2e[i][:, slot:slot + 1], sm[:, 1:2], sm[:, 1:2], EPS, ALU.mult, ALU.mult, [b_sm], [b_st3[i]])

            def emit_norm(h):
                i = h % 2
                for slot in range(TC):
                    j = slot % 2
                    TT(pool, a_j[j], a_h[i][:, slot, :], a_h[i][:, slot, :], ALU.mult, [b_ah[i]], [b_aj[j]])
                    dve.op((lambda i=i, slot=slot, j=j: (lambda e: e.tensor_reduce(out=ssq[i][:, slot:slot + 1], in_=a_j[j], axis=AX.X, op=ALU.add)))(),
                           [b_aj[j]], [b_st3[i]])
                TS(pool, ssq[i], ssq[i], 1.0 / 128, 0.0, ALU.mult, ALU.add, [b_st3[i]], [b_st3[i]])
                TT(pool, ssq[i], ssq[i], c2e[i], ALU.add, [b_st3[i]], [b_st3[i]])
                A(c2e[i], ssq[i], AF.Sqrt, [b_st3[i]], [b_st3[i]])
                dve.op((lambda i=i: (lambda e: e.reciprocal(out=rstd[i], in_=c2e[i])))(), [b_st3[i]], [b_st3[i]])
                ptb = banks[7][:].bitcast(BF16)
                for slot in range(TC):
                    j = slot % 2
                    TS(pool, a_k, a_h[i][:, slot, :], rstd[i][:, slot:slot + 1], 0.0, ALU.mult, ALU.add, [b_ah[i], b_st3[i]], [b_ak])
                    TT(pool, yh2[j], a_k, subg[:], ALU.mult, [b_ak, b_subg], [b_yh2[j]])
                    tr(ptb[:, 0:128], yh2[j], identb[:], [b_yh2[j], b_identb], [bbuf[7]])
                    CP(dve, yaT[:, h, slot * 128:(slot + 1) * 128], ptb[:, 0:128], [bbuf[7]], [b_yaT])

            pairs = [(qt, kc) for qt in range(S // QT) for kc in range(TC)]
            npair = len(pairs)
            emit_proj(0)
            for h in range(NH):
                for p in range(npair + 1):
                    if p < npair:
                        emit_score_pair(h, p, *pairs[p])
                    if p == min(12, npair - 1) and h >= 1:
                        emit_norm(h - 1)
                    if p == npair // 2 and h + 1 < NH:
                        emit_proj(h + 1)
                    if p >= 1:
                        emit_av_pair(h, p - 1, *pairs[p - 1])
            emit_norm(NH - 1)
            if b == 0:
                dump("yaT", yaT, [128, NH, S], BF16, [b_yaT])
            K.barrier()

            NQC_ = QT // 128
            wu = wview(0, DC * D // 2, BF16, "p (c n) -> p c n", c=DC); b_wu = GB("wu")
            wsw = wview(DC * D // 2, DC * D // 2, BF16, "p (c n) -> p c n", c=DC); b_wsw = GB("wsw")
            LD(pool, wu, w_in[:, 3 * AW:3 * AW + D].rearrange("(c p) n -> p c n", p=128), b_wu)
            LD(pool, wsw, w_in[:, 3 * AW + D:3 * AW + 2 * D].rearrange("(c p) n -> p c n", p=128), b_wsw)
            o2 = DC * D
            uTt = wview(o2, G * QT // 2, BF16, "p (g t) -> p g t", g=G); b_uTt = GB("uTt")
            sfulls = [wview(o2 + G * QT // 2 + i * D, D, F32) for i in range(2)]; b_sfulls = [GB("sfull0"), GB("sfull1")]
            lnG = yview(0, D, F32); b_lnG = GB("lnG")
            lnB = yview(D, D, F32); b_lnB = GB("lnB")
            bsbc = yview(2 * D, G * 128, F32); b_bsbc = GB("bsbc")
            tmp2 = yview(2 * D + G * 128, G * 128, F32); b_tmp2 = GB("tmp2")
            g1s = [yview(2 * D + 2 * G * 128 + i * 512, 512, F32) for i in range(2)]; b_g1s = [GB("g1_0"), GB("g1_1")]
            g2s = [yview(YW - 1024 + i * 512, 512, F32) for i in range(2)]; b_g2s = [GB("g2_0"), GB("g2_1")]
            gcnt = [0]
            wsT = yview(2 * D + 2 * G * 128 + 1024, G * 64, BF16, "p (g t) -> p g t", g=G); b_wsT = GB("wsT")
            st4s = [yview(2 * D + 2 * G * 128 + 1024 + G * 64 + i * 4 + 136, 4, F32) for i in range(2)]
            st4s = [yview(2 * D + 2 * G * 128 + 1024 + G * 64 + 136 + i * 8, 8, F32) for i in range(2)]; b_st4s = [GB("st4_0"), GB("st4_1")]
            vss = [yview(2 * D + 2 * G * 128 + 1024 + G * 64 + 160 + i * (D // 2), D // 2, BF16) for i in range(2)]; b_vss = [GB("vs0"), GB("vs1")]
            assert 2 * D + 2 * G * 128 + 1024 + G * 64 + 160 + D <= YW - 1024
            wst = yview(2 * D + 2 * G * 128 + 1024 + G * 64 + 8, 128, F32); b_wst = GB("wst")
            LD(sp, lnG, ln_g.broadcast_to([128, D]), b_lnG)
            LD(sp, lnB, ln_b.broadcast_to([128, D]), b_lnB)
            LD(sp, bsbc, b_s_d.broadcast_to([128, G * 128]), b_bsbc)
            for g in range(G):
                LD(sp, wst, w_s_d[g], b_wst)
                tr(banks[0][:, 0:128], wst, ident[:], [b_wst, b_ident], [bbuf[0]])
                CP(dve, wsT[:, g, :], banks[0][:, 0:128], [bbuf[0]], [b_wsT])

            C_G = 0.044715 ** 0.5

            def gelu_A(gi, src, n, rb):
                A(g1s[gi][:, 0:n], src, AF.Square, rb, [b_g1s[gi]], scale=C_G)
                STT(dve, g1s[gi][:, 0:n], g1s[gi][:, 0:n], 1.0, src, ALU.add, ALU.mult, [b_g1s[gi]] + rb, [b_g1s[gi]])

            def gelu_B(gi, dst, src, n, rb, wb):
                A(g2s[gi][:, 0:n], g1s[gi][:, 0:n], AF.Sigmoid, [b_g1s[gi]], [b_g2s[gi]], scale=1.5957691216057308)
                TT(dve, dst, g2s[gi][:, 0:n], src, ALU.mult, [b_g2s[gi]] + rb, wb)

            for t5 in range(S // QT):
                def uA(g):
                    bk = g % 2
                    for dc in range(DC):
                        mm(banks[bk][:, 0:QT], wu[:, dc, g * 128:(g + 1) * 128], hT[:, dc, t5 * QT:(t5 + 1) * QT], dc == 0, dc == DC - 1,
                           [b_wu, b_hT], [bbuf[bk]])
                    gelu_A(g % 2, banks[bk][:, 0:QT], QT, [bbuf[bk]])
                uA(0)
                for g in range(G):
                    if g + 1 < G:
                        uA(g + 1)
                    gelu_B(g % 2, uTt[:, g, :], banks[g % 2][:, 0:QT], QT, [bbuf[g % 2]], [b_uTt])
                for tcl in range(NQC_):
                    tc = t5 * NQC_ + tcl
                    NHB = D // DB
                    sfull, b_sfull = sfulls[tc % 2], b_sfulls[tc % 2]
                    vs, b_vs = vss[tc % 2], b_vss[tc % 2]
                    st4, b_st4 = st4s[tc % 2], b_st4s[tc % 2]

                    def sA(hb_):
                        bk = 2 + hb_ % 2
                        for dc in range(DC):
                            mm(banks[bk][:, 0:DB], hT[:, dc, tc * 128:(tc + 1) * 128], wsw[:, dc, hb_ * DB:(hb_ + 1) * DB], dc == 0, dc == DC - 1,
                               [b_hT, b_wsw], [bbuf[bk]])
                        gelu_A(hb_ % 2, banks[bk][:, 0:DB], DB, [bbuf[bk]])
                    sA(0)
                    for hb_ in range(NHB):
                        if hb_ + 1 < NHB:
                            sA(hb_ + 1)
                        gelu_B(hb_ % 2, sfull[:, hb_ * DB:(hb_ + 1) * DB], banks[2 + hb_ % 2][:, 0:DB], DB, [bbuf[2 + hb_ % 2]], [b_sfull])
                    dve.op((lambda st4=st4, sfull=sfull: (lambda e: e.tensor_reduce(out=st4[:, 0:1], in_=sfull, axis=AX.X, op=ALU.add)))(), [b_sfull], [b_st4])
                    TS(dve, st4[:, 1:2], st4[:, 0:1], -1.0 / D, None, ALU.mult, None, [b_st4], [b_st4])
                    TS(dve, sfull, sfull, st4[:, 1:2], None, ALU.add, None, [b_sfull, b_st4], [b_sfull])
                    A(tmp2[:, 0:D] if G * 128 >= D else tmp2, sfull, AF.Square, [b_sfull], [b_tmp2, b_st4], accum_out=st4[:, 2:3])
                    rsqrt_col(st4[:, 4:5], st4[:, 2:3], 1.0 / D, st4[:, 3:4], [b_st4, b_eps], [b_st4])
                    STT(dve, sfull, sfull, st4[:, 4:5], lnG, ALU.mult, ALU.mult, [b_sfull, b_st4, b_lnG], [b_sfull])
                    TT(pool, vs, sfull, lnB, ALU.add, [b_sfull, b_lnB], [b_vs])
                    for g in range(G):
                        bk = 4 + g // 4
                        pe.op((lambda bk=bk, g=g, vs=vs: (lambda e: e.matmul(banks[bk][:, (g % 4) * 128:(g % 4) * 128 + 128], lhsT=vs[:, g * 128:(g + 1) * 128],
                                                                      rhs=wsT[:, g, :], start=True, stop=True, skip_group_check=True)))(),
                              [b_vs, b_wsT], [bbuf[bk]])
                    for gb in range((G + 3) // 4):
                        ng = min(4, G - gb * 4)
                        TT(dve, tmp2[:, gb * 512:gb * 512 + ng * 128], banks[4 + gb][:, 0:ng * 128], bsbc[:, gb * 512:gb * 512 + ng * 128], ALU.add,
                           [bbuf[4 + gb], b_bsbc], [b_tmp2])
                    TT(pool, zT[:, :, tc * 128:(tc + 1) * 128], tmp2.rearrange("p (g t) -> p g t", g=G), uTt[:, :, tcl * 128:(tcl + 1) * 128], ALU.mult,
                       [b_tmp2, b_uTt], [b_zT])
            if b == 0:
                dump("zT", zT, [128, G, S], BF16, [b_zT])
            K.barrier()

            CW = DC * 64
            wsl = [[wview(i * 4 * CW + k * CW, CW, BF16, "p (c n) -> p c n", c=DC) for k in range(4)] for i in range(2)]
            b_wsl = [GB("wsl0"), GB("wsl1")]
            so5 = 8 * CW
            s12 = [[wview(so5 + (i * 2 + k) * QT, QT, F32) for k in range(2)] for i in range(2)]
            b_s12 = [[GB("s12_%d%d" % (i, k)) for k in range(2)] for i in range(2)]
            it = 0

            def load_s5(j):
                i = j % 2
                srcs = (w_ap[:, j * 128:(j + 1) * 128], w_in[:, 3 * AW + 2 * D + j * 128:3 * AW + 2 * D + (j + 1) * 128],
                        w_sp[:, j * 128:(j + 1) * 128], w_in[:, 3 * AW + 3 * D + j * 128:3 * AW + 3 * D + (j + 1) * 128])
                for k in range(4):
                    LD(pool, wsl[i][k], srcs[k].rearrange("(c p) n -> p c n", p=128), b_wsl[i])
            load_s5(0)
            for j in range(DC):
                i = j % 2
                if j + 1 < DC:
                    load_s5(j + 1)
                for t5 in range(S // QT):
                    p = (it % 2) * 4
                    ii = it % 2
                    it += 1
                    tok = slice(t5 * QT, (t5 + 1) * QT)
                    opnds = ((yaT, b_yaT, NH), (hT, b_hT, DC), (zT, b_zT, G), (hT, b_hT, DC))
                    for k in range(4):
                        src, b_src, nk = opnds[k]
                        for kc in range(nk):
                            mm(banks[p + k][:, 0:QT], wsl[i][k][:, kc, :], src[:, kc, tok], kc == 0, kc == nk - 1, [b_wsl[i], b_src], [bbuf[p + k]])
                    A(s12[ii][0], banks[p + 1][:, 0:QT], AF.Sigmoid, [bbuf[p + 1]], [b_s12[ii][0]])
                    A(s12[ii][1], banks[p + 3][:, 0:QT], AF.Sigmoid, [bbuf[p + 3]], [b_s12[ii][1]])
                    TT(dve, s12[ii][0], s12[ii][0], banks[p + 0][:, 0:QT], ALU.mult, [b_s12[ii][0], bbuf[p + 0]], [b_s12[ii][0]])
                    TT(dve, s12[ii][1], s12[ii][1], banks[p + 2][:, 0:QT], ALU.mult, [b_s12[ii][1], bbuf[p + 2]], [b_s12[ii][1]])
                    TT(pool, yT[:, j, tok], s12[ii][0], s12[ii][1], ALU.add, [b_s12[ii][0], b_s12[ii][1]], [b_yT])
            if b == 0:
                dump("yT", yT, [128, DC, S], BF16, [b_yT])
            K.barrier()

            bcast_mod(b, 2, bcA, b_bcA)
            wo = wview(0, DC * D // 2, BF16, "p (c n) -> p c n", c=DC); b_wo = GB("wo")
            LD(pool, wo, w_out.rearrange("(c p) n -> p c n", p=128), b_wo)
            xt6 = [wview(DC * D // 2 + i * D, D, F32) for i in range(2)]; b_xt6 = [GB("xt6_0"), GB("xt6_1")]
            tm6 = wview(DC * D // 2 + 2 * D, D, F32); b_tm6 = GB("tm6")
            for tc in range(TC):
                i = tc % 2
                LD(sp, xt6[i], x_d[b, tc * 128:(tc + 1) * 128, :], b_xt6[i])
                for hb_ in range(D // DB):
                    bk = (tc % 2) * 2 + hb_ % 2
                    for kc in range(DC):
                        mm(banks[bk][:, 0:DB], yT[:, kc, tc * 128:(tc + 1) * 128], wo[:, kc, hb_ * DB:(hb_ + 1) * DB], kc == 0, kc == DC - 1,
                           [b_yT, b_wo], [bbuf[bk]])
                    TT(dve, tm6[:, hb_ * DB:(hb_ + 1) * DB], banks[bk][:, 0:DB], bcA[:, hb_ * DB:(hb_ + 1) * DB], ALU.mult, [bbuf[bk], b_bcA], [b_tm6])
                TT(pool, x1[:, tc, :], tm6, xt6[i], ALU.add, [b_tm6, b_xt6[i]], [b_x1[tc]])
            if b == 0:
                dump("x1", x1, [128, TC, D], F32, b_x1)
            K.barrier()

            bcast_mod(b, 4, bcA, b_bcA)
            bcast_mod(b, 3, bcB, b_bcB)
            norm_to_hT(b, lambda tc, xt_i, b_xt_i: (x1[:, tc, :], b_x1[tc]), None, "n2")
            if b == 0:
                dump("h2T", hT, [128, DC, S], BF16, [b_hT])
            K.barrier()

            lg = wview(0, NR + 4, F32)[:, 0:NR]; b_lg = GB("lg")
            r8 = wview(64, 16, F32); b_r8 = GB("r8")
            gm = wview(96, NG, F32); b_gm = GB("gm")
            els = wview(128, EPG, F32); b_els = GB("els")
            m8 = wview(160, 8, F32); b_m8 = GB("m8")
            cws = wview(192, EPG, F32); b_cws = GB("cws")
            cws2 = wview(224, EPG, F32)
            cw = wview(256, NE, F32); b_cw = GB("cw")
            cwT = [wview(512 + i * 128, 128, F32) for i in range(2)]; b_cwT = [GB("cwT0"), GB("cwT1")]
            for tc in range(TC):
                bk = tc % 2
                for dc in range(DC):
                    mm(banks[bk][:, 0:NR], hT[:, dc, tc * 128:(tc + 1) * 128], wrt[:, dc, :], dc == 0, dc == DC - 1, [b_hT, b_wrt], [bbuf[bk]])
                TT(dve, lg, banks[bk][:, 0:NR], brt[:], ALU.add, [bbuf[bk], b_brt], [b_lg])
                dve.op(lambda e: e.tensor_reduce(out=r8[:, 0:1], in_=lg[:, 0:NG], axis=AX.X, op=ALU.max), [b_lg], [b_r8])
                TS(dve, gm, lg[:, 0:NG], r8[:, 0:1], None, ALU.is_equal, None, [b_lg, b_r8], [b_gm])
                TS(dve, r8[:, 1:2], r8[:, 0:1], -1.0, None, ALU.mult, None, [b_r8], [b_r8])
                A(cws2[:, 0:NG], lg[:, 0:NG], AF.Exp, [b_lg, b_r8], [b_cws, b_r8], bias=r8[:, 1:2], scale=1.0, accum_out=r8[:, 2:3])
                dve.op(lambda e: e.reciprocal(out=r8[:, 3:4], in_=r8[:, 2:3]), [b_r8], [b_r8])
                TS(dve, els, lg[:, NG:NG + EPG], gm[:, 0:1], None, ALU.mult, None, [b_lg, b_gm], [b_els])
                for g in range(1, NG):
                    STT(dve, els, lg[:, NG + g * EPG:NG + (g + 1) * EPG], gm[:, g:g + 1], els, ALU.mult, ALU.add, [b_lg, b_gm, b_els], [b_els])
                dve.op(lambda e: e.max(out=m8, in_=els), [b_els], [b_m8])
                TT(dve, r8[:, 4:5], m8[:, 1:2], m8[:, 0:1], ALU.subtract, [b_m8], [b_r8])
                A(r8[:, 5:6], r8[:, 4:5], AF.Exp, [b_r8], [b_r8])
                TS(dve, r8[:, 6:7], r8[:, 5:6], 1.0, None, ALU.add, None, [b_r8], [b_r8])
                dve.op(lambda e: e.reciprocal(out=r8[:, 7:8], in_=r8[:, 6:7]), [b_r8], [b_r8])
                TT(dve, r8[:, 8:9], r8[:, 7:8], r8[:, 3:4], ALU.mult, [b_r8], [b_r8])
                TT(dve, r8[:, 9:10], r8[:, 3:4], r8[:, 8:9], ALU.subtract, [b_r8], [b_r8])
                TS(dve, cws, els, m8[:, 0:1], r8[:, 8:9], ALU.is_equal, ALU.mult, [b_els, b_m8, b_r8], [b_cws])
                TS(dve, cws2, els, m8[:, 1:2], r8[:, 9:10], ALU.is_equal, ALU.mult, [b_els, b_m8, b_r8, b_cws], [b_cws])
                TT(dve, cws, cws, cws2, ALU.add, [b_cws], [b_cws])
                for g in range(NG):
                    TS(dve, cw[:, g * EPG:(g + 1) * EPG], cws, gm[:, g:g + 1], None, ALU.mult, None, [b_cws, b_gm], [b_cw])
                tr(banks[2 + bk][0:NE, 0:128], cw, ident[:], [b_cw, b_ident], [bbuf[2 + bk]])
                CP(dve, cwT[bk][0:NE, :], banks[2 + bk][0:NE, 0:128], [bbuf[2 + bk]], [b_cwT[bk]])
                sp.dma((lambda bk=bk, tc=tc: (lambda e: e.dma_start(out=cw_scr[:, tc * 128:(tc + 1) * 128], in_=cwT[bk][0:NE, :])))(),
                       b_cwT[bk], [b_cwT[bk]], [b_cwscr])
            if b == 0:
                dump("cw", cw_scr, [NE, S], F32, [b_cwscr])
            K.barrier()

            bcast_mod(b, 5, bcA, b_bcA)
            MT = min(512, S)
            NMT = S // MT
            MC = MT // 128
            GUW = DC * DE
            DNW = FC * D // 2
            EW = GUW + DNW
            wslot = []
            for si in range(4):
                base_ = (o_y + si * EW) if si < 2 else (o_w + (si - 2) * EW)
                wslot.append((view(base_, GUW, BF16, "p (c n) -> p c n", c=DC), view(base_ + GUW, DNW, BF16, "p (f d) -> p f d", f=FC)))
            assert 2 * EW <= YW
            b_wslot = [GB("wslot%d" % si) for si in range(4)]
            o9 = 2 * EW
            cwbc = [[wview(o9 + (i * 2 + k) * MT, MT, F32) for k in range(2)] for i in range(2)]
            b_cwbc = [[GB("cwbc%d%d" % (i, k)) for k in range(2)] for i in range(2)]
            o9 += 4 * MT
            sg9 = [wview(o9 + i * FC * MT, FC * MT, F32) for i in range(2)]; b_sg9 = [GB("sg9_0"), GB("sg9_1")]
            o9 += 2 * FC * MT
            actp2 = [[wview(o9 + (i * 2 + k) * (FC * MT // 2), FC * MT // 2, BF16, "p (f t) -> p f t", f=FC) for k in range(2)] for i in range(2)]
            b_actp2 = [[GB("actp%d%d" % (i, k)) for k in range(2)] for i in range(2)]
            o9 += 2 * FC * MT
            assert o9 <= WW, o9
            ycnt = [0]

            def load_pair(ep):
                for e_ in range(2):
                    e = ep * 2 + e_
                    si = e % 4
                    LD(pool, wslot[si][0], w_gu[e].rearrange("(c p) n -> p c n", p=128), b_wslot[si])
                    LD(pool, wslot[si][1], w_dn[e].rearrange("(f p) d -> p f d", p=128), b_wslot[si])
                    for f in range(FC):
                        TT(pool, wslot[si][1][:, f, :], wslot[si][1][:, f, :], bcA[:], ALU.mult, [b_wslot[si], b_bcA], [b_wslot[si]])

            def blk(n):
                return banks[(n * MT) // 512][:, (n * MT) % 512:(n * MT) % 512 + MT], bbuf[(n * MT) // 512]

            def emit_gu1(k, ep, mt, e_):
                tok = slice(mt * MT, (mt + 1) * MT)
                e = ep * 2 + e_
                si = e % 4
                wg = wslot[si][0]
                LD(sp, cwbc[e_][mt % 2], cw_scr[e:e + 1, tok].broadcast_to([128, MT]), b_cwbc[e_][mt % 2], reads=[b_cwscr])
                for n in range(2 * FC):
                    o_ap, o_b = blk(n)
                    for dc in range(DC):
                        pe.op((lambda o_ap=o_ap, wg=wg, dc=dc, n=n, tok=tok: (lambda e__: e__.matmul(
                            o_ap, lhsT=wg[:, dc, n * 128:(n + 1) * 128], rhs=hT[:, dc, tok],
                            start=(dc == 0), stop=(dc == DC - 1), skip_group_check=True)))(),
                            [b_wslot[si], b_hT], [o_b], sig=(dc == DC - 1))
                for f in range(FC):
                    g_ap, g_b = blk(f)
                    u_ap, u_b = blk(FC + f)
                    sgf = sg9[e_][:, f * MT:(f + 1) * MT]
                    A(sgf, g_ap, AF.Sigmoid, [g_b], [b_sg9[e_]])
                    TT(dve, sgf, sgf, g_ap, ALU.mult, [b_sg9[e_], g_b], [b_sg9[e_]])
                    TT(dve, sgf, sgf, u_ap, ALU.mult, [b_sg9[e_], u_b], [b_sg9[e_]])
                    TT(pool, actp2[e_][k % 2][:, f, :], sgf, cwbc[e_][mt % 2], ALU.mult,
                       [b_sg9[e_], b_cwbc[e_][mt % 2]], [b_actp2[e_][k % 2]])

            def emit_down_half(k, ep, mt, half):
                groups = [(mc, hb_) for mc in range(MC) for hb_ in range(D // DB)]
                hsz = (len(groups) + 1) // 2
                for (mc, hb_) in groups[half * hsz:(half + 1) * hsz]:
                    tc = mt * MC + mc
                    bk = 4 + ycnt[0] % 4
                    ycnt[0] += 1
                    nmm = 0
                    for e_ in range(2):
                        wd_ = wslot[(ep * 2 + e_) % 4][1]
                        for f in range(FC):
                            mm(banks[bk][:, 0:DB], actp2[e_][k % 2][:, f, mc * 128:(mc + 1) * 128], wd_[:, f, hb_ * DB:(hb_ + 1) * DB],
                               nmm == 0, nmm == 2 * FC - 1, [b_actp2[e_][k % 2], b_wslot[(ep * 2 + e_) % 4]], [bbuf[bk]])
                            nmm += 1
                    TT(dve, x1[:, tc, hb_ * DB:(hb_ + 1) * DB], banks[bk][:, 0:DB], x1[:, tc, hb_ * DB:(hb_ + 1) * DB], ALU.add,
                       [bbuf[bk], b_x1[tc]], [b_x1[tc]])

            items = [(ep, mt) for ep in range(NE // 2) for mt in range(NMT)]
            load_pair(0)
            for k, (ep, mt) in enumerate(items):
                emit_gu1(k, ep, mt, 0)
                if k >= 1:
                    emit_down_half(k - 1, *items[k - 1], 0)
                emit_gu1(k, ep, mt, 1)
                if k >= 1:
                    emit_down_half(k - 1, *items[k - 1], 1)
                if mt == 0 and ep + 1 < NE // 2:
                    load_pair(ep + 1)
            emit_down_half(len(items) - 1, *items[-1], 0)
            emit_down_half(len(items) - 1, *items[-1], 1)
            if b == 0:
                dump("x2", x1, [128, TC, D], F32, b_x1)
            K.barrier()

            ot = [wview(i * D, D, F32) for i in range(2)]; b_ot = [GB("ot0"), GB("ot1")]
            jk = wview(2 * D, D, F32); b_jk = GB("jk10")
            st10 = wview(3 * D, 8, F32); b_st10 = GB("st10")
            for tc in range(TC):
                i = tc % 2
                A(jk, x1[:, tc, :], AF.Square, [b_x1[tc]], [b_jk, b_st10], accum_out=st10[:, 0:1])
                rsqrt_col(st10[:, 2:3], st10[:, 0:1], 1.0 / D, st10[:, 1:2], [b_st10, b_eps], [b_st10])
                STT(dve, ot[i], x1[:, tc, :], st10[:, 2:3], fgbc[:], ALU.mult, ALU.mult, [b_x1[tc], b_st10, b_fgbc], [b_ot[i]])
                sp.dma((lambda i=i, tc=tc, b=b: (lambda e: e.dma_start(out=y_d[b, tc * 128:(tc + 1) * 128, :], in_=ot[i])))(), b_ot[i], [b_ot[i]], [])
            K.barrier()
        K.barrier()
        K.emit()
    return nc


_NC_CACHE = {}


def _prep_shared(inp, cfg):
    f = lambda a: np.ascontiguousarray(np.asarray(a, dtype=np.float32))
    L = 0
    sh = {
        "w_ada": f(inp["w_ada"][L]), "b_ada": f(inp["b_ada"][L]).reshape(1, -1), "norm1_g": f(inp["norm1_g"][L]).reshape(1, -1),
        "w_in": f(inp["w_in"][L]),
        "lam4": f(np.stack([np.asarray(inp["lambda_q1"][L]), np.asarray(inp["lambda_k1"][L]),
                            np.asarray(inp["lambda_q2"][L]), np.asarray(inp["lambda_k2"][L])], axis=0)),
        "subln_g": f(inp["subln_g"][L]).reshape(1, -1), "w_attn_proj": f(inp["w_attn_proj"][L]),
        "sgu_ln_g": f(inp["sgu_ln_g"][L]).reshape(1, -1), "sgu_ln_b": f(inp["sgu_ln_b"][L]).reshape(1, -1),
        "sgu_w_s": f(inp["sgu_w_s"][L]), "sgu_b_s": f(inp["sgu_b_s"][L]).reshape(1, -1),
        "w_sgu_proj": f(inp["w_sgu_proj"][L]), "w_out": f(inp["w_out"][L]), "norm2_g": f(inp["norm2_g"][L]).reshape(1, -1),
        "w_router": f(np.concatenate([np.asarray(inp["w_router_group"][L]), np.asarray(inp["w_router_expert"][L])], axis=1)),
        "b_router": f(np.concatenate([np.asarray(inp["b_router_group"][L]), np.asarray(inp["b_router_expert"][L])], axis=0)).reshape(1, -1),
        "w_expert_gate_up": f(inp["w_expert_gate_up"][L]), "w_expert_down": f(inp["w_expert_down"][L]),
        "final_g": f(inp["final_g"]).reshape(1, -1),
    }
    return sh


def kernel(**inputs):
    cfg = Cfg()
    n_cores = 8
    x = np.asarray(inputs["x"], dtype=np.float32)
    c = np.asarray(inputs["c"], dtype=np.float32)
    sh = _prep_shared(inputs, cfg)
    if "nc" not in _NC_CACHE:
        _NC_CACHE["nc"] = build(cfg)
    nc = _NC_CACHE["nc"]
    in_maps = []
    for i in range(n_cores):
        m = dict(sh)
        m["x"] = np.ascontiguousarray(x[i * cfg.NB:(i + 1) * cfg.NB])
        m["c"] = np.ascontiguousarray(c[i * cfg.NB:(i + 1) * cfg.NB])
        in_maps.append(m)
    res = run_bass_kernel_spmd(nc, in_maps, core_ids=list(range(n_cores)))
    return np.concatenate([r["y"] for r in res.results], axis=0).astype(np.float32)
```

```python
import math
from contextlib import ExitStack
import numpy as np
import concourse.bass as bass
import concourse.mybir as mybir
from concourse.bass_utils import run_bass_kernel_spmd

F32 = mybir.dt.float32
BF16 = mybir.dt.bfloat16
AF = mybir.ActivationFunctionType
ALU = mybir.AluOpType
AX = mybir.AxisListType
EPS = 1e-6


class Buf:
    __slots__ = ("name", "w", "r", "dsem", "dcnt")

    def __init__(self, name):
        self.name = name
        self.w = None
        self.r = {}
        self.dsem = None
        self.dcnt = 0


class Eng:
    def __init__(self, K, name, sem, self_raw=True):
        self.K = K
        self.name = name
        self.sem = sem
        self.cnt = 0
        self.known = {}
        self.self_raw = self_raw
        self.prog = []

    def wait(self, ev):
        if ev is None:
            return
        sem, val = ev
        if self.known.get(id(sem), 0) >= val:
            return
        self.prog.append(("w", sem, val))
        self.known[id(sem)] = val

    def _deps(self, reads, writes):
        for b in reads:
            if b.w is not None:
                if b.w[0] is self.sem and not self.self_raw:
                    continue
                self.wait(b.w)
        for b in writes:
            if b.w is not None and (b.w[0] is not self.sem or self.self_raw):
                self.wait(b.w)
            for sem, v in b.r.values():
                if sem is not self.sem or self.self_raw:
                    self.wait((sem, v))

    def op(self, fn, reads=(), writes=(), sig=True):
        self._deps(reads, writes)
        if sig:
            self.cnt += 1
            self.prog.append(("o", fn, self.sem, 1))
            ev = (self.sem, self.cnt)
        else:
            self.prog.append(("o", fn, None, 0))
            ev = (self.sem, self.cnt + 1)
        for b in reads:
            b.r[id(self.sem)] = ev
        for b in writes:
            b.w = ev
            b.r = {}
        return ev

    def dma(self, fn, anchor, reads=(), writes=()):
        self._deps(reads, writes)
        if anchor.dsem is None:
            anchor.dsem = self.K.new_sem("d_" + anchor.name)
            self.K.dma_bufs.append(anchor)
        anchor.dcnt += 16
        self.prog.append(("o", fn, anchor.dsem, 16))
        ev = (anchor.dsem, anchor.dcnt)
        for b in reads:
            b.r[id(anchor.dsem)] = ev
        for b in writes:
            b.w = ev
            b.r = {}
        return ev

    def replay(self, eng):
        for it in self.prog:
            if it[0] == "w":
                eng.wait_ge(it[1], it[2])
            else:
                ins = it[1](eng)
                if it[2] is not None:
                    ins.then_inc(it[2], it[3])


class Kern:
    def __init__(self, nc, stack):
        self.nc = nc
        self.stack = stack
        self.dma_bufs = []
        self.pe = Eng(self, "pe", self.new_sem("s_pe"), self_raw=False)
        self.act = Eng(self, "act", self.new_sem("s_act"))
        self.dve = Eng(self, "dve", self.new_sem("s_dve"))
        self.pool = Eng(self, "pool", self.new_sem("s_pool"))
        self.sp = Eng(self, "sp", self.new_sem("s_sp"))
        self.engs = [self.pe, self.act, self.dve, self.pool, self.sp]

    def new_sem(self, name):
        return self.stack.enter_context(self.nc.semaphore(name))

    def sb(self, name, shape, dt):
        return self.stack.enter_context(self.nc.sbuf_tensor(name, shape, dt))

    def ps(self, name, shape, dt):
        return self.stack.enter_context(self.nc.psum_tensor(name, shape, dt))

    def barrier(self, engs=None):
        evs = [(e.sem, e.cnt) for e in self.engs if e.cnt > 0]
        evs += [(b.dsem, b.dcnt) for b in self.dma_bufs]
        for e in (engs or self.engs):
            for ev in evs:
                if ev[0] is not e.sem:
                    e.wait(ev)

    def emit(self):
        with self.nc.Block() as block:
            block.tensor(lambda e: self.pe.replay(e))
            block.scalar(lambda e: self.act.replay(e))
            block.vector(lambda e: self.dve.replay(e))
            block.gpsimd(lambda e: self.pool.replay(e))
            block.sync(lambda e: self.sp.replay(e))


class Cfg:
    def __init__(self, D=1024, S=2048, NB=2, NH=8, NG=4, EPG=8, DE=256, depth_l=0):
        self.D, self.S, self.NB, self.NH, self.NG, self.EPG, self.DE = D, S, NB, NH, NG, EPG, DE
        self.DC = D // 128
        self.TC = S // 128
        self.AW = NH * 128
        self.G = D // 128
        self.NE = NG * EPG
        self.FC = DE // 128
        self.INW = 3 * self.AW + 4 * D
        self.QT = min(512, S)
        self.lam_init = 0.8 - 0.6 * math.exp(-0.3 * depth_l)
        self.slopes = [2.0 ** (-8.0 * (h + 1) / NH) for h in range(NH)]


def build(cfg, stop_after=None):
    DB = min(512, cfg.D)
    D, S, NB, NH, NG, EPG, DE = cfg.D, cfg.S, cfg.NB, cfg.NH, cfg.NG, cfg.EPG, cfg.DE
    DC, TC, AW, G, NE, FC, INW, QT = cfg.DC, cfg.TC, cfg.AW, cfg.G, cfg.NE, cfg.FC, cfg.INW, cfg.QT
    NR = NG + NE
    nc = bass.Bass("TRN2", target_bir_lowering=False)
    _bufs = {}

    def GB(name):
        if name not in _bufs:
            _bufs[name] = Buf(name)
        return _bufs[name]

    def din(name, shape):
        return nc.dram_tensor(name, list(shape), F32, kind="ExternalInput").ap()

    x_d = din("x", [NB, S, D])
    c_d = din("c", [NB, D])
    w_ada = din("w_ada", [D, 6 * D])
    b_ada = din("b_ada", [1, 6 * D])
    norm1_g = din("norm1_g", [1, D])
    w_in = din("w_in", [D, INW])
    lam_d = din("lam4", [4, 64])
    subln_g = din("subln_g", [1, 128])
    w_ap = din("w_attn_proj", [AW, D])
    ln_g = din("sgu_ln_g", [1, D])
    ln_b = din("sgu_ln_b", [1, D])
    w_s_d = din("sgu_w_s", [G, 128, 128])
    b_s_d = din("sgu_b_s", [1, G * 128])
    w_sp = din("w_sgu_proj", [D, D])
    w_out = din("w_out", [D, D])
    norm2_g = din("norm2_g", [1, D])
    w_rt = din("w_router", [D, NR])
    b_rt = din("b_router", [1, NR])
    w_gu = din("w_expert_gate_up", [NE, D, 2 * DE])
    w_dn = din("w_expert_down", [NE, DE, D])
    final_g = din("final_g", [1, D])
    y_d = nc.dram_tensor("y", [NB, S, D], F32, kind="ExternalOutput").ap()

    with ExitStack() as st:
        K = Kern(nc, st)
        pe, act, dve, pool, sp = K.pe, K.act, K.dve, K.pool, K.sp

        banks = [K.ps("bank%d" % i, [128, 512], F32) for i in range(8)]
        bbuf = [GB("bank%d" % i) for i in range(8)]

        ident = K.sb("ident", [128, 128], F32); b_ident = GB("ident")
        identb = K.sb("identb", [128, 128], BF16); b_identb = GB("identb")
        bcA = K.sb("bcA", [128, D], F32); b_bcA = GB("bcA")
        bcB = K.sb("bcB", [128, D], F32); b_bcB = GB("bcB")
        fgbc = K.sb("fgbc", [128, D], F32); b_fgbc = GB("fgbc")
        subg = K.sb("subg", [128, 128], F32); b_subg = GB("subg")
        lam_t = K.sb("lam_t", [128, 8], F32); b_lam = GB("lam")
        brt = K.sb("brt", [128, NR], F32); b_brt = GB("brt")
        wrt = K.sb("wrt", [128, DC, NR], BF16); b_wrt = GB("wrt")
        mod_scr = nc.dram_tensor("mod_scr", [NB, 6 * D], F32, kind="Internal").ap(); b_modscr = GB("mod_scr")
        cw_scr = nc.dram_tensor("cw_scr", [NE, S], F32, kind="Internal").ap(); b_cwscr = GB("cw_scr")

        HW_ = DC * S // 2
        VW = TC * NH * 130 // 2 + 2
        o_hT = 0
        o_ya = o_hT + HW_
        o_z = o_ya + HW_
        ZW = max(HW_, VW)
        o_y = o_z + ZW
        YW = max(HW_, 2 * S + 4096)
        o_w = o_y + YW
        WW = 12 * 1024
        ARENA = o_w + WW
        arena = K.sb("arena", [128, ARENA], F32)

        def view(off, words, dt, pattern=None, **kw):
            a = arena[:, off:off + words]
            if dt is BF16:
                a = a.bitcast(BF16)
            if pattern:
                a = a.rearrange(pattern, **kw)
            return a

        hT = view(o_hT, HW_, BF16, "p (c s) -> p c s", c=DC); b_hT = GB("hT")
        yaT = view(o_ya, HW_, BF16, "p (c s) -> p c s", c=NH) if NH == DC else None
        assert NH == DC
        b_yaT = GB("yaT")
        zT = view(o_z, HW_, BF16, "p (c s) -> p c s", c=G); b_zT = GB("zT")
        vaug = view(o_z, (TC * NH * 130) // 2, BF16, "p (t h e) -> p t h e", t=TC, h=NH); b_vaug = GB("vaug")
        yT = view(o_y, HW_, BF16, "p (c s) -> p c s", c=DC); b_yT = GB("yT")
        x1 = view(o_ya, TC * D, F32, "p (t d) -> p t d", t=TC); b_x1 = [GB("x1_%d" % i) for i in range(TC)]
        assert TC * D <= HW_ + ZW

        def mm(out, lhsT, rhs, start, stop, reads, writes, sig=None):
            if sig is None:
                sig = stop
            pe.op(lambda e: e.matmul(out, lhsT=lhsT, rhs=rhs, start=start, stop=stop), reads, writes, sig=sig)

        def tr(out, in_, idt, reads, writes, sig=True):
            pe.op(lambda e: e.transpose(out, in_, idt), reads, writes, sig=sig)

        def A(out, in_, func, reads, writes, **kw):
            act.op(lambda e: e.activation(out=out, in_=in_, func=func, **kw), reads, writes)

        def TT(eng, out, in0, in1, op, reads, writes):
            eng.op(lambda e: e.tensor_tensor(out=out, in0=in0, in1=in1, op=op), reads, writes)

        def TS(eng, out, in0, s1, s2, op0, op1, reads, writes, **kw):
            if op1 is None:
                eng.op(lambda e: e.tensor_scalar(out=out, in0=in0, scalar1=s1, scalar2=None, op0=op0, **kw), reads, writes)
            else:
                eng.op(lambda e: e.tensor_scalar(out=out, in0=in0, scalar1=s1, scalar2=s2, op0=op0, op1=op1, **kw), reads, writes)

        def STT(eng, out, in0, scalar, in1, op0, op1, reads, writes):
            eng.op(lambda e: e.scalar_tensor_tensor(out=out, in0=in0, scalar=scalar, in1=in1, op0=op0, op1=op1), reads, writes)

        def CP(eng, out, in_, reads, writes):
            if eng is act:
                act.op(lambda e: e.copy(out=out, in_=in_), reads, writes)
            else:
                eng.op(lambda e: e.tensor_copy(out=out, in_=in_), reads, writes)

        def LD(q, out, in_, anchor, reads=(), writes=None):
            q.dma(lambda e: e.dma_start(out=out, in_=in_), anchor, reads, [anchor] if writes is None else writes)

        dbg_outs = {}

        def dump(name, ap_, shape, dt, rb):
            if not getattr(cfg, "debug", False):
                return
            d = nc.dram_tensor("dbg_" + name, list(shape), dt, kind="ExternalOutput").ap()
            bb = GB("dbg_" + name)
            sp.dma(lambda e: e.dma_start(out=d, in_=ap_), bb, rb, [bb])
            dbg_outs[name] = d

        def rsqrt_col(dst, src, scale, tmp, rb, wb):
            A(tmp, src, AF.Sqrt, rb, wb, bias=epsc[:src.shape[0], 0:1], scale=scale)
            dve.op(lambda e: e.reciprocal(out=dst, in_=tmp), wb, wb)

        epsc = K.sb("epsc", [128, 1], F32); b_eps = GB("epsc")
        pool.op(lambda e: e.memset(epsc[:], EPS), (), [b_eps])
        pool.op(lambda e: e.memset(ident[:], 1.0), (), [b_ident])
        pool.op(lambda e: e.affine_select(out=ident[:], in_=ident[:], pattern=[[-1, 128]], compare_op=ALU.is_equal,
                                          fill=0.0, base=0, channel_multiplier=1), [b_ident], [b_ident])
        CP(dve, identb[:], ident[:], [b_ident], [b_identb])
        LD(sp, fgbc[:], final_g.broadcast_to([128, D]), b_fgbc)
        LD(sp, subg[:], subln_g.broadcast_to([128, 128]), b_subg)
        LD(sp, brt[:], b_rt.broadcast_to([128, NR]), b_brt)
        TS(dve, subg[:], subg[:], 1.0 - cfg.lam_init, None, ALU.mult, None, [b_subg], [b_subg])
        LD(pool, wrt[:], w_rt.rearrange("(c p) n -> p c n", p=128), b_wrt)
        lamw = K.sb("lamw", [128, 4, 64], F32); b_lamw = GB("lamw")
        LD(sp, lamw[:].rearrange("p a b -> p (a b)"), lam_d.rearrange("a b -> (a b)").rearrange("(o n) -> o n", o=1).broadcast_to([128, 256]), b_lamw)
        TT(dve, lamw[:, 0, :], lamw[:, 0, :], lamw[:, 1, :], ALU.mult, [b_lamw], [b_lamw])
        TT(dve, lamw[:, 2, :], lamw[:, 2, :], lamw[:, 3, :], ALU.mult, [b_lamw], [b_lamw])
        dve.op(lambda e: e.tensor_reduce(out=lam_t[:, 0:1], in_=lamw[:, 0, :], axis=AX.X, op=ALU.add), [b_lamw], [b_lam])
        dve.op(lambda e: e.tensor_reduce(out=lam_t[:, 1:2], in_=lamw[:, 2, :], axis=AX.X, op=ALU.add), [b_lamw], [b_lam])
        A(lam_t[:, 2:4], lam_t[:, 0:2], AF.Exp, [b_lam], [b_lam])
        TT(dve, lam_t[:, 4:5], lam_t[:, 2:3], lam_t[:, 3:4], ALU.subtract, [b_lam], [b_lam])
        TS(dve, lam_t[:, 5:6], lam_t[:, 4:5], cfg.lam_init, -1.0, ALU.add, ALU.mult, [b_lam], [b_lam])

        def wview(off_words, words, dt, pattern=None, rows=None, **kw):
            assert off_words + words <= WW, (off_words, words, WW)
            a_ = arena[:, o_w + off_words:o_w + off_words + words] if rows is None else arena[0:rows, o_w + off_words:o_w + off_words + words]
            if dt is BF16:
                a_ = a_.bitcast(BF16)
            if pattern:
                a_ = a_.rearrange(pattern, **kw)
            return a_

        def yview(off_words, words, dt, pattern=None, **kw):
            assert off_words + words <= YW, (off_words, words, YW)
            return view(o_y + off_words, words, dt, pattern, **kw)

        modv = view(o_y, 6 * D, F32)[0:NB, :]; b_modv = GB("modv")
        c_sb = view(o_y + 6 * D, D, F32)[0:NB, :]; b_c = GB("c_sb")
        sgc = view(o_y + 7 * D, D, F32)[0:NB, :]
        g2row = view(o_hT, 2 * D, F32, "p (a d) -> p a d", a=2)[0:NB]; b_g2row = GB("g2row")
        siluT = wview(0, DC * NB // 2 + 1, BF16)[:, 0:DC * NB].rearrange("p (c b) -> p c b", c=DC); b_siluT = GB("siluT")
        LD(sp, c_sb, c_d, b_c)
        A(sgc, c_sb, AF.Sigmoid, [b_c], [b_c])
        TT(dve, c_sb, c_sb, sgc, ALU.mult, [b_c], [b_c])
        pt = banks[0]
        for dc in range(DC):
            tr(pt[:, dc * NB:(dc + 1) * NB], c_sb[:, dc * 128:(dc + 1) * 128], ident[0:NB, 0:NB], [b_c, b_ident], [bbuf[0]])
        CP(dve, siluT.rearrange("p c b -> p (c b)"), pt[:, 0:DC * NB], [bbuf[0]], [b_siluT])
        LD(sp, modv, b_ada.broadcast_to([NB, 6 * D]), b_modv)
        NBLK = 6 * D // 512
        wa = [wview(64 + i * (DC * 256), DC * 256, BF16, "p (c n) -> p c n", c=DC) for i in range(2)]
        b_wa = [GB("wa0"), GB("wa1")]
        for blk in range(NBLK):
            i = blk % 2
            LD(pool, wa[i], w_ada[:, blk * 512:(blk + 1) * 512].rearrange("(c p) n -> p c n", p=128), b_wa[i])
            bk = 1 + (blk % 2)
            for dc in range(DC):
                mm(banks[bk][0:NB, :], siluT[:, dc, :], wa[i][:, dc, :], dc == 0, dc == DC - 1, [b_siluT, b_wa[i]], [bbuf[bk]])
            TT(dve, modv[:, blk * 512:(blk + 1) * 512], modv[:, blk * 512:(blk + 1) * 512], banks[bk][0:NB, :], ALU.add,
               [b_modv, bbuf[bk]], [b_modv])
        LD(sp, g2row[:, 0, :], norm1_g.broadcast_to([NB, D]), b_g2row)
        LD(sp, g2row[:, 1, :], norm2_g.broadcast_to([NB, D]), b_g2row)
        for (gi, off) in ((0, 1), (1, 4)):
            STT(dve, modv[:, off * D:(off + 1) * D], modv[:, off * D:(off + 1) * D], 1.0, g2row[:, gi, :], ALU.add, ALU.mult,
                [b_modv, b_g2row], [b_modv])
        sp.dma(lambda e: e.dma_start(out=mod_scr, in_=modv), b_modv, [b_modv], [b_modscr])
        dump("modv", modv, [NB, 6 * D], F32, [b_modv])
        K.barrier()

        def bcast_mod(b, idx, dst, b_dst):
            sp.dma(lambda e: e.dma_start(out=dst[:], in_=mod_scr[b:b + 1, idx * D:(idx + 1) * D].broadcast_to([128, D])),
                   b_dst, [b_modscr], [b_dst])

        def norm_to_hT(b, src_fn, b_src_fn, tag):
            xt = [wview(i * D, D, F32) for i in range(2)]; b_xt = [GB(tag + "xt0"), GB(tag + "xt1")]
            junk = wview(2 * D, D, F32); b_junk = GB(tag + "junk")
            hb = [wview(3 * D + i * (D // 2), D // 2, BF16) for i in range(2)]; b_hb = [GB(tag + "hb0"), GB(tag + "hb1")]
            st_ = wview(4 * D, 3 * TC, F32); b_st = GB(tag + "st")
            nj = [wview(4 * D + 3 * TC + i * D, D, F32) for i in range(2)]; b_nj = [GB(tag + "nj0"), GB(tag + "nj1")]
            for tc in range(TC):
                src, b_src = src_fn(tc, xt[tc % 2], b_xt[tc % 2])
                act.op((lambda src=src, tc=tc: (lambda e: e.activation(out=junk, in_=src, func=AF.Square, accum_out=st_[:, tc:tc + 1])))(),
                       [b_src], [b_junk, b_st])
            rsqrt_col(st_[:, 2 * TC:3 * TC], st_[:, 0:TC], 1.0 / D, st_[:, TC:2 * TC], [b_st, b_eps], [b_st])
            for tc in range(TC):
                i = tc % 2
                src, b_src = src_fn(tc, xt[i], b_xt[i])
                STT(dve, nj[i], src, st_[:, 2 * TC + tc:2 * TC + tc + 1], bcA[:], ALU.mult, ALU.mult, [b_src, b_st, b_bcA], [b_nj[i]])
                TT(pool, hb[i], nj[i], bcB[:], ALU.add, [b_nj[i], b_bcB], [b_hb[i]])
                bk = tc % 2
                ptb = banks[bk][:].bitcast(BF16)
                for dc in range(DC):
                    tr(ptb[:, dc * 128:(dc + 1) * 128], hb[i][:, dc * 128:(dc + 1) * 128], identb[:], [b_hb[i], b_identb], [bbuf[bk]],
                       sig=(dc == DC - 1))
                CP(act, hT[:, :, tc * 128:(tc + 1) * 128], ptb[:, 0:DC * 128].rearrange("p (c t) -> p c t", c=DC), [bbuf[bk]], [b_hT])

        for b in range(NB):
            bcast_mod(b, 1, bcA, b_bcA)
            bcast_mod(b, 0, bcB, b_bcB)

            def src_x(tc, xt_i, b_xt_i):
                LD(sp, xt_i, x_d[b, tc * 128:(tc + 1) * 128, :], b_xt_i)
                return xt_i, b_xt_i
            norm_to_hT(b, src_x, None, "n1")
            if b == 0:
                dump("hT", hT, [128, DC, S], BF16, [b_hT])
            K.barrier()

            wv = wview(0, DC * AW // 2, BF16, "p (c n) -> p c n", c=DC); b_wv = GB("wv")
            LD(pool, wv, w_in[:, 2 * AW:3 * AW].rearrange("(c p) n -> p c n", p=128), b_wv)
            pool.op(lambda e: e.memset(vaug[:, :, :, 128:130], 1.0), (), [b_vaug])
            VB = min(512, AW)
            HPB = VB // 128
            for tc in range(TC):
                for hb_ in range(AW // VB):
                    bk = (tc * (AW // VB) + hb_) % 4
                    for dc in range(DC):
                        mm(banks[bk][:, 0:VB], hT[:, dc, tc * 128:(tc + 1) * 128], wv[:, dc, hb_ * VB:(hb_ + 1) * VB], dc == 0, dc == DC - 1,
                           [b_hT, b_wv], [bbuf[bk]])
                    eng = act if (hb_ % 2 == 0) else dve
                    CP(eng, vaug[:, tc, hb_ * HPB:(hb_ + 1) * HPB, 0:128], banks[bk][:, 0:VB].rearrange("p (h e) -> p h e", h=HPB), [bbuf[bk]], [b_vaug])
            if b == 0:
                dump("vaug", vaug, [128, TC, NH, 130], BF16, [b_vaug])
            K.barrier()

            Ttab = yview(0, 2 * S, F32); b_T = GB("Ttab")
            pool.op(lambda e: e.iota(Ttab, pattern=[[1, 2 * S]], base=-S, channel_multiplier=-1,
                                     allow_small_or_imprecise_dtypes=True), (), [b_T])
            Ttab2 = yview(2 * S, 2 * S, F32)
            pool.op(lambda e: e.iota(Ttab2, pattern=[[-1, 2 * S]], base=S, channel_multiplier=1,
                                     allow_small_or_imprecise_dtypes=True), (), [b_T])
            TT(dve, Ttab, Ttab, Ttab2, ALU.max, [b_T], [b_T])
            wqk = [wview(i * (DC * 128), DC * 128, BF16, "p (c n) -> p c n", c=DC) for i in range(2)]
            b_wqk = [GB("wqk0"), GB("wqk1")]
            qo = 2 * DC * 128
            qT = [wview(qo + i * (S // 2), S // 2, BF16) for i in range(2)]; b_qT = [GB("qT0"), GB("qT1")]
            ko = qo + S
            kT = [wview(ko + i * (S // 2), S // 2, BF16) for i in range(2)]; b_kT = [GB("kT0"), GB("kT1")]
            to = ko + S
            NTB = 4
            tmpb = [wview(to + i * QT, QT, F32) for i in range(NTB)]; b_tmp = [GB("tmp%d" % i) for i in range(NTB)]
            po = to + NTB * QT
            pTb = [wview(po + i * (QT // 2), QT // 2, BF16) for i in range(NTB)]; b_pT = [GB("pT%d" % i) for i in range(NTB)]
            so = po + NTB * (QT // 2)
            NQC = QT // 128
            nab = (2 * NQC + 2) // 3
            accs = [wview(so, nab * 387, F32, "p (k c) -> p k c", k=nab)] * 2
            b_accs = [GB("accs0")] * 2
            so += nab * 387
            a_h = [yview(2 * S + i * (TC * 128), TC * 128, F32, "p (t e) -> p t e", t=TC) for i in range(2)]
            b_ah = [GB("a_h0"), GB("a_h1")]
            ssq = [wview(so + i * 3 * TC, TC, F32) for i in range(2)]
            c2e = [wview(so + i * 3 * TC + TC, TC, F32) for i in range(2)]
            rstd = [wview(so + i * 3 * TC + 2 * TC, TC, F32) for i in range(2)]
            b_st3 = [GB("st3_0"), GB("st3_1")]
            so += 6 * TC
            sm = wview(so, 8, F32); b_sm = GB("sm")
            a_j = [wview(so + 8 + 256 + i * 128, 128, F32) for i in range(2)]; b_aj = [GB("a_j0"), GB("a_j1")]
            a_k = wview(so + 8 + 512, 128, F32); b_ak = GB("a_k")
            yh2 = [wview(so + 8 + 128 + i * 64, 64, BF16) for i in range(2)]; b_yh2 = [GB("yh0"), GB("yh1")]
            assert so + 8 + 640 <= WW, so
            SB_ = [3, 4, 5, 6]

            def emit_proj(h):
                i = h % 2
                LD(pool, wqk[i][:, :, 0:128], w_in[:, h * 128:(h + 1) * 128].rearrange("(c p) n -> p c n", p=128), b_wqk[i])
                LD(pool, wqk[i][:, :, 128:256], w_in[:, AW + h * 128:AW + (h + 1) * 128].rearrange("(c p) n -> p c n", p=128), b_wqk[i])
                for t5 in range(S // QT):
                    for (which, dstT, b_dst) in ((0, qT[i], b_qT[i]), (1, kT[i], b_kT[i])):
                        for dc in range(DC):
                            mm(banks[7][:, 0:QT], wqk[i][:, dc, which * 128:(which + 1) * 128], hT[:, dc, t5 * QT:(t5 + 1) * QT],
                               dc == 0, dc == DC - 1, [b_wqk[i], b_hT], [bbuf[7]])
                        CP(act if which == 0 else dve, dstT[:, t5 * QT:(t5 + 1) * QT], banks[7][:, 0:QT], [bbuf[7]], [b_dst])

            def acc(m, qc):
                a_ = m * NQC + qc
                bk = a_ // 3
                return banks[bk][:, (a_ % 3) * 129:(a_ % 3) * 129 + 129], bbuf[bk], (a_ % 3 == 0)

            def emit_score_pair(h, p, qt, kc):
                i = h % 2
                for m in range(2):
                    sbk = SB_[(2 * p + m) % 4]
                    mm(banks[sbk][:, 0:QT], kT[i][m * 64:(m + 1) * 64, kc * 128:(kc + 1) * 128],
                       qT[i][m * 64:(m + 1) * 64, qt * QT:(qt + 1) * QT], True, True, [b_kT[i], b_qT[i]], [bbuf[sbk]])
                off = qt * QT - kc * 128 + S
                for m in range(2):
                    sbk = SB_[(2 * p + m) % 4]
                    ti = (2 * p + m) % NTB
                    STT(dve, tmpb[ti], Ttab[:, off:off + QT], -8.0 * cfg.slopes[h], banks[sbk][:, 0:QT], ALU.mult, ALU.add,
                        [b_T, bbuf[sbk]], [b_tmp[ti]])
                    A(pTb[ti], tmpb[ti], AF.Exp, [b_tmp[ti]], [b_pT[ti]], scale=0.125)

            def emit_av_pair(h, p, qt, kc):
                for m in range(2):
                    ti = (2 * p + m) % NTB
                    for qc in range(NQC):
                        a_ap, a_b, first = acc(m, qc)
                        pe.op((lambda a_ap=a_ap, ti=ti, qc=qc, kc=kc, h=h, first=first: (lambda e: e.matmul(
                            a_ap, lhsT=pTb[ti][:, qc * 128:(qc + 1) * 128], rhs=vaug[:, kc, h, 0:129],
                            start=(kc == 0 and first), stop=(kc == TC - 1), skip_group_check=True)))(),
                            [b_pT[ti], b_vaug], [a_b], sig=(qc == NQC - 1))
                if kc == TC - 1:
                    emit_post(h, qt)

            def emit_post(h, qt):
                i = h % 2
                par = qt % 2
                for bk in range(nab):
                    ncol = min(3, 2 * NQC - 3 * bk) * 129
                    CP(dve, accs[par][:, bk, 0:ncol], banks[bk][:, 0:ncol], [bbuf[bk]], [b_accs[par]])
                for qc in range(NQC):
                    slot = qt * NQC + qc
                    a0_, a1_ = qc, NQC + qc
                    c0 = (a0_ % 3) * 129; c1 = (a1_ % 3) * 129
                    o0 = accs[par][:, a0_ // 3, c0:c0 + 128]; l0 = accs[par][:, a0_ // 3, c0 + 128:c0 + 129]
                    o1 = accs[par][:, a1_ // 3, c1:c1 + 128]; l1 = accs[par][:, a1_ // 3, c1 + 128:c1 + 129]
                    at = a_h[i][:, slot, :]
                    TS(pool, at, o0, l1, 0.0, ALU.mult, ALU.add, [b_accs[par]], [b_ah[i]])
                    TT(pool, sm[:, 0:1], l0, lam_t[:, 5:6], ALU.mult, [b_accs[par], b_lam], [b_sm])
                    TS(pool, a_k, o1, sm[:, 0:1], 0.0, ALU.mult, ALU.add, [b_accs[par], b_sm], [b_ak])
                    TT(pool, at, at, a_k, ALU.add, [b_ak, b_ah[i]], [b_ah[i]])
                    TS(pool, sm[:, 1:2], l0, l1, 0.0, ALU.mult, ALU.add, [b_accs[par]], [b_sm])
                    TS(pool, c2e[i][:, slot:slot + 1], sm[:, 1:2], sm[:, 1:2], EPS, ALU.mult, ALU.mult, [b_sm], [b_st3[i]])

            def emit_norm(h):
                i = h % 2
                for slot in range(TC):
                    j = slot % 2
                    TT(pool, a_j[j], a_h[i][:, slot, :], a_h[i][:, slot, :], ALU.mult, [b_ah[i]], [b_aj[j]])
                    dve.op((lambda i=i, slot=slot, j=j: (lambda e: e.tensor_reduce(out=ssq[i][:, slot:slot + 1], in_=a_j[j], axis=AX.X, op=ALU.add)))(),
                           [b_aj[j]], [b_st3[i]])
                TS(pool, ssq[i], ssq[i], 1.0 / 128, 0.0, ALU.mult, ALU.add, [b_st3[i]], [b_st3[i]])
                TT(pool, ssq[i], ssq[i], c2e[i], ALU.add, [b_st3[i]], [b_st3[i]])
                A(c2e[i], ssq[i], AF.Sqrt, [b_st3[i]], [b_st3[i]])
                dve.op((lambda i=i: (lambda e: e.reciprocal(out=rstd[i], in_=c2e[i])))(), [b_st3[i]], [b_st3[i]])
                ptb = banks[7][:].bitcast(BF16)
                for slot in range(TC):
                    j = slot % 2
                    TS(pool, a_k, a_h[i][:, slot, :], rstd[i][:, slot:slot + 1], 0.0, ALU.mult, ALU.add, [b_ah[i], b_st3[i]], [b_ak])
                    TT(pool, yh2[j], a_k, subg[:], ALU.mult, [b_ak, b_subg], [b_yh2[j]])
                    tr(ptb[:, 0:128], yh2[j], identb[:], [b_yh2[j], b_identb], [bbuf[7]])
                    CP(dve, yaT[:, h, slot * 128:(slot + 1) * 128], ptb[:, 0:128], [bbuf[7]], [b_yaT])

            pairs = [(qt, kc) for qt in range(S // QT) for kc in range(TC)]
            npair = len(pairs)
            emit_proj(0)
            for h in range(NH):
                for p in range(npair + 1):
                    if p < npair:
                        emit_score_pair(h, p, *pairs[p])
                    if p == min(12, npair - 1) and h >= 1:
                        emit_norm(h - 1)
                    if p == npair // 2 and h + 1 < NH:
                        emit_proj(h + 1)
                    if p >= 1:
                        emit_av_pair(h, p - 1, *pairs[p - 1])
            emit_norm(NH - 1)
            if b == 0:
                dump("yaT", yaT, [128, NH, S], BF16, [b_yaT])
            K.barrier()

            NQC_ = QT // 128
            wu = wview(0, DC * D // 2, BF16, "p (c n) -> p c n", c=DC); b_wu = GB("wu")
            wsw = wview(DC * D // 2, DC * D // 2, BF16, "p (c n) -> p c n", c=DC); b_wsw = GB("wsw")
            LD(pool, wu, w_in[:, 3 * AW:3 * AW + D].rearrange("(c p) n -> p c n", p=128), b_wu)
            LD(pool, wsw, w_in[:, 3 * AW + D:3 * AW + 2 * D].rearrange("(c p) n -> p c n", p=128), b_wsw)
            o2 = DC * D
            uTt = wview(o2, G * QT // 2, BF16, "p (g t) -> p g t", g=G); b_uTt = GB("uTt")
            sfulls = [wview(o2 + G * QT // 2 + i * D, D, F32) for i in range(2)]; b_sfulls = [GB("sfull0"), GB("sfull1")]
            lnG = yview(0, D, F32); b_lnG = GB("lnG")
            lnB = yview(D, D, F32); b_lnB = GB("lnB")
            bsbc = yview(2 * D, G * 128, F32); b_bsbc = GB("bsbc")
            tmp2 = yview(2 * D + G * 128, G * 128, F32); b_tmp2 = GB("tmp2")
            g1s = [yview(2 * D + 2 * G * 128 + i * 512, 512, F32) for i in range(2)]; b_g1s = [GB("g1_0"), GB("g1_1")]
            g2s = [yview(YW - 1024 + i * 512, 512, F32) for i in range(2)]; b_g2s = [GB("g2_0"), GB("g2_1")]
            gcnt = [0]
            wsT = yview(2 * D + 2 * G * 128 + 1024, G * 64, BF16, "p (g t) -> p g t", g=G); b_wsT = GB("wsT")
            st4s = [yview(2 * D + 2 * G * 128 + 1024 + G * 64 + 136 + i * 8, 8, F32) for i in range(2)]; b_st4s = [GB("st4_0"), GB("st4_1")]
            vss = [yview(2 * D + 2 * G * 128 + 1024 + G * 64 + 160 + i * (D // 2), D // 2, BF16) for i in range(2)]; b_vss = [GB("vs0"), GB("vs1")]
            assert 2 * D + 2 * G * 128 + 1024 + G * 64 + 160 + D <= YW - 1024
            wst = yview(2 * D + 2 * G * 128 + 1024 + G * 64 + 8, 128, F32); b_wst = GB("wst")
            LD(sp, lnG, ln_g.broadcast_to([128, D]), b_lnG)
            LD(sp, lnB, ln_b.broadcast_to([128, D]), b_lnB)
            LD(sp, bsbc, b_s_d.broadcast_to([128, G * 128]), b_bsbc)
            for g in range(G):
                LD(sp, wst, w_s_d[g], b_wst)
                tr(banks[0][:, 0:128], wst, ident[:], [b_wst, b_ident], [bbuf[0]])
                CP(dve, wsT[:, g, :], banks[0][:, 0:128], [bbuf[0]], [b_wsT])

            C_G = 0.044715 ** 0.5

            def gelu_A(gi, src, n, rb):
                A(g1s[gi][:, 0:n], src, AF.Square, rb, [b_g1s[gi]], scale=C_G)
                STT(dve, g1s[gi][:, 0:n], g1s[gi][:, 0:n], 1.0, src, ALU.add, ALU.mult, [b_g1s[gi]] + rb, [b_g1s[gi]])

            def gelu_B(gi, dst, src, n, rb, wb):
                A(g2s[gi][:, 0:n], g1s[gi][:, 0:n], AF.Sigmoid, [b_g1s[gi]], [b_g2s[gi]], scale=1.5957691216057308)
                TT(dve, dst, g2s[gi][:, 0:n], src, ALU.mult, [b_g2s[gi]] + rb, wb)

            for t5 in range(S // QT):
                def uA(g):
                    bk = g % 2
                    for dc in range(DC):
                        mm(banks[bk][:, 0:QT], wu[:, dc, g * 128:(g + 1) * 128], hT[:, dc, t5 * QT:(t5 + 1) * QT], dc == 0, dc == DC - 1,
                           [b_wu, b_hT], [bbuf[bk]])
                    gelu_A(g % 2, banks[bk][:, 0:QT], QT, [bbuf[bk]])
                uA(0)
                for g in range(G):
                    if g + 1 < G:
                        uA(g + 1)
                    gelu_B(g % 2, uTt[:, g, :], banks[g % 2][:, 0:QT], QT, [bbuf[g % 2]], [b_uTt])
                NHB = D // DB

                def stA(tcl):
                    tc = t5 * NQC_ + tcl
                    sfull, b_sfull = sfulls[tc % 2], b_sfulls[tc % 2]

                    def sA(hb_):
                        bk = 2 + hb_ % 2
                        for dc in range(DC):
                            mm(banks[bk][:, 0:DB], hT[:, dc, tc * 128:(tc + 1) * 128], wsw[:, dc, hb_ * DB:(hb_ + 1) * DB], dc == 0, dc == DC - 1,
                               [b_hT, b_wsw], [bbuf[bk]])
                        gelu_A(hb_ % 2, banks[bk][:, 0:DB], DB, [bbuf[bk]])
                    sA(0)
                    for hb_ in range(NHB):
                        if hb_ + 1 < NHB:
                            sA(hb_ + 1)
                        gelu_B(hb_ % 2, sfull[:, hb_ * DB:(hb_ + 1) * DB], banks[2 + hb_ % 2][:, 0:DB], DB, [bbuf[2 + hb_ % 2]], [b_sfull])

                def stB(tcl):
                    tc = t5 * NQC_ + tcl
                    sfull, b_sfull = sfulls[tc % 2], b_sfulls[tc % 2]
                    vs, b_vs = vss[tc % 2], b_vss[tc % 2]
                    st4, b_st4 = st4s[tc % 2], b_st4s[tc % 2]
                    dve.op((lambda st4=st4, sfull=sfull: (lambda e: e.tensor_reduce(out=st4[:, 0:1], in_=sfull, axis=AX.X, op=ALU.add)))(), [b_sfull], [b_st4])
                    TS(dve, st4[:, 1:2], st4[:, 0:1], -1.0 / D, None, ALU.mult, None, [b_st4], [b_st4])
                    TS(dve, sfull, sfull, st4[:, 1:2], None, ALU.add, None, [b_sfull, b_st4], [b_sfull])
                    A(tmp2[:, 0:D] if G * 128 >= D else tmp2, sfull, AF.Square, [b_sfull], [b_tmp2, b_st4], accum_out=st4[:, 2:3])
                    rsqrt_col(st4[:, 4:5], st4[:, 2:3], 1.0 / D, st4[:, 3:4], [b_st4, b_eps], [b_st4])
                    STT(dve, sfull, sfull, st4[:, 4:5], lnG, ALU.mult, ALU.mult, [b_sfull, b_st4, b_lnG], [b_sfull])
                    TT(pool, vs, sfull, lnB, ALU.add, [b_sfull, b_lnB], [b_vs])

                def stC(tcl):
                    tc = t5 * NQC_ + tcl
                    vs, b_vs = vss[tc % 2], b_vss[tc % 2]
                    for g in range(G):
                        bk = 4 + g // 4
                        pe.op((lambda bk=bk, g=g, vs=vs: (lambda e: e.matmul(banks[bk][:, (g % 4) * 128:(g % 4) * 128 + 128], lhsT=vs[:, g * 128:(g + 1) * 128],
                                                                             rhs=wsT[:, g, :], start=True, stop=True, skip_group_check=True)))(),
                              [b_vs, b_wsT], [bbuf[bk]])
                    for gb in range((G + 3) // 4):
                        ng = min(4, G - gb * 4)
                        TT(dve, tmp2[:, gb * 512:gb * 512 + ng * 128], banks[4 + gb][:, 0:ng * 128], bsbc[:, gb * 512:gb * 512 + ng * 128], ALU.add,
                           [bbuf[4 + gb], b_bsbc], [b_tmp2])
                    TT(pool, zT[:, :, tc * 128:(tc + 1) * 128], tmp2.rearrange("p (g t) -> p g t", g=G), uTt[:, :, tcl * 128:(tcl + 1) * 128], ALU.mult,
                       [b_tmp2, b_uTt], [b_zT])

                stA(0)
                for tcl in range(NQC_):
                    if tcl + 1 < NQC_:
                        stA(tcl + 1)
                    stB(tcl)
                    stC(tcl)
            if b == 0:
                dump("zT", zT, [128, G, S], BF16, [b_zT])
            K.barrier()

            CW = DC * 64
            wsl = [[wview(i * 4 * CW + k * CW, CW, BF16, "p (c n) -> p c n", c=DC) for k in range(4)] for i in range(2)]
            b_wsl = [GB("wsl0"), GB("wsl1")]
            so5 = 8 * CW
            s12 = [[wview(so5 + (i * 2 + k) * QT, QT, F32) for k in range(2)] for i in range(2)]
            b_s12 = [[GB("s12_%d%d" % (i, k)) for k in range(2)] for i in range(2)]
            it = 0

            def load_s5(j):
                i = j % 2
                srcs = (w_ap[:, j * 128:(j + 1) * 128], w_in[:, 3 * AW + 2 * D + j * 128:3 * AW + 2 * D + (j + 1) * 128],
                        w_sp[:, j * 128:(j + 1) * 128], w_in[:, 3 * AW + 3 * D + j * 128:3 * AW + 3 * D + (j + 1) * 128])
                for k in range(4):
                    LD(pool, wsl[i][k], srcs[k].rearrange("(c p) n -> p c n", p=128), b_wsl[i])
            load_s5(0)
            for j in range(DC):
                i = j % 2
                if j + 1 < DC:
                    load_s5(j + 1)
                for t5 in range(S // QT):
                    p = (it % 2) * 4
                    ii = it % 2
                    it += 1
                    tok = slice(t5 * QT, (t5 + 1) * QT)
                    opnds = ((yaT, b_yaT, NH), (hT, b_hT, DC), (zT, b_zT, G), (hT, b_hT, DC))
                    for k in range(4):
                        src, b_src, nk = opnds[k]
                        for kc in range(nk):
                            mm(banks[p + k][:, 0:QT], wsl[i][k][:, kc, :], src[:, kc, tok], kc == 0, kc == nk - 1, [b_wsl[i], b_src], [bbuf[p + k]])
                    A(s12[ii][0], banks[p + 1][:, 0:QT], AF.Sigmoid, [bbuf[p + 1]], [b_s12[ii][0]])
                    A(s12[ii][1], banks[p + 3][:, 0:QT], AF.Sigmoid, [bbuf[p + 3]], [b_s12[ii][1]])
                    TT(dve, s12[ii][0], s12[ii][0], banks[p + 0][:, 0:QT], ALU.mult, [b_s12[ii][0], bbuf[p + 0]], [b_s12[ii][0]])
                    TT(dve, s12[ii][1], s12[ii][1], banks[p + 2][:, 0:QT], ALU.mult, [b_s12[ii][1], bbuf[p + 2]], [b_s12[ii][1]])
                    TT(pool, yT[:, j, tok], s12[ii][0], s12[ii][1], ALU.add, [b_s12[ii][0], b_s12[ii][1]], [b_yT])
            if b == 0:
                dump("yT", yT, [128, DC, S], BF16, [b_yT])
            K.barrier()

            bcast_mod(b, 2, bcA, b_bcA)
            wo = wview(0, DC * D // 2, BF16, "p (c n) -> p c n", c=DC); b_wo = GB("wo")
            LD(pool, wo, w_out.rearrange("(c p) n -> p c n", p=128), b_wo)
            xt6 = [wview(DC * D // 2 + i * D, D, F32) for i in range(2)]; b_xt6 = [GB("xt6_0"), GB("xt6_1")]
            tm6 = wview(DC * D // 2 + 2 * D, D, F32); b_tm6 = GB("tm6")
            for tc in range(TC):
                i = tc % 2
                LD(sp, xt6[i], x_d[b, tc * 128:(tc + 1) * 128, :], b_xt6[i])
                for hb_ in range(D // DB):
                    bk = (tc % 2) * 2 + hb_ % 2
                    for kc in range(DC):
                        mm(banks[bk][:, 0:DB], yT[:, kc, tc * 128:(tc + 1) * 128], wo[:, kc, hb_ * DB:(hb_ + 1) * DB], kc == 0, kc == DC - 1,
                           [b_yT, b_wo], [bbuf[bk]])
                    TT(dve, tm6[:, hb_ * DB:(hb_ + 1) * DB], banks[bk][:, 0:DB], bcA[:, hb_ * DB:(hb_ + 1) * DB], ALU.mult, [bbuf[bk], b_bcA], [b_tm6])
                TT(pool, x1[:, tc, :], tm6, xt6[i], ALU.add, [b_tm6, b_xt6[i]], [b_x1[tc]])
            if b == 0:
                dump("x1", x1, [128, TC, D], F32, b_x1)
            K.barrier()

            bcast_mod(b, 4, bcA, b_bcA)
            bcast_mod(b, 3, bcB, b_bcB)
            norm_to_hT(b, lambda tc, xt_i, b_xt_i: (x1[:, tc, :], b_x1[tc]), None, "n2")
            if b == 0:
                dump("h2T", hT, [128, DC, S], BF16, [b_hT])
            K.barrier()

            lg = wview(0, NR + 4, F32)[:, 0:NR]; b_lg = GB("lg")
            r8 = wview(64, 16, F32); b_r8 = GB("r8")
            gm = wview(96, NG, F32); b_gm = GB("gm")
            els = wview(128, EPG, F32); b_els = GB("els")
            m8 = wview(160, 8, F32); b_m8 = GB("m8")
            cws = wview(192, EPG, F32); b_cws = GB("cws")
            cws2 = wview(224, EPG, F32)
            cw = wview(256, NE, F32); b_cw = GB("cw")
            cwT = [wview(512 + i * 128, 128, F32) for i in range(2)]; b_cwT = [GB("cwT0"), GB("cwT1")]
            for tc in range(TC):
                bk = tc % 2
                for dc in range(DC):
                    mm(banks[bk][:, 0:NR], hT[:, dc, tc * 128:(tc + 1) * 128], wrt[:, dc, :], dc == 0, dc == DC - 1, [b_hT, b_wrt], [bbuf[bk]])
                TT(dve, lg, banks[bk][:, 0:NR], brt[:], ALU.add, [bbuf[bk], b_brt], [b_lg])
                dve.op(lambda e: e.tensor_reduce(out=r8[:, 0:1], in_=lg[:, 0:NG], axis=AX.X, op=ALU.max), [b_lg], [b_r8])
                TS(dve, gm, lg[:, 0:NG], r8[:, 0:1], None, ALU.is_equal, None, [b_lg, b_r8], [b_gm])
                TS(dve, r8[:, 1:2], r8[:, 0:1], -1.0, None, ALU.mult, None, [b_r8], [b_r8])
                A(cws2[:, 0:NG], lg[:, 0:NG], AF.Exp, [b_lg, b_r8], [b_cws, b_r8], bias=r8[:, 1:2], scale=1.0, accum_out=r8[:, 2:3])
                dve.op(lambda e: e.reciprocal(out=r8[:, 3:4], in_=r8[:, 2:3]), [b_r8], [b_r8])
                TS(dve, els, lg[:, NG:NG + EPG], gm[:, 0:1], None, ALU.mult, None, [b_lg, b_gm], [b_els])
                for g in range(1, NG):
                    STT(dve, els, lg[:, NG + g * EPG:NG + (g + 1) * EPG], gm[:, g:g + 1], els, ALU.mult, ALU.add, [b_lg, b_gm, b_els], [b_els])
                dve.op(lambda e: e.max(out=m8, in_=els), [b_els], [b_m8])
                TT(dve, r8[:, 4:5], m8[:, 1:2], m8[:, 0:1], ALU.subtract, [b_m8], [b_r8])
                A(r8[:, 5:6], r8[:, 4:5], AF.Exp, [b_r8], [b_r8])
                TS(dve, r8[:, 6:7], r8[:, 5:6], 1.0, None, ALU.add, None, [b_r8], [b_r8])
                dve.op(lambda e: e.reciprocal(out=r8[:, 7:8], in_=r8[:, 6:7]), [b_r8], [b_r8])
                TT(dve, r8[:, 8:9], r8[:, 7:8], r8[:, 3:4], ALU.mult, [b_r8], [b_r8])
                TT(dve, r8[:, 9:10], r8[:, 3:4], r8[:, 8:9], ALU.subtract, [b_r8], [b_r8])
                TS(dve, cws, els, m8[:, 0:1], r8[:, 8:9], ALU.is_equal, ALU.mult, [b_els, b_m8, b_r8], [b_cws])
                TS(dve, cws2, els, m8[:, 1:2], r8[:, 9:10], ALU.is_equal, ALU.mult, [b_els, b_m8, b_r8, b_cws], [b_cws])
                TT(dve, cws, cws, cws2, ALU.add, [b_cws], [b_cws])
                for g in range(NG):
                    TS(dve, cw[:, g * EPG:(g + 1) * EPG], cws, gm[:, g:g + 1], None, ALU.mult, None, [b_cws, b_gm], [b_cw])
                tr(banks[2 + bk][0:NE, 0:128], cw, ident[:], [b_cw, b_ident], [bbuf[2 + bk]])
                CP(dve, cwT[bk][0:NE, :], banks[2 + bk][0:NE, 0:128], [bbuf[2 + bk]], [b_cwT[bk]])
                sp.dma((lambda bk=bk, tc=tc: (lambda e: e.dma_start(out=cw_scr[:, tc * 128:(tc + 1) * 128], in_=cwT[bk][0:NE, :])))(),
                       b_cwT[bk], [b_cwT[bk]], [b_cwscr])
            if b == 0:
                dump("cw", cw_scr, [NE, S], F32, [b_cwscr])
            K.barrier()

            bcast_mod(b, 5, bcA, b_bcA)
            MT = min(256, S)
            NMT = S // MT
            MC = MT // 128
            GUW = DC * DE
            DNW = FC * D // 2
            EW = GUW + DNW
            wslot = []
            for si in range(4):
                base_ = (o_y + si * EW) if si < 2 else (o_w + (si - 2) * EW)
                wslot.append((view(base_, GUW, BF16, "p (c n) -> p c n", c=DC), view(base_ + GUW, DNW, BF16, "p (f d) -> p f d", f=FC)))
            assert 2 * EW <= YW
            b_wslot = [GB("wslot%d" % si) for si in range(4)]
            o9 = 2 * EW
            cwbc = [[wview(o9 + (i * 2 + k) * MT, MT, F32) for k in range(2)] for i in range(2)]
            b_cwbc = [[GB("cwbc%d%d" % (i, k)) for k in range(2)] for i in range(2)]
            o9 += 4 * MT
            sg9 = [wview(o9 + i * FC * MT, FC * MT, F32) for i in range(2)]; b_sg9 = [GB("sg9_0"), GB("sg9_1")]
            o9 += 2 * FC * MT
            tm9 = [wview(o9 + i * 512, 512, F32) for i in range(2)]; b_tm9 = [GB("tm9_0"), GB("tm9_1")]
            o9 += 1024
            assert FC * MT <= 512
            actp2 = [[wview(o9 + (i * 2 + k) * (FC * MT // 2), FC * MT // 2, BF16, "p (f t) -> p f t", f=FC) for k in range(2)] for i in range(2)]
            b_actp2 = [[GB("actp%d%d" % (i, k)) for k in range(2)] for i in range(2)]
            o9 += 2 * FC * MT
            assert o9 <= WW, o9
            ycnt = [0]

            def load_pair(ep):
                for e_ in range(2):
                    e = ep * 2 + e_
                    si = e % 4
                    LD(pool, wslot[si][0], w_gu[e].rearrange("(c p) n -> p c n", p=128), b_wslot[si])
                    LD(pool, wslot[si][1], w_dn[e].rearrange("(f p) d -> p f d", p=128), b_wslot[si])
                    for f in range(FC):
                        TT(pool, wslot[si][1][:, f, :], wslot[si][1][:, f, :], bcA[:], ALU.mult, [b_wslot[si], b_bcA], [b_wslot[si]])

            def emit_gu(k, ep, mt):
                tok = slice(mt * MT, (mt + 1) * MT)
                for e_ in range(2):
                    e = ep * 2 + e_
                    si = e % 4
                    wg = wslot[si][0]
                    LD(sp, cwbc[e_][mt % 2], cw_scr[e:e + 1, tok].broadcast_to([128, MT]), b_cwbc[e_][mt % 2], reads=[b_cwscr])
                    pb = e_ * 2
                    for n in range(2 * FC):
                        bk = pb + n // FC
                        col = (n % FC) * MT
                        for dc in range(DC):
                            pe.op((lambda bk=bk, col=col, wg=wg, dc=dc, n=n, tok=tok: (lambda e__: e__.matmul(
                                banks[bk][:, col:col + MT], lhsT=wg[:, dc, n * 128:(n + 1) * 128], rhs=hT[:, dc, tok],
                                start=(dc == 0), stop=(dc == DC - 1), skip_group_check=True)))(),
                                [b_wslot[si], b_hT], [bbuf[bk]], sig=(dc == DC - 1))
                    gps = banks[pb][:, 0:FC * MT]
                    ups = banks[pb + 1][:, 0:FC * MT]
                    A(sg9[e_], gps, AF.Sigmoid, [bbuf[pb]], [b_sg9[e_]])
                    TT(dve, sg9[e_], sg9[e_], gps, ALU.mult, [b_sg9[e_], bbuf[pb]], [b_sg9[e_]])
                    TT(dve, sg9[e_], sg9[e_], ups, ALU.mult, [b_sg9[e_], bbuf[pb + 1]], [b_sg9[e_]])
                    for f in range(FC):
                        TT(pool, actp2[e_][k % 2][:, f, :], sg9[e_][:, f * MT:(f + 1) * MT], cwbc[e_][mt % 2], ALU.mult,
                           [b_sg9[e_], b_cwbc[e_][mt % 2]], [b_actp2[e_][k % 2]])

            def emit_down(k, ep, mt):
                for mc in range(MC):
                    tc = mt * MC + mc
                    for hb_ in range(D // DB):
                        bk = 4 + ycnt[0] % 4
                        ti = ycnt[0] % 2
                        ycnt[0] += 1
                        nmm = 0
                        for e_ in range(2):
                            wd_ = wslot[(ep * 2 + e_) % 4][1]
                            for f in range(FC):
                                mm(banks[bk][:, 0:DB], actp2[e_][k % 2][:, f, mc * 128:(mc + 1) * 128], wd_[:, f, hb_ * DB:(hb_ + 1) * DB],
                                   nmm == 0, nmm == 2 * FC - 1, [b_actp2[e_][k % 2], b_wslot[(ep * 2 + e_) % 4]], [bbuf[bk]])
                                nmm += 1
                        TT(dve, x1[:, tc, hb_ * DB:(hb_ + 1) * DB], banks[bk][:, 0:DB], x1[:, tc, hb_ * DB:(hb_ + 1) * DB], ALU.add,
                           [bbuf[bk], b_x1[tc]], [b_x1[tc]])

            items = [(ep, mt) for ep in range(NE // 2) for mt in range(NMT)]
            load_pair(0)
            for k, (ep, mt) in enumerate(items):
                emit_gu(k, ep, mt)
                if k >= 1:
                    emit_down(k - 1, *items[k - 1])
                if mt == 0 and ep + 1 < NE // 2:
                    load_pair(ep + 1)
            emit_down(len(items) - 1, *items[-1])
            if b == 0:
                dump("x2", x1, [128, TC, D], F32, b_x1)
            K.barrier()

            ot = [wview(i * D, D, F32) for i in range(2)]; b_ot = [GB("ot0"), GB("ot1")]
            jk = wview(2 * D, D, F32); b_jk = GB("jk10")
            st10 = wview(3 * D, 3 * TC, F32); b_st10 = GB("st10")
            for tc in range(TC):
                act.op((lambda tc=tc: (lambda e: e.activation(out=jk, in_=x1[:, tc, :], func=AF.Square, accum_out=st10[:, tc:tc + 1])))(),
                       [b_x1[tc]], [b_jk, b_st10])
            rsqrt_col(st10[:, 2 * TC:3 * TC], st10[:, 0:TC], 1.0 / D, st10[:, TC:2 * TC], [b_st10, b_eps], [b_st10])
            for tc in range(TC):
                i = tc % 2
                STT(dve, ot[i], x1[:, tc, :], st10[:, 2 * TC + tc:2 * TC + tc + 1], fgbc[:], ALU.mult, ALU.mult, [b_x1[tc], b_st10, b_fgbc], [b_ot[i]])
                sp.dma((lambda i=i, tc=tc, b=b: (lambda e: e.dma_start(out=y_d[b, tc * 128:(tc + 1) * 128, :], in_=ot[i])))(), b_ot[i], [b_ot[i]], [])
            K.barrier()
        K.barrier()
        K.emit()
    return nc


_NC_CACHE = {}


def _prep_shared(inp, cfg):
    f = lambda a: np.ascontiguousarray(np.asarray(a, dtype=np.float32))
    L = 0
    sh = {
        "w_ada": f(inp["w_ada"][L]), "b_ada": f(inp["b_ada"][L]).reshape(1, -1), "norm1_g": f(inp["norm1_g"][L]).reshape(1, -1),
        "w_in": f(inp["w_in"][L]),
        "lam4": f(np.stack([np.asarray(inp["lambda_q1"][L]), np.asarray(inp["lambda_k1"][L]),
                            np.asarray(inp["lambda_q2"][L]), np.asarray(inp["lambda_k2"][L])], axis=0)),
        "subln_g": f(inp["subln_g"][L]).reshape(1, -1), "w_attn_proj": f(inp["w_attn_proj"][L]),
        "sgu_ln_g": f(inp["sgu_ln_g"][L]).reshape(1, -1), "sgu_ln_b": f(inp["sgu_ln_b"][L]).reshape(1, -1),
        "sgu_w_s": f(inp["sgu_w_s"][L]), "sgu_b_s": f(inp["sgu_b_s"][L]).reshape(1, -1),
        "w_sgu_proj": f(inp["w_sgu_proj"][L]), "w_out": f(inp["w_out"][L]), "norm2_g": f(inp["norm2_g"][L]).reshape(1, -1),
        "w_router": f(np.concatenate([np.asarray(inp["w_router_group"][L]), np.asarray(inp["w_router_expert"][L])], axis=1)),
        "b_router": f(np.concatenate([np.asarray(inp["b_router_group"][L]), np.asarray(inp["b_router_expert"][L])], axis=0)).reshape(1, -1),
        "w_expert_gate_up": f(inp["w_expert_gate_up"][L]), "w_expert_down": f(inp["w_expert_down"][L]),
        "final_g": f(inp["final_g"]).reshape(1, -1),
    }
    return sh


def kernel(**inputs):
    cfg = Cfg()
    n_cores = 8
    x = np.asarray(inputs["x"], dtype=np.float32)
    c = np.asarray(inputs["c"], dtype=np.float32)
    sh = _prep_shared(inputs, cfg)
    if "nc" not in _NC_CACHE:
        _NC_CACHE["nc"] = build(cfg)
    nc = _NC_CACHE["nc"]
    in_maps = []
    for i in range(n_cores):
        m = dict(sh)
        m["x"] = np.ascontiguousarray(x[i * cfg.NB:(i + 1) * cfg.NB])
        m["c"] = np.ascontiguousarray(c[i * cfg.NB:(i + 1) * cfg.NB])
        in_maps.append(m)
    res = run_bass_kernel_spmd(nc, in_maps, core_ids=list(range(n_cores)))
    return np.concatenate([r["y"] for r in res.results], axis=0).astype(np.float32)
```

```python
import math
from contextlib import ExitStack
import numpy as np
import concourse.bass as bass
import concourse.mybir as mybir
from concourse.bass_utils import run_bass_kernel_spmd

F32 = mybir.dt.float32
BF16 = mybir.dt.bfloat16
AF = mybir.ActivationFunctionType
ALU = mybir.AluOpType
AX = mybir.AxisListType
EPS = 1e-6


class Buf:
    __slots__ = ("name", "w", "r", "dsem", "dcnt")

    def __init__(self, name):
        self.name = name
        self.w = None
        self.r = {}
        self.dsem = None
        self.dcnt = 0


class Eng:
    def __init__(self, K, name, sem, self_raw=True):
        self.K = K
        self.name = name
        self.sem = sem
        self.cnt = 0
        self.known = {}
        self.self_raw = self_raw
        self.prog = []

    def wait(self, ev):
        if ev is None:
            return
        sem, val = ev
        if self.known.get(id(sem), 0) >= val:
            return
        self.prog.append(("w", sem, val))
        self.known[id(sem)] = val

    def _deps(self, reads, writes):
        for b in reads:
            if b.w is not None:
                if b.w[0] is self.sem and not self.self_raw:
                    continue
                self.wait(b.w)
        for b in writes:
            if b.w is not None and (b.w[0] is not self.sem or self.self_raw):
                self.wait(b.w)
            for sem, v in b.r.values():
                if sem is not self.sem or self.self_raw:
                    self.wait((sem, v))

    def op(self, fn, reads=(), writes=(), sig=True):
        self._deps(reads, writes)
        if sig:
            self.cnt += 1
            self.prog.append(("o", fn, self.sem, 1))
            ev = (self.sem, self.cnt)
        else:
            self.prog.append(("o", fn, None, 0))
            ev = (self.sem, self.cnt + 1)
        for b in reads:
            b.r[id(self.sem)] = ev
        for b in writes:
            b.w = ev
            b.r = {}
        return ev

    def dma(self, fn, anchor, reads=(), writes=()):
        self._deps(reads, writes)
        if anchor.dsem is None:
            anchor.dsem = self.K.new_sem("d_" + anchor.name)
            self.K.dma_bufs.append(anchor)
        anchor.dcnt += 16
        self.prog.append(("o", fn, anchor.dsem, 16))
        ev = (anchor.dsem, anchor.dcnt)
        for b in reads:
            b.r[id(anchor.dsem)] = ev
        for b in writes:
            b.w = ev
            b.r = {}
        return ev

    def replay(self, eng):
        for it in self.prog:
            if it[0] == "w":
                eng.wait_ge(it[1], it[2])
            else:
                ins = it[1](eng)
                if it[2] is not None:
                    ins.then_inc(it[2], it[3])


class Kern:
    def __init__(self, nc, stack):
        self.nc = nc
        self.stack = stack
        self.dma_bufs = []
        self.pe = Eng(self, "pe", self.new_sem("s_pe"), self_raw=False)
        self.act = Eng(self, "act", self.new_sem("s_act"))
        self.dve = Eng(self, "dve", self.new_sem("s_dve"))
        self.pool = Eng(self, "pool", self.new_sem("s_pool"))
        self.sp = Eng(self, "sp", self.new_sem("s_sp"))
        self.engs = [self.pe, self.act, self.dve, self.pool, self.sp]

    def new_sem(self, name):
        return self.stack.enter_context(self.nc.semaphore(name))

    def sb(self, name, shape, dt):
        return self.stack.enter_context(self.nc.sbuf_tensor(name, shape, dt))

    def ps(self, name, shape, dt):
        return self.stack.enter_context(self.nc.psum_tensor(name, shape, dt))

    def barrier(self, engs=None):
        evs = [(e.sem, e.cnt) for e in self.engs if e.cnt > 0]
        evs += [(b.dsem, b.dcnt) for b in self.dma_bufs]
        for e in (engs or self.engs):
            for ev in evs:
                if ev[0] is not e.sem:
                    e.wait(ev)

    def emit(self):
        with self.nc.Block() as block:
            block.tensor(lambda e: self.pe.replay(e))
            block.scalar(lambda e: self.act.replay(e))
            block.vector(lambda e: self.dve.replay(e))
            block.gpsimd(lambda e: self.pool.replay(e))
            block.sync(lambda e: self.sp.replay(e))


class Cfg:
    def __init__(self, D=1024, S=2048, NB=2, NH=8, NG=4, EPG=8, DE=256, depth_l=0):
        self.D, self.S, self.NB, self.NH, self.NG, self.EPG, self.DE = D, S, NB, NH, NG, EPG, DE
        self.DC = D // 128
        self.TC = S // 128
        self.AW = NH * 128
        self.G = D // 128
        self.NE = NG * EPG
        self.FC = DE // 128
        self.INW = 3 * self.AW + 4 * D
        self.QT = min(512, S)
        self.lam_init = 0.8 - 0.6 * math.exp(-0.3 * depth_l)
        self.slopes = [2.0 ** (-8.0 * (h + 1) / NH) for h in range(NH)]


def build(cfg, stop_after=None):
    DB = min(512, cfg.D)
    D, S, NB, NH, NG, EPG, DE = cfg.D, cfg.S, cfg.NB, cfg.NH, cfg.NG, cfg.EPG, cfg.DE
    DC, TC, AW, G, NE, FC, INW, QT = cfg.DC, cfg.TC, cfg.AW, cfg.G, cfg.NE, cfg.FC, cfg.INW, cfg.QT
    NR = NG + NE
    nc = bass.Bass("TRN2", target_bir_lowering=False)
    _bufs = {}

    def GB(name):
        if name not in _bufs:
            _bufs[name] = Buf(name)
        return _bufs[name]

    def din(name, shape):
        return nc.dram_tensor(name, list(shape), F32, kind="ExternalInput").ap()

    x_d = din("x", [NB, S, D])
    c_d = din("c", [NB, D])
    w_ada = din("w_ada", [D, 6 * D])
    b_ada = din("b_ada", [1, 6 * D])
    norm1_g = din("norm1_g", [1, D])
    w_in = din("w_in", [D, INW])
    lam_d = din("lam4", [4, 64])
    subln_g = din("subln_g", [1, 128])
    w_ap = din("w_attn_proj", [AW, D])
    ln_g = din("sgu_ln_g", [1, D])
    ln_b = din("sgu_ln_b", [1, D])
    w_s_d = din("sgu_w_s", [G, 128, 128])
    b_s_d = din("sgu_b_s", [1, G * 128])
    w_sp = din("w_sgu_proj", [D, D])
    w_out = din("w_out", [D, D])
    norm2_g = din("norm2_g", [1, D])
    w_rt = din("w_router", [D, NR])
    b_rt = din("b_router", [1, NR])
    w_gu = din("w_expert_gate_up", [NE, D, 2 * DE])
    w_dn = din("w_expert_down", [NE, DE, D])
    final_g = din("final_g", [1, D])
    y_d = nc.dram_tensor("y", [NB, S, D], F32, kind="ExternalOutput").ap()

    with ExitStack() as st:
        K = Kern(nc, st)
        pe, act, dve, pool, sp = K.pe, K.act, K.dve, K.pool, K.sp

        banks = [K.ps("bank%d" % i, [128, 512], F32) for i in range(8)]
        bbuf = [GB("bank%d" % i) for i in range(8)]

        ident = K.sb("ident", [128, 128], F32); b_ident = GB("ident")
        identb = K.sb("identb", [128, 128], BF16); b_identb = GB("identb")
        bcA = K.sb("bcA", [128, D], F32); b_bcA = GB("bcA")
        bcB = K.sb("bcB", [128, D], F32); b_bcB = GB("bcB")
        fgbc = K.sb("fgbc", [128, D], F32); b_fgbc = GB("fgbc")
        subg = K.sb("subg", [128, 128], F32); b_subg = GB("subg")
        lam_t = K.sb("lam_t", [128, 8], F32); b_lam = GB("lam")
        brt = K.sb("brt", [128, NR], F32); b_brt = GB("brt")
        wrt = K.sb("wrt", [128, DC, NR], BF16); b_wrt = GB("wrt")
        mod_scr = nc.dram_tensor("mod_scr", [NB, 6 * D], F32, kind="Internal").ap(); b_modscr = GB("mod_scr")
        cw_scr = nc.dram_tensor("cw_scr", [NE, S], F32, kind="Internal").ap(); b_cwscr = GB("cw_scr")

        HW_ = DC * S // 2
        VW = TC * NH * 130 // 2 + 2
        o_hT = 0
        o_ya = o_hT + HW_
        o_z = o_ya + HW_
        ZW = max(HW_, VW)
        o_y = o_z + ZW
        YW = max(HW_, 2 * S + 4096)
        o_w = o_y + YW
        WW = 12 * 1024
        ARENA = o_w + WW
        arena = K.sb("arena", [128, ARENA], F32)

        def view(off, words, dt, pattern=None, **kw):
            a = arena[:, off:off + words]
            if dt is BF16:
                a = a.bitcast(BF16)
            if pattern:
                a = a.rearrange(pattern, **kw)
            return a

        hT = view(o_hT, HW_, BF16, "p (c s) -> p c s", c=DC); b_hT = GB("hT")
        yaT = view(o_ya, HW_, BF16, "p (c s) -> p c s", c=NH) if NH == DC else None
        assert NH == DC
        b_yaT = GB("yaT")
        zT = view(o_z, HW_, BF16, "p (c s) -> p c s", c=G); b_zT = GB("zT")
        vaug = view(o_z, (TC * NH * 130) // 2, BF16, "p (t h e) -> p t h e", t=TC, h=NH); b_vaug = GB("vaug")
        yT = view(o_y, HW_, BF16, "p (c s) -> p c s", c=DC); b_yT = GB("yT")
        x1 = view(o_ya, TC * D, F32, "p (t d) -> p t d", t=TC); b_x1 = [GB("x1_%d" % i) for i in range(TC)]
        assert TC * D <= HW_ + ZW

        def mm(out, lhsT, rhs, start, stop, reads, writes, sig=None):
            if sig is None:
                sig = stop
            pe.op(lambda e: e.matmul(out, lhsT=lhsT, rhs=rhs, start=start, stop=stop), reads, writes, sig=sig)

        def tr(out, in_, idt, reads, writes, sig=True):
            pe.op(lambda e: e.transpose(out, in_, idt), reads, writes, sig=sig)

        def A(out, in_, func, reads, writes, **kw):
            act.op(lambda e: e.activation(out=out, in_=in_, func=func, **kw), reads, writes)

        def TT(eng, out, in0, in1, op, reads, writes):
            eng.op(lambda e: e.tensor_tensor(out=out, in0=in0, in1=in1, op=op), reads, writes)

        def TS(eng, out, in0, s1, s2, op0, op1, reads, writes, **kw):
            if op1 is None:
                eng.op(lambda e: e.tensor_scalar(out=out, in0=in0, scalar1=s1, scalar2=None, op0=op0, **kw), reads, writes)
            else:
                eng.op(lambda e: e.tensor_scalar(out=out, in0=in0, scalar1=s1, scalar2=s2, op0=op0, op1=op1, **kw), reads, writes)

        def STT(eng, out, in0, scalar, in1, op0, op1, reads, writes):
            eng.op(lambda e: e.scalar_tensor_tensor(out=out, in0=in0, scalar=scalar, in1=in1, op0=op0, op1=op1), reads, writes)

        def CP(eng, out, in_, reads, writes):
            if eng is act:
                act.op(lambda e: e.copy(out=out, in_=in_), reads, writes)
            else:
                eng.op(lambda e: e.tensor_copy(out=out, in_=in_), reads, writes)

        def LD(q, out, in_, anchor, reads=(), writes=None):
            q.dma(lambda e: e.dma_start(out=out, in_=in_), anchor, reads, [anchor] if writes is None else writes)

        dbg_outs = {}

        def dump(name, ap_, shape, dt, rb):
            if not getattr(cfg, "debug", False):
                return
            d = nc.dram_tensor("dbg_" + name, list(shape), dt, kind="ExternalOutput").ap()
            bb = GB("dbg_" + name)
            sp.dma(lambda e: e.dma_start(out=d, in_=ap_), bb, rb, [bb])
            dbg_outs[name] = d

        def rsqrt_col(dst, src, scale, tmp, rb, wb):
            A(tmp, src, AF.Sqrt, rb, wb, bias=epsc[:src.shape[0], 0:1], scale=scale)
            dve.op(lambda e: e.reciprocal(out=dst, in_=tmp), wb, wb)

        epsc = K.sb("epsc", [128, 1], F32); b_eps = GB("epsc")
        pool.op(lambda e: e.memset(epsc[:], EPS), (), [b_eps])
        pool.op(lambda e: e.memset(ident[:], 1.0), (), [b_ident])
        pool.op(lambda e: e.affine_select(out=ident[:], in_=ident[:], pattern=[[-1, 128]], compare_op=ALU.is_equal,
                                          fill=0.0, base=0, channel_multiplier=1), [b_ident], [b_ident])
        CP(dve, identb[:], ident[:], [b_ident], [b_identb])
        LD(sp, fgbc[:], final_g.broadcast_to([128, D]), b_fgbc)
        LD(sp, subg[:], subln_g.broadcast_to([128, 128]), b_subg)
        LD(sp, brt[:], b_rt.broadcast_to([128, NR]), b_brt)
        TS(dve, subg[:], subg[:], 1.0 - cfg.lam_init, None, ALU.mult, None, [b_subg], [b_subg])
        LD(pool, wrt[:], w_rt.rearrange("(c p) n -> p c n", p=128), b_wrt)
        lamw = K.sb("lamw", [128, 4, 64], F32); b_lamw = GB("lamw")
        LD(sp, lamw[:].rearrange("p a b -> p (a b)"), lam_d.rearrange("a b -> (a b)").rearrange("(o n) -> o n", o=1).broadcast_to([128, 256]), b_lamw)
        TT(dve, lamw[:, 0, :], lamw[:, 0, :], lamw[:, 1, :], ALU.mult, [b_lamw], [b_lamw])
        TT(dve, lamw[:, 2, :], lamw[:, 2, :], lamw[:, 3, :], ALU.mult, [b_lamw], [b_lamw])
        dve.op(lambda e: e.tensor_reduce(out=lam_t[:, 0:1], in_=lamw[:, 0, :], axis=AX.X, op=ALU.add), [b_lamw], [b_lam])
        dve.op(lambda e: e.tensor_reduce(out=lam_t[:, 1:2], in_=lamw[:, 2, :], axis=AX.X, op=ALU.add), [b_lamw], [b_lam])
        A(lam_t[:, 2:4], lam_t[:, 0:2], AF.Exp, [b_lam], [b_lam])
        TT(dve, lam_t[:, 4:5], lam_t[:, 2:3], lam_t[:, 3:4], ALU.subtract, [b_lam], [b_lam])
        TS(dve, lam_t[:, 5:6], lam_t[:, 4:5], cfg.lam_init, -1.0, ALU.add, ALU.mult, [b_lam], [b_lam])

        def wview(off_words, words, dt, pattern=None, rows=None, **kw):
            assert off_words + words <= WW, (off_words, words, WW)
            a_ = arena[:, o_w + off_words:o_w + off_words + words] if rows is None else arena[0:rows, o_w + off_words:o_w + off_words + words]
            if dt is BF16:
                a_ = a_.bitcast(BF16)
            if pattern:
                a_ = a_.rearrange(pattern, **kw)
            return a_

        def yview(off_words, words, dt, pattern=None, **kw):
            assert off_words + words <= YW, (off_words, words, YW)
            return view(o_y + off_words, words, dt, pattern, **kw)

        modv = view(o_y, 6 * D, F32)[0:NB, :]; b_modv = GB("modv")
        c_sb = view(o_y + 6 * D, D, F32)[0:NB, :]; b_c = GB("c_sb")
        sgc = view(o_y + 7 * D, D, F32)[0:NB, :]
        g2row = view(o_hT, 2 * D, F32, "p (a d) -> p a d", a=2)[0:NB]; b_g2row = GB("g2row")
        siluT = wview(0, DC * NB // 2 + 1, BF16)[:, 0:DC * NB].rearrange("p (c b) -> p c b", c=DC); b_siluT = GB("siluT")
        LD(sp, c_sb, c_d, b_c)
        A(sgc, c_sb, AF.Sigmoid, [b_c], [b_c])
        TT(dve, c_sb, c_sb, sgc, ALU.mult, [b_c], [b_c])
        pt = banks[0]
        for dc in range(DC):
            tr(pt[:, dc * NB:(dc + 1) * NB], c_sb[:, dc * 128:(dc + 1) * 128], ident[0:NB, 0:NB], [b_c, b_ident], [bbuf[0]])
        CP(dve, siluT.rearrange("p c b -> p (c b)"), pt[:, 0:DC * NB], [bbuf[0]], [b_siluT])
        LD(sp, modv, b_ada.broadcast_to([NB, 6 * D]), b_modv)
        NBLK = 6 * D // 512
        wa = [wview(64 + i * (DC * 256), DC * 256, BF16, "p (c n) -> p c n", c=DC) for i in range(2)]
        b_wa = [GB("wa0"), GB("wa1")]
        for blk in range(NBLK):
            i = blk % 2
            LD(pool, wa[i], w_ada[:, blk * 512:(blk + 1) * 512].rearrange("(c p) n -> p c n", p=128), b_wa[i])
            bk = 1 + (blk % 2)
            for dc in range(DC):
                mm(banks[bk][0:NB, :], siluT[:, dc, :], wa[i][:, dc, :], dc == 0, dc == DC - 1, [b_siluT, b_wa[i]], [bbuf[bk]])
            TT(dve, modv[:, blk * 512:(blk + 1) * 512], modv[:, blk * 512:(blk + 1) * 512], banks[bk][0:NB, :], ALU.add,
               [b_modv, bbuf[bk]], [b_modv])
        LD(sp, g2row[:, 0, :], norm1_g.broadcast_to([NB, D]), b_g2row)
        LD(sp, g2row[:, 1, :], norm2_g.broadcast_to([NB, D]), b_g2row)
        for (gi, off) in ((0, 1), (1, 4)):
            STT(dve, modv[:, off * D:(off + 1) * D], modv[:, off * D:(off + 1) * D], 1.0, g2row[:, gi, :], ALU.add, ALU.mult,
                [b_modv, b_g2row], [b_modv])
        sp.dma(lambda e: e.dma_start(out=mod_scr, in_=modv), b_modv, [b_modv], [b_modscr])
        dump("modv", modv, [NB, 6 * D], F32, [b_modv])
        K.barrier()

        def bcast_mod(b, idx, dst, b_dst):
            sp.dma(lambda e: e.dma_start(out=dst[:], in_=mod_scr[b:b + 1, idx * D:(idx + 1) * D].broadcast_to([128, D])),
                   b_dst, [b_modscr], [b_dst])

        def norm_to_hT(b, src_fn, b_src_fn, tag):
            xt = [wview(i * D, D, F32) for i in range(2)]; b_xt = [GB(tag + "xt0"), GB(tag + "xt1")]
            junk = wview(2 * D, D, F32); b_junk = GB(tag + "junk")
            hb = [wview(3 * D + i * (D // 2), D // 2, BF16) for i in range(2)]; b_hb = [GB(tag + "hb0"), GB(tag + "hb1")]
            st_ = wview(4 * D, 3 * TC, F32); b_st = GB(tag + "st")
            nj = [wview(4 * D + 3 * TC + i * D, D, F32) for i in range(2)]; b_nj = [GB(tag + "nj0"), GB(tag + "nj1")]
            for tc in range(TC):
                src, b_src = src_fn(tc, xt[tc % 2], b_xt[tc % 2])
                act.op((lambda src=src, tc=tc: (lambda e: e.activation(out=junk, in_=src, func=AF.Square, accum_out=st_[:, tc:tc + 1])))(),
                       [b_src], [b_junk, b_st])
            rsqrt_col(st_[:, 2 * TC:3 * TC], st_[:, 0:TC], 1.0 / D, st_[:, TC:2 * TC], [b_st, b_eps], [b_st])
            for tc in range(TC):
                i = tc % 2
                src, b_src = src_fn(tc, xt[i], b_xt[i])
                STT(dve, nj[i], src, st_[:, 2 * TC + tc:2 * TC + tc + 1], bcA[:], ALU.mult, ALU.mult, [b_src, b_st, b_bcA], [b_nj[i]])
                TT(pool, hb[i], nj[i], bcB[:], ALU.add, [b_nj[i], b_bcB], [b_hb[i]])
                bk = tc % 2
                ptb = banks[bk][:].bitcast(BF16)
                for dc in range(DC):
                    tr(ptb[:, dc * 128:(dc + 1) * 128], hb[i][:, dc * 128:(dc + 1) * 128], identb[:], [b_hb[i], b_identb], [bbuf[bk]],
                       sig=(dc == DC - 1))
                CP(act, hT[:, :, tc * 128:(tc + 1) * 128], ptb[:, 0:DC * 128].rearrange("p (c t) -> p c t", c=DC), [bbuf[bk]], [b_hT])

        for b in range(NB):
            bcast_mod(b, 1, bcA, b_bcA)
            bcast_mod(b, 0, bcB, b_bcB)

            def src_x(tc, xt_i, b_xt_i):
                LD(sp, xt_i, x_d[b, tc * 128:(tc + 1) * 128, :], b_xt_i)
                return xt_i, b_xt_i
            norm_to_hT(b, src_x, None, "n1")
            if b == 0:
                dump("hT", hT, [128, DC, S], BF16, [b_hT])
            K.barrier()

            wv = wview(0, DC * AW // 2, BF16, "p (c n) -> p c n", c=DC); b_wv = GB("wv")
            LD(pool, wv, w_in[:, 2 * AW:3 * AW].rearrange("(c p) n -> p c n", p=128), b_wv)
            pool.op(lambda e: e.memset(vaug[:, :, :, 128:130], 1.0), (), [b_vaug])
            VB = min(512, AW)
            HPB = VB // 128
            for tc in range(TC):
                for hb_ in range(AW // VB):
                    bk = (tc * (AW // VB) + hb_) % 4
                    for dc in range(DC):
                        mm(banks[bk][:, 0:VB], hT[:, dc, tc * 128:(tc + 1) * 128], wv[:, dc, hb_ * VB:(hb_ + 1) * VB], dc == 0, dc == DC - 1,
                           [b_hT, b_wv], [bbuf[bk]])
                    eng = act if (hb_ % 2 == 0) else dve
                    CP(eng, vaug[:, tc, hb_ * HPB:(hb_ + 1) * HPB, 0:128], banks[bk][:, 0:VB].rearrange("p (h e) -> p h e", h=HPB), [bbuf[bk]], [b_vaug])
            if b == 0:
                dump("vaug", vaug, [128, TC, NH, 130], BF16, [b_vaug])
            K.barrier()

            Ttab = yview(0, 2 * S, F32); b_T = GB("Ttab")
            pool.op(lambda e: e.iota(Ttab, pattern=[[1, 2 * S]], base=-S, channel_multiplier=-1,
                                     allow_small_or_imprecise_dtypes=True), (), [b_T])
            Ttab2 = yview(2 * S, 2 * S, F32)
            pool.op(lambda e: e.iota(Ttab2, pattern=[[-1, 2 * S]], base=S, channel_multiplier=1,
                                     allow_small_or_imprecise_dtypes=True), (), [b_T])
            TT(dve, Ttab, Ttab, Ttab2, ALU.max, [b_T], [b_T])
            wqk = [wview(i * (DC * 128), DC * 128, BF16, "p (c n) -> p c n", c=DC) for i in range(2)]
            b_wqk = [GB("wqk0"), GB("wqk1")]
            qo = 2 * DC * 128
            qT = [wview(qo + i * (S // 2), S // 2, BF16) for i in range(2)]; b_qT = [GB("qT0"), GB("qT1")]
            ko = qo + S
            kT = [wview(ko + i * (S // 2), S // 2, BF16) for i in range(2)]; b_kT = [GB("kT0"), GB("kT1")]
            to = ko + S
            NTB = 4
            tmpb = [wview(to + i * QT, QT, F32) for i in range(NTB)]; b_tmp = [GB("tmp%d" % i) for i in range(NTB)]
            po = to + NTB * QT
            pTb = [wview(po + i * (QT // 2), QT // 2, BF16) for i in range(NTB)]; b_pT = [GB("pT%d" % i) for i in range(NTB)]
            so = po + NTB * (QT // 2)
            NQC = QT // 128
            nab = (2 * NQC + 2) // 3
            accs = [wview(so, nab * 387, F32, "p (k c) -> p k c", k=nab)] * 2
            b_accs = [GB("accs0")] * 2
            so += nab * 387
            a_h = [yview(2 * S + i * (TC * 128), TC * 128, F32, "p (t e) -> p t e", t=TC) for i in range(2)]
            b_ah = [GB("a_h0"), GB("a_h1")]
            ssq = [wview(so + i * 3 * TC, TC, F32) for i in range(2)]
            c2e = [wview(so + i * 3 * TC + TC, TC, F32) for i in range(2)]
            rstd = [wview(so + i * 3 * TC + 2 * TC, TC, F32) for i in range(2)]
            b_st3 = [GB("st3_0"), GB("st3_1")]
            so += 6 * TC
            sm = wview(so, 8, F32); b_sm = GB("sm")
            a_j = [wview(so + 8 + 256 + i * 128, 128, F32) for i in range(2)]; b_aj = [GB("a_j0"), GB("a_j1")]
            a_k = wview(so + 8 + 512, 128, F32); b_ak = GB("a_k")
            yh2 = [wview(so + 8 + 128 + i * 64, 64, BF16) for i in range(2)]; b_yh2 = [GB("yh0"), GB("yh1")]
            assert so + 8 + 640 <= WW, so
            SB_ = [3, 4, 5, 6]

            def emit_proj(h):
                i = h % 2
                LD(pool, wqk[i][:, :, 0:128], w_in[:, h * 128:(h + 1) * 128].rearrange("(c p) n -> p c n", p=128), b_wqk[i])
                LD(pool, wqk[i][:, :, 128:256], w_in[:, AW + h * 128:AW + (h + 1) * 128].rearrange("(c p) n -> p c n", p=128), b_wqk[i])
                for t5 in range(S // QT):
                    for (which, dstT, b_dst) in ((0, qT[i], b_qT[i]), (1, kT[i], b_kT[i])):
                        for dc in range(DC):
                            mm(banks[7][:, 0:QT], wqk[i][:, dc, which * 128:(which + 1) * 128], hT[:, dc, t5 * QT:(t5 + 1) * QT],
                               dc == 0, dc == DC - 1, [b_wqk[i], b_hT], [bbuf[7]])
                        CP(act if which == 0 else dve, dstT[:, t5 * QT:(t5 + 1) * QT], banks[7][:, 0:QT], [bbuf[7]], [b_dst])

            def acc(m, qc):
                a_ = m * NQC + qc
                bk = a_ // 3
                return banks[bk][:, (a_ % 3) * 129:(a_ % 3) * 129 + 129], bbuf[bk], (a_ % 3 == 0)

            def emit_score_pair(h, p, qt, kc):
                i = h % 2
                for m in range(2):
                    sbk = SB_[(2 * p + m) % 4]
                    mm(banks[sbk][:, 0:QT], kT[i][m * 64:(m + 1) * 64, kc * 128:(kc + 1) * 128],
                       qT[i][m * 64:(m + 1) * 64, qt * QT:(qt + 1) * QT], True, True, [b_kT[i], b_qT[i]], [bbuf[sbk]])
                off = qt * QT - kc * 128 + S
                for m in range(2):
                    sbk = SB_[(2 * p + m) % 4]
                    ti = (2 * p + m) % NTB
                    STT(dve, tmpb[ti], Ttab[:, off:off + QT], -8.0 * cfg.slopes[h], banks[sbk][:, 0:QT], ALU.mult, ALU.add,
                        [b_T, bbuf[sbk]], [b_tmp[ti]])
                    A(pTb[ti], tmpb[ti], AF.Exp, [b_tmp[ti]], [b_pT[ti]], scale=0.125)

            def emit_av_pair(h, p, qt, kc):
                for m in range(2):
                    ti = (2 * p + m) % NTB
                    for qc in range(NQC):
                        a_ap, a_b, first = acc(m, qc)
                        pe.op((lambda a_ap=a_ap, ti=ti, qc=qc, kc=kc, h=h, first=first: (lambda e: e.matmul(
                            a_ap, lhsT=pTb[ti][:, qc * 128:(qc + 1) * 128], rhs=vaug[:, kc, h, 0:129],
                            start=(kc == 0 and first), stop=(kc == TC - 1), skip_group_check=True)))(),
                            [b_pT[ti], b_vaug], [a_b], sig=(qc == NQC - 1))
                if kc == TC - 1:
                    emit_post(h, qt)

            def emit_post(h, qt):
                i = h % 2
                par = qt % 2
                for bk in range(nab):
                    ncol = min(3, 2 * NQC - 3 * bk) * 129
                    CP(dve, accs[par][:, bk, 0:ncol], banks[bk][:, 0:ncol], [bbuf[bk]], [b_accs[par]])
                for qc in range(NQC):
                    slot = qt * NQC + qc
                    a0_, a1_ = qc, NQC + qc
                    c0 = (a0_ % 3) * 129; c1 = (a1_ % 3) * 129
                    o0 = accs[par][:, a0_ // 3, c0:c0 + 128]; l0 = accs[par][:, a0_ // 3, c0 + 128:c0 + 129]
                    o1 = accs[par][:, a1_ // 3, c1:c1 + 128]; l1 = accs[par][:, a1_ // 3, c1 + 128:c1 + 129]
                    at = a_h[i][:, slot, :]
                    TS(pool, at, o0, l1, 0.0, ALU.mult, ALU.add, [b_accs[par]], [b_ah[i]])
                    TT(pool, sm[:, 0:1], l0, lam_t[:, 5:6], ALU.mult, [b_accs[par], b_lam], [b_sm])
                    TS(pool, a_k, o1, sm[:, 0:1], 0.0, ALU.mult, ALU.add, [b_accs[par], b_sm], [b_ak])
                    TT(pool, at, at, a_k, ALU.add, [b_ak, b_ah[i]], [b_ah[i]])
                    TS(pool, sm[:, 1:2], l0, l1, 0.0, ALU.mult, ALU.add, [b_accs[par]], [b_sm])
                    TS(pool, c2e[i][:, slot:slot + 1], sm[:, 1:2], sm[:, 1:2], EPS, ALU.mult, ALU.mult, [b_sm], [b_st3[i]])

            def emit_norm(h):
                i = h % 2
                for slot in range(TC):
                    j = slot % 2
                    TT(pool, a_j[j], a_h[i][:, slot, :], a_h[i][:, slot, :], ALU.mult, [b_ah[i]], [b_aj[j]])
                    dve.op((lambda i=i, slot=slot, j=j: (lambda e: e.tensor_reduce(out=ssq[i][:, slot:slot + 1], in_=a_j[j], axis=AX.X, op=ALU.add)))(),
                           [b_aj[j]], [b_st3[i]])
                TS(pool, ssq[i], ssq[i], 1.0 / 128, 0.0, ALU.mult, ALU.add, [b_st3[i]], [b_st3[i]])
                TT(pool, ssq[i], ssq[i], c2e[i], ALU.add, [b_st3[i]], [b_st3[i]])
                A(c2e[i], ssq[i], AF.Sqrt, [b_st3[i]], [b_st3[i]])
                dve.op((lambda i=i: (lambda e: e.reciprocal(out=rstd[i], in_=c2e[i])))(), [b_st3[i]], [b_st3[i]])
                ptb = banks[7][:].bitcast(BF16)
                for slot in range(TC):
                    j = slot % 2
                    TS(pool, a_k, a_h[i][:, slot, :], rstd[i][:, slot:slot + 1], 0.0, ALU.mult, ALU.add, [b_ah[i], b_st3[i]], [b_ak])
                    TT(pool, yh2[j], a_k, subg[:], ALU.mult, [b_ak, b_subg], [b_yh2[j]])
                    tr(ptb[:, 0:128], yh2[j], identb[:], [b_yh2[j], b_identb], [bbuf[7]])
                    CP(dve, yaT[:, h, slot * 128:(slot + 1) * 128], ptb[:, 0:128], [bbuf[7]], [b_yaT])

            pairs = [(qt, kc) for qt in range(S // QT) for kc in range(TC)]
            npair = len(pairs)
            emit_proj(0)
            for h in range(NH):
                for p in range(npair + 1):
                    if p < npair:
                        emit_score_pair(h, p, *pairs[p])
                    if p == min(12, npair - 1) and h >= 1:
                        emit_norm(h - 1)
                    if p == npair // 2 and h + 1 < NH:
                        emit_proj(h + 1)
                    if p >= 1:
                        emit_av_pair(h, p - 1, *pairs[p - 1])
            emit_norm(NH - 1)
            if b == 0:
                dump("yaT", yaT, [128, NH, S], BF16, [b_yaT])
            K.barrier()

            NQC_ = QT // 128
            wu = wview(0, DC * D // 2, BF16, "p (c n) -> p c n", c=DC); b_wu = GB("wu")
            wsw = wview(DC * D // 2, DC * D // 2, BF16, "p (c n) -> p c n", c=DC); b_wsw = GB("wsw")
            LD(pool, wu, w_in[:, 3 * AW:3 * AW + D].rearrange("(c p) n -> p c n", p=128), b_wu)
            LD(pool, wsw, w_in[:, 3 * AW + D:3 * AW + 2 * D].rearrange("(c p) n -> p c n", p=128), b_wsw)
            o2 = DC * D
            uTt = wview(o2, G * QT // 2, BF16, "p (g t) -> p g t", g=G); b_uTt = GB("uTt")
            sfulls = [wview(o2 + G * QT // 2 + i * D, D, F32) for i in range(2)]; b_sfulls = [GB("sfull0"), GB("sfull1")]
            lnG = yview(0, D, F32); b_lnG = GB("lnG")
            lnB = yview(D, D, F32); b_lnB = GB("lnB")
            bsbc = yview(2 * D, G * 128, F32); b_bsbc = GB("bsbc")
            tmp2 = yview(2 * D + G * 128, G * 128, F32); b_tmp2 = GB("tmp2")
            g1s = [yview(2 * D + 2 * G * 128 + i * 512, 512, F32) for i in range(2)]; b_g1s = [GB("g1_0"), GB("g1_1")]
            g2s = [yview(YW - 1024 + i * 512, 512, F32) for i in range(2)]; b_g2s = [GB("g2_0"), GB("g2_1")]
            gcnt = [0]
            wsT = yview(2 * D + 2 * G * 128 + 1024, G * 64, BF16, "p (g t) -> p g t", g=G); b_wsT = GB("wsT")
            st4s = [yview(2 * D + 2 * G * 128 + 1024 + G * 64 + 136 + i * 8, 8, F32) for i in range(2)]; b_st4s = [GB("st4_0"), GB("st4_1")]
            vss = [yview(2 * D + 2 * G * 128 + 1024 + G * 64 + 160 + i * (D // 2), D // 2, BF16) for i in range(2)]; b_vss = [GB("vs0"), GB("vs1")]
            assert 2 * D + 2 * G * 128 + 1024 + G * 64 + 160 + D <= YW - 1024
            wst = yview(2 * D + 2 * G * 128 + 1024 + G * 64 + 8, 128, F32); b_wst = GB("wst")
            LD(sp, lnG, ln_g.broadcast_to([128, D]), b_lnG)
            LD(sp, lnB, ln_b.broadcast_to([128, D]), b_lnB)
            LD(sp, bsbc, b_s_d.broadcast_to([128, G * 128]), b_bsbc)
            for g in range(G):
                LD(sp, wst, w_s_d[g], b_wst)
                tr(banks[0][:, 0:128], wst, ident[:], [b_wst, b_ident], [bbuf[0]])
                CP(dve, wsT[:, g, :], banks[0][:, 0:128], [bbuf[0]], [b_wsT])

            C_G = 0.044715 ** 0.5

            def gelu_A(gi, src, n, rb):
                A(g1s[gi][:, 0:n], src, AF.Square, rb, [b_g1s[gi]], scale=C_G)
                STT(dve, g1s[gi][:, 0:n], g1s[gi][:, 0:n], 1.0, src, ALU.add, ALU.mult, [b_g1s[gi]] + rb, [b_g1s[gi]])

            def gelu_B(gi, dst, src, n, rb, wb):
                A(g2s[gi][:, 0:n], g1s[gi][:, 0:n], AF.Sigmoid, [b_g1s[gi]], [b_g2s[gi]], scale=1.5957691216057308)
                TT(dve, dst, g2s[gi][:, 0:n], src, ALU.mult, [b_g2s[gi]] + rb, wb)

            for t5 in range(S // QT):
                def uA(g):
                    bk = g % 2
                    for dc in range(DC):
                        mm(banks[bk][:, 0:QT], wu[:, dc, g * 128:(g + 1) * 128], hT[:, dc, t5 * QT:(t5 + 1) * QT], dc == 0, dc == DC - 1,
                           [b_wu, b_hT], [bbuf[bk]])
                    gelu_A(g % 2, banks[bk][:, 0:QT], QT, [bbuf[bk]])
                uA(0)
                for g in range(G):
                    if g + 1 < G:
                        uA(g + 1)
                    gelu_B(g % 2, uTt[:, g, :], banks[g % 2][:, 0:QT], QT, [bbuf[g % 2]], [b_uTt])
                NHB = D // DB

                def stA(tcl):
                    tc = t5 * NQC_ + tcl
                    sfull, b_sfull = sfulls[tc % 2], b_sfulls[tc % 2]

                    def sA(hb_):
                        bk = 2 + hb_ % 2
                        for dc in range(DC):
                            mm(banks[bk][:, 0:DB], hT[:, dc, tc * 128:(tc + 1) * 128], wsw[:, dc, hb_ * DB:(hb_ + 1) * DB], dc == 0, dc == DC - 1,
                               [b_hT, b_wsw], [bbuf[bk]])
                        gelu_A(hb_ % 2, banks[bk][:, 0:DB], DB, [bbuf[bk]])
                    sA(0)
                    for hb_ in range(NHB):
                        if hb_ + 1 < NHB:
                            sA(hb_ + 1)
                        gelu_B(hb_ % 2, sfull[:, hb_ * DB:(hb_ + 1) * DB], banks[2 + hb_ % 2][:, 0:DB], DB, [bbuf[2 + hb_ % 2]], [b_sfull])

                def stB(tcl):
                    tc = t5 * NQC_ + tcl
                    sfull, b_sfull = sfulls[tc % 2], b_sfulls[tc % 2]
                    vs, b_vs = vss[tc % 2], b_vss[tc % 2]
                    st4, b_st4 = st4s[tc % 2], b_st4s[tc % 2]
                    dve.op((lambda st4=st4, sfull=sfull: (lambda e: e.tensor_reduce(out=st4[:, 0:1], in_=sfull, axis=AX.X, op=ALU.add)))(), [b_sfull], [b_st4])
                    TS(dve, st4[:, 1:2], st4[:, 0:1], -1.0 / D, None, ALU.mult, None, [b_st4], [b_st4])
                    TS(dve, sfull, sfull, st4[:, 1:2], None, ALU.add, None, [b_sfull, b_st4], [b_sfull])
                    for hb_ in range(NHB):
                        act.op((lambda hb_=hb_, sfull=sfull, st4=st4: (lambda e: e.activation(
                            out=banks[6 + hb_][:, 0:DB], in_=sfull[:, hb_ * DB:(hb_ + 1) * DB], func=AF.Square, accum_out=st4[:, 5 + hb_:6 + hb_])))(),
                            [b_sfull], [bbuf[6 + hb_], b_st4])
                    if NHB == 2:
                        TT(dve, st4[:, 2:3], st4[:, 5:6], st4[:, 6:7], ALU.add, [b_st4], [b_st4])
                    else:
                        CP(dve, st4[:, 2:3], st4[:, 5:6], [b_st4], [b_st4])
                    rsqrt_col(st4[:, 4:5], st4[:, 2:3], 1.0 / D, st4[:, 3:4], [b_st4, b_eps], [b_st4])
                    STT(dve, sfull, sfull, st4[:, 4:5], lnG, ALU.mult, ALU.mult, [b_sfull, b_st4, b_lnG], [b_sfull])
                    TT(pool, vs, sfull, lnB, ALU.add, [b_sfull, b_lnB], [b_vs])

                def stC(tcl):
                    tc = t5 * NQC_ + tcl
                    vs, b_vs = vss[tc % 2], b_vss[tc % 2]
                    for g in range(G):
                        bk = 4 + g // 4
                        pe.op((lambda bk=bk, g=g, vs=vs: (lambda e: e.matmul(banks[bk][:, (g % 4) * 128:(g % 4) * 128 + 128], lhsT=vs[:, g * 128:(g + 1) * 128],
                                                                             rhs=wsT[:, g, :], start=True, stop=True, skip_group_check=True)))(),
                              [b_vs, b_wsT], [bbuf[bk]])
                    for gb in range((G + 3) // 4):
                        ng = min(4, G - gb * 4)
                        TT(dve, tmp2[:, gb * 512:gb * 512 + ng * 128], banks[4 + gb][:, 0:ng * 128], bsbc[:, gb * 512:gb * 512 + ng * 128], ALU.add,
                           [bbuf[4 + gb], b_bsbc], [b_tmp2])
                    TT(pool, zT[:, :, tc * 128:(tc + 1) * 128], tmp2.rearrange("p (g t) -> p g t", g=G), uTt[:, :, tcl * 128:(tcl + 1) * 128], ALU.mult,
                       [b_tmp2, b_uTt], [b_zT])

                for step in range(NQC_ + 2):
                    if step < NQC_:
                        stA(step)
                    if 1 <= step < NQC_ + 1:
                        stB(step - 1)
                    if step >= 2:
                        stC(step - 2)
            if b == 0:
                dump("zT", zT, [128, G, S], BF16, [b_zT])
            K.barrier()

            CW = DC * 64
            wsl = [[wview(i * 4 * CW + k * CW, CW, BF16, "p (c n) -> p c n", c=DC) for k in range(4)] for i in range(2)]
            b_wsl = [GB("wsl0"), GB("wsl1")]
            so5 = 8 * CW
            s12 = [[wview(so5 + (i * 2 + k) * QT, QT, F32) for k in range(2)] for i in range(2)]
            b_s12 = [[GB("s12_%d%d" % (i, k)) for k in range(2)] for i in range(2)]
            it = 0

            def load_s5(j):
                i = j % 2
                srcs = (w_ap[:, j * 128:(j + 1) * 128], w_in[:, 3 * AW + 2 * D + j * 128:3 * AW + 2 * D + (j + 1) * 128],
                        w_sp[:, j * 128:(j + 1) * 128], w_in[:, 3 * AW + 3 * D + j * 128:3 * AW + 3 * D + (j + 1) * 128])
                for k in range(4):
                    LD(pool, wsl[i][k], srcs[k].rearrange("(c p) n -> p c n", p=128), b_wsl[i])
            load_s5(0)
            for j in range(DC):
                i = j % 2
                if j + 1 < DC:
                    load_s5(j + 1)
                for t5 in range(S // QT):
                    p = (it % 2) * 4
                    ii = it % 2
                    it += 1
                    tok = slice(t5 * QT, (t5 + 1) * QT)
                    opnds = ((yaT, b_yaT, NH), (hT, b_hT, DC), (zT, b_zT, G), (hT, b_hT, DC))
                    for k in range(4):
                        src, b_src, nk = opnds[k]
                        for kc in range(nk):
                            mm(banks[p + k][:, 0:QT], wsl[i][k][:, kc, :], src[:, kc, tok], kc == 0, kc == nk - 1, [b_wsl[i], b_src], [bbuf[p + k]])
                    A(s12[ii][0], banks[p + 1][:, 0:QT], AF.Sigmoid, [bbuf[p + 1]], [b_s12[ii][0]])
                    A(s12[ii][1], banks[p + 3][:, 0:QT], AF.Sigmoid, [bbuf[p + 3]], [b_s12[ii][1]])
                    TT(dve, s12[ii][0], s12[ii][0], banks[p + 0][:, 0:QT], ALU.mult, [b_s12[ii][0], bbuf[p + 0]], [b_s12[ii][0]])
                    TT(dve, s12[ii][1], s12[ii][1], banks[p + 2][:, 0:QT], ALU.mult, [b_s12[ii][1], bbuf[p + 2]], [b_s12[ii][1]])
                    TT(pool, yT[:, j, tok], s12[ii][0], s12[ii][1], ALU.add, [b_s12[ii][0], b_s12[ii][1]], [b_yT])
            if b == 0:
                dump("yT", yT, [128, DC, S], BF16, [b_yT])
            K.barrier()

            bcast_mod(b, 2, bcA, b_bcA)
            wo = wview(0, DC * D // 2, BF16, "p (c n) -> p c n", c=DC); b_wo = GB("wo")
            LD(pool, wo, w_out.rearrange("(c p) n -> p c n", p=128), b_wo)
            xt6 = [wview(DC * D // 2 + i * D, D, F32) for i in range(2)]; b_xt6 = [GB("xt6_0"), GB("xt6_1")]
            tm6 = wview(DC * D // 2 + 2 * D, D, F32); b_tm6 = GB("tm6")
            for tc in range(TC):
                i = tc % 2
                LD(sp, xt6[i], x_d[b, tc * 128:(tc + 1) * 128, :], b_xt6[i])
                for hb_ in range(D // DB):
                    bk = (tc % 2) * 2 + hb_ % 2
                    for kc in range(DC):
                        mm(banks[bk][:, 0:DB], yT[:, kc, tc * 128:(tc + 1) * 128], wo[:, kc, hb_ * DB:(hb_ + 1) * DB], kc == 0, kc == DC - 1,
                           [b_yT, b_wo], [bbuf[bk]])
                    TT(dve, tm6[:, hb_ * DB:(hb_ + 1) * DB], banks[bk][:, 0:DB], bcA[:, hb_ * DB:(hb_ + 1) * DB], ALU.mult, [bbuf[bk], b_bcA], [b_tm6])
                TT(pool, x1[:, tc, :], tm6, xt6[i], ALU.add, [b_tm6, b_xt6[i]], [b_x1[tc]])
            if b == 0:
                dump("x1", x1, [128, TC, D], F32, b_x1)
            K.barrier()

            bcast_mod(b, 4, bcA, b_bcA)
            bcast_mod(b, 3, bcB, b_bcB)
            norm_to_hT(b, lambda tc, xt_i, b_xt_i: (x1[:, tc, :], b_x1[tc]), None, "n2")
            if b == 0:
                dump("h2T", hT, [128, DC, S], BF16, [b_hT])
            K.barrier()

            lg = wview(0, NR + 4, F32)[:, 0:NR]; b_lg = GB("lg")
            r8 = wview(64, 16, F32); b_r8 = GB("r8")
            gm = wview(96, NG, F32); b_gm = GB("gm")
            els = wview(128, EPG, F32); b_els = GB("els")
            m8 = wview(160, 8, F32); b_m8 = GB("m8")
            cws = wview(192, EPG, F32); b_cws = GB("cws")
            cws2 = wview(224, EPG, F32)
            cw = wview(256, NE, F32); b_cw = GB("cw")
            cwT = [wview(512 + i * 128, 128, F32) for i in range(2)]; b_cwT = [GB("cwT0"), GB("cwT1")]
            for tc in range(TC):
                bk = tc % 2
                for dc in range(DC):
                    mm(banks[bk][:, 0:NR], hT[:, dc, tc * 128:(tc + 1) * 128], wrt[:, dc, :], dc == 0, dc == DC - 1, [b_hT, b_wrt], [bbuf[bk]])
                TT(dve, lg, banks[bk][:, 0:NR], brt[:], ALU.add, [bbuf[bk], b_brt], [b_lg])
                dve.op(lambda e: e.tensor_reduce(out=r8[:, 0:1], in_=lg[:, 0:NG], axis=AX.X, op=ALU.max), [b_lg], [b_r8])
                TS(dve, gm, lg[:, 0:NG], r8[:, 0:1], None, ALU.is_equal, None, [b_lg, b_r8], [b_gm])
                TS(dve, r8[:, 1:2], r8[:, 0:1], -1.0, None, ALU.mult, None, [b_r8], [b_r8])
                A(cws2[:, 0:NG], lg[:, 0:NG], AF.Exp, [b_lg, b_r8], [b_cws, b_r8], bias=r8[:, 1:2], scale=1.0, accum_out=r8[:, 2:3])
                dve.op(lambda e: e.reciprocal(out=r8[:, 3:4], in_=r8[:, 2:3]), [b_r8], [b_r8])
                TS(dve, els, lg[:, NG:NG + EPG], gm[:, 0:1], None, ALU.mult, None, [b_lg, b_gm], [b_els])
                for g in range(1, NG):
                    STT(dve, els, lg[:, NG + g * EPG:NG + (g + 1) * EPG], gm[:, g:g + 1], els, ALU.mult, ALU.add, [b_lg, b_gm, b_els], [b_els])
                dve.op(lambda e: e.max(out=m8, in_=els), [b_els], [b_m8])
                TT(dve, r8[:, 4:5], m8[:, 1:2], m8[:, 0:1], ALU.subtract, [b_m8], [b_r8])
                A(r8[:, 5:6], r8[:, 4:5], AF.Exp, [b_r8], [b_r8])
                TS(dve, r8[:, 6:7], r8[:, 5:6], 1.0, None, ALU.add, None, [b_r8], [b_r8])
                dve.op(lambda e: e.reciprocal(out=r8[:, 7:8], in_=r8[:, 6:7]), [b_r8], [b_r8])
                TT(dve, r8[:, 8:9], r8[:, 7:8], r8[:, 3:4], ALU.mult, [b_r8], [b_r8])
                TT(dve, r8[:, 9:10], r8[:, 3:4], r8[:, 8:9], ALU.subtract, [b_r8], [b_r8])
                TS(dve, cws, els, m8[:, 0:1], r8[:, 8:9], ALU.is_equal, ALU.mult, [b_els, b_m8, b_r8], [b_cws])
                TS(dve, cws2, els, m8[:, 1:2], r8[:, 9:10], ALU.is_equal, ALU.mult, [b_els, b_m8, b_r8, b_cws], [b_cws])
                TT(dve, cws, cws, cws2, ALU.add, [b_cws], [b_cws])
                for g in range(NG):
                    TS(dve, cw[:, g * EPG:(g + 1) * EPG], cws, gm[:, g:g + 1], None, ALU.mult, None, [b_cws, b_gm], [b_cw])
                tr(banks[2 + bk][0:NE, 0:128], cw, ident[:], [b_cw, b_ident], [bbuf[2 + bk]])
                CP(dve, cwT[bk][0:NE, :], banks[2 + bk][0:NE, 0:128], [bbuf[2 + bk]], [b_cwT[bk]])
                sp.dma((lambda bk=bk, tc=tc: (lambda e: e.dma_start(out=cw_scr[:, tc * 128:(tc + 1) * 128], in_=cwT[bk][0:NE, :])))(),
                       b_cwT[bk], [b_cwT[bk]], [b_cwscr])
            if b == 0:
                dump("cw", cw_scr, [NE, S], F32, [b_cwscr])
            K.barrier()

            bcast_mod(b, 5, bcA, b_bcA)
            MT = min(256, S)
            NMT = S // MT
            MC = MT // 128
            GUW = DC * DE
            DNW = FC * D // 2
            EW = GUW + DNW
            wslot = []
            for si in range(4):
                base_ = (o_y + si * EW) if si < 2 else (o_w + (si - 2) * EW)
                wslot.append((view(base_, GUW, BF16, "p (c n) -> p c n", c=DC), view(base_ + GUW, DNW, BF16, "p (f d) -> p f d", f=FC)))
            assert 2 * EW <= YW
            b_wslot = [GB("wslot%d" % si) for si in range(4)]
            o9 = 2 * EW
            cwbc = [[wview(o9 + (i * 2 + k) * MT, MT, F32) for k in range(2)] for i in range(2)]
            b_cwbc = [[GB("cwbc%d%d" % (i, k)) for k in range(2)] for i in range(2)]
            o9 += 4 * MT
            sg9 = [wview(o9 + i * FC * MT, FC * MT, F32) for i in range(2)]; b_sg9 = [GB("sg9_0"), GB("sg9_1")]
            o9 += 2 * FC * MT
            tm9 = [wview(o9 + i * 512, 512, F32) for i in range(2)]; b_tm9 = [GB("tm9_0"), GB("tm9_1")]
            o9 += 1024
            assert FC * MT <= 512
            actp2 = [[wview(o9 + (i * 2 + k) * (FC * MT // 2), FC * MT // 2, BF16, "p (f t) -> p f t", f=FC) for k in range(2)] for i in range(2)]
            b_actp2 = [[GB("actp%d%d" % (i, k)) for k in range(2)] for i in range(2)]
            o9 += 2 * FC * MT
            assert o9 <= WW, o9
            ycnt = [0]

            def load_pair(ep):
                for e_ in range(2):
                    e = ep * 2 + e_
                    si = e % 4
                    LD(pool, wslot[si][0], w_gu[e].rearrange("(c p) n -> p c n", p=128), b_wslot[si])
                    LD(pool, wslot[si][1], w_dn[e].rearrange("(f p) d -> p f d", p=128), b_wslot[si])
                    for f in range(FC):
                        TT(pool, wslot[si][1][:, f, :], wslot[si][1][:, f, :], bcA[:], ALU.mult, [b_wslot[si], b_bcA], [b_wslot[si]])

            def emit_gu(k, ep, mt):
                tok = slice(mt * MT, (mt + 1) * MT)
                for e_ in range(2):
                    e = ep * 2 + e_
                    si = e % 4
                    wg = wslot[si][0]
                    LD(sp, cwbc[e_][mt % 2], cw_scr[e:e + 1, tok].broadcast_to([128, MT]), b_cwbc[e_][mt % 2], reads=[b_cwscr])
                    pb = e_ * 2
                    for n in range(2 * FC):
                        bk = pb + n // FC
                        col = (n % FC) * MT
                        for dc in range(DC):
                            pe.op((lambda bk=bk, col=col, wg=wg, dc=dc, n=n, tok=tok: (lambda e__: e__.matmul(
                                banks[bk][:, col:col + MT], lhsT=wg[:, dc, n * 128:(n + 1) * 128], rhs=hT[:, dc, tok],
                                start=(dc == 0), stop=(dc == DC - 1), skip_group_check=True)))(),
                                [b_wslot[si], b_hT], [bbuf[bk]], sig=(dc == DC - 1))
                    gps = banks[pb][:, 0:FC * MT]
                    ups = banks[pb + 1][:, 0:FC * MT]
                    A(sg9[e_], gps, AF.Sigmoid, [bbuf[pb]], [b_sg9[e_]])
                    TT(dve, sg9[e_], sg9[e_], gps, ALU.mult, [b_sg9[e_], bbuf[pb]], [b_sg9[e_]])
                    TT(dve, sg9[e_], sg9[e_], ups, ALU.mult, [b_sg9[e_], bbuf[pb + 1]], [b_sg9[e_]])
                    for f in range(FC):
                        TT(pool, actp2[e_][k % 2][:, f, :], sg9[e_][:, f * MT:(f + 1) * MT], cwbc[e_][mt % 2], ALU.mult,
                           [b_sg9[e_], b_cwbc[e_][mt % 2]], [b_actp2[e_][k % 2]])

            def emit_down(k, ep, mt):
                for mc in range(MC):
                    tc = mt * MC + mc
                    for hb_ in range(D // DB):
                        bk = 4 + ycnt[0] % 4
                        ti = ycnt[0] % 2
                        ycnt[0] += 1
                        nmm = 0
                        for e_ in range(2):
                            wd_ = wslot[(ep * 2 + e_) % 4][1]
                            for f in range(FC):
                                mm(banks[bk][:, 0:DB], actp2[e_][k % 2][:, f, mc * 128:(mc + 1) * 128], wd_[:, f, hb_ * DB:(hb_ + 1) * DB],
                                   nmm == 0, nmm == 2 * FC - 1, [b_actp2[e_][k % 2], b_wslot[(ep * 2 + e_) % 4]], [bbuf[bk]])
                                nmm += 1
                        TT(dve, x1[:, tc, hb_ * DB:(hb_ + 1) * DB], banks[bk][:, 0:DB], x1[:, tc, hb_ * DB:(hb_ + 1) * DB], ALU.add,
                           [bbuf[bk], b_x1[tc]], [b_x1[tc]])

            items = [(ep, mt) for ep in range(NE // 2) for mt in range(NMT)]
            load_pair(0)
            for k, (ep, mt) in enumerate(items):
                emit_gu(k, ep, mt)
                if k >= 1:
                    emit_down(k - 1, *items[k - 1])
                if mt == 0 and ep + 1 < NE // 2:
                    load_pair(ep + 1)
            emit_down(len(items) - 1, *items[-1])
            if b == 0:
                dump("x2", x1, [128, TC, D], F32, b_x1)
            K.barrier()

            ot = [wview(i * D, D, F32) for i in range(2)]; b_ot = [GB("ot0"), GB("ot1")]
            jk = wview(2 * D, D, F32); b_jk = GB("jk10")
            st10 = wview(3 * D, 3 * TC, F32); b_st10 = GB("st10")
            for tc in range(TC):
                act.op((lambda tc=tc: (lambda e: e.activation(out=jk, in_=x1[:, tc, :], func=AF.Square, accum_out=st10[:, tc:tc + 1])))(),
                       [b_x1[tc]], [b_jk, b_st10])
            rsqrt_col(st10[:, 2 * TC:3 * TC], st10[:, 0:TC], 1.0 / D, st10[:, TC:2 * TC], [b_st10, b_eps], [b_st10])
            for tc in range(TC):
                i = tc % 2
                STT(dve, ot[i], x1[:, tc, :], st10[:, 2 * TC + tc:2 * TC + tc + 1], fgbc[:], ALU.mult, ALU.mult, [b_x1[tc], b_st10, b_fgbc], [b_ot[i]])
                sp.dma((lambda i=i, tc=tc, b=b: (lambda e: e.dma_start(out=y_d[b, tc * 128:(tc + 1) * 128, :], in_=ot[i])))(), b_ot[i], [b_ot[i]], [])
            K.barrier()
        K.barrier()
        K.emit()
    return nc


_NC_CACHE = {}


def _prep_shared(inp, cfg):
    f = lambda a: np.ascontiguousarray(np.asarray(a, dtype=np.float32))
    L = 0
    sh = {
        "w_ada": f(inp["w_ada"][L]), "b_ada": f(inp["b_ada"][L]).reshape(1, -1), "norm1_g": f(inp["norm1_g"][L]).reshape(1, -1),
        "w_in": f(inp["w_in"][L]),
        "lam4": f(np.stack([np.asarray(inp["lambda_q1"][L]), np.asarray(inp["lambda_k1"][L]),
                            np.asarray(inp["lambda_q2"][L]), np.asarray(inp["lambda_k2"][L])], axis=0)),
        "subln_g": f(inp["subln_g"][L]).reshape(1, -1), "w_attn_proj": f(inp["w_attn_proj"][L]),
        "sgu_ln_g": f(inp["sgu_ln_g"][L]).reshape(1, -1), "sgu_ln_b": f(inp["sgu_ln_b"][L]).reshape(1, -1),
        "sgu_w_s": f(inp["sgu_w_s"][L]), "sgu_b_s": f(inp["sgu_b_s"][L]).reshape(1, -1),
        "w_sgu_proj": f(inp["w_sgu_proj"][L]), "w_out": f(inp["w_out"][L]), "norm2_g": f(inp["norm2_g"][L]).reshape(1, -1),
        "w_router": f(np.concatenate([np.asarray(inp["w_router_group"][L]), np.asarray(inp["w_router_expert"][L])], axis=1)),
        "b_router": f(np.concatenate([np.asarray(inp["b_router_group"][L]), np.asarray(inp["b_router_expert"][L])], axis=0)).reshape(1, -1),
        "w_expert_gate_up": f(inp["w_expert_gate_up"][L]), "w_expert_down": f(inp["w_expert_down"][L]),
        "final_g": f(inp["final_g"]).reshape(1, -1),
    }
    return sh


def kernel(**inputs):
    cfg = Cfg()
    n_cores = 8
    x = np.asarray(inputs["x"], dtype=np.float32)
    c = np.asarray(inputs["c"], dtype=np.float32)
    sh = _prep_shared(inputs, cfg)
    if "nc" not in _NC_CACHE:
        _NC_CACHE["nc"] = build(cfg)
    nc = _NC_CACHE["nc"]
    in_maps = []
    for i in range(n_cores):
        m = dict(sh)
        m["x"] = np.ascontiguousarray(x[i * cfg.NB:(i + 1) * cfg.NB])
        m["c"] = np.ascontiguousarray(c[i * cfg.NB:(i + 1) * cfg.NB])
        in_maps.append(m)
    res = run_bass_kernel_spmd(nc, in_maps, core_ids=list(range(n_cores)))
    return np.concatenate([r["y"] for r in res.results], axis=0).astype(np.float32)
```

```python
import math
from contextlib import ExitStack
import numpy as np
import concourse.bass as bass
import concourse.mybir as mybir
from concourse.bass_utils import run_bass_kernel_spmd

F32 = mybir.dt.float32
BF16 = mybir.dt.bfloat16
AF = mybir.ActivationFunctionType
ALU = mybir.AluOpType
AX = mybir.AxisListType
EPS = 1e-6


class Buf:
    __slots__ = ("name", "w", "r", "dsem", "dcnt")

    def __init__(self, name):
        self.name = name
        self.w = None
        self.r = {}
        self.dsem = None
        self.dcnt = 0


class Eng:
    def __init__(self, K, name, sem, self_raw=True):
        self.K = K
        self.name = name
        self.sem = sem
        self.cnt = 0
        self.known = {}
        self.self_raw = self_raw
        self.prog = []

    def wait(self, ev):
        if ev is None:
            return
        sem, val = ev
        if self.known.get(id(sem), 0) >= val:
            return
        self.prog.append(("w", sem, val))
        self.known[id(sem)] = val

    def _deps(self, reads, writes):
        for b in reads:
            if b.w is not None:
                if b.w[0] is self.sem and not self.self_raw:
                    continue
                self.wait(b.w)
        for b in writes:
            if b.w is not None and (b.w[0] is not self.sem or self.self_raw):
                self.wait(b.w)
            for sem, v in b.r.values():
                if sem is not self.sem or self.self_raw:
                    self.wait((sem, v))

    def op(self, fn, reads=(), writes=(), sig=True):
        self._deps(reads, writes)
        if sig:
            self.cnt += 1
            self.prog.append(("o", fn, self.sem, 1))
            ev = (self.sem, self.cnt)
        else:
            self.prog.append(("o", fn, None, 0))
            ev = (self.sem, self.cnt + 1)
        for b in reads:
            b.r[id(self.sem)] = ev
        for b in writes:
            b.w = ev
            b.r = {}
        return ev

    def dma(self, fn, anchor, reads=(), writes=()):
        self._deps(reads, writes)
        if anchor.dsem is None:
            anchor.dsem = self.K.new_sem("d_" + anchor.name)
            self.K.dma_bufs.append(anchor)
        anchor.dcnt += 16
        self.prog.append(("o", fn, anchor.dsem, 16))
        ev = (anchor.dsem, anchor.dcnt)
        for b in reads:
            b.r[id(anchor.dsem)] = ev
        for b in writes:
            b.w = ev
            b.r = {}
        return ev

    def replay(self, eng):
        for it in self.prog:
            if it[0] == "w":
                eng.wait_ge(it[1], it[2])
            else:
                ins = it[1](eng)
                if it[2] is not None:
                    ins.then_inc(it[2], it[3])


class Kern:
    def __init__(self, nc, stack):
        self.nc = nc
        self.stack = stack
        self.dma_bufs = []
        self.pe = Eng(self, "pe", self.new_sem("s_pe"), self_raw=False)
        self.act = Eng(self, "act", self.new_sem("s_act"))
        self.dve = Eng(self, "dve", self.new_sem("s_dve"))
        self.pool = Eng(self, "pool", self.new_sem("s_pool"))
        self.sp = Eng(self, "sp", self.new_sem("s_sp"))
        self.engs = [self.pe, self.act, self.dve, self.pool, self.sp]

    def new_sem(self, name):
        return self.stack.enter_context(self.nc.semaphore(name))

    def sb(self, name, shape, dt):
        return self.stack.enter_context(self.nc.sbuf_tensor(name, shape, dt))

    def ps(self, name, shape, dt):
        return self.stack.enter_context(self.nc.psum_tensor(name, shape, dt))

    def barrier(self, engs=None):
        evs = [(e.sem, e.cnt) for e in self.engs if e.cnt > 0]
        evs += [(b.dsem, b.dcnt) for b in self.dma_bufs]
        for e in (engs or self.engs):
            for ev in evs:
                if ev[0] is not e.sem:
                    e.wait(ev)

    def emit(self):
        with self.nc.Block() as block:
            block.tensor(lambda e: self.pe.replay(e))
            block.scalar(lambda e: self.act.replay(e))
            block.vector(lambda e: self.dve.replay(e))
            block.gpsimd(lambda e: self.pool.replay(e))
            block.sync(lambda e: self.sp.replay(e))


class Cfg:
    def __init__(self, D=1024, S=2048, NB=2, NH=8, NG=4, EPG=8, DE=256, depth_l=0):
        self.D, self.S, self.NB, self.NH, self.NG, self.EPG, self.DE = D, S, NB, NH, NG, EPG, DE
        self.DC = D // 128
        self.TC = S // 128
        self.AW = NH * 128
        self.G = D // 128
        self.NE = NG * EPG
        self.FC = DE // 128
        self.INW = 3 * self.AW + 4 * D
        self.QT = min(512, S)
        self.lam_init = 0.8 - 0.6 * math.exp(-0.3 * depth_l)
        self.slopes = [2.0 ** (-8.0 * (h + 1) / NH) for h in range(NH)]


def build(cfg, stop_after=None):
    DB = min(512, cfg.D)
    D, S, NB, NH, NG, EPG, DE = cfg.D, cfg.S, cfg.NB, cfg.NH, cfg.NG, cfg.EPG, cfg.DE
    DC, TC, AW, G, NE, FC, INW, QT = cfg.DC, cfg.TC, cfg.AW, cfg.G, cfg.NE, cfg.FC, cfg.INW, cfg.QT
    NR = NG + NE
    nc = bass.Bass("TRN2", target_bir_lowering=False)
    _bufs = {}

    def GB(name):
        if name not in _bufs:
            _bufs[name] = Buf(name)
        return _bufs[name]

    def din(name, shape):
        return nc.dram_tensor(name, list(shape), F32, kind="ExternalInput").ap()

    x_d = din("x", [NB, S, D])
    c_d = din("c", [NB, D])
    w_ada = din("w_ada", [D, 6 * D])
    b_ada = din("b_ada", [1, 6 * D])
    norm1_g = din("norm1_g", [1, D])
    w_in = din("w_in", [D, INW])
    lam_d = din("lam4", [4, 64])
    subln_g = din("subln_g", [1, 128])
    w_ap = din("w_attn_proj", [AW, D])
    ln_g = din("sgu_ln_g", [1, D])
    ln_b = din("sgu_ln_b", [1, D])
    w_s_d = din("sgu_w_s", [G, 128, 128])
    b_s_d = din("sgu_b_s", [1, G * 128])
    w_sp = din("w_sgu_proj", [D, D])
    w_out = din("w_out", [D, D])
    norm2_g = din("norm2_g", [1, D])
    w_rt = din("w_router", [D, NR])
    b_rt = din("b_router", [1, NR])
    w_gu = din("w_expert_gate_up", [NE, D, 2 * DE])
    w_dn = din("w_expert_down", [NE, DE, D])
    final_g = din("final_g", [1, D])
    y_d = nc.dram_tensor("y", [NB, S, D], F32, kind="ExternalOutput").ap()

    with ExitStack() as st:
        K = Kern(nc, st)
        pe, act, dve, pool, sp = K.pe, K.act, K.dve, K.pool, K.sp

        banks = [K.ps("bank%d" % i, [128, 512], F32) for i in range(8)]
        bbuf = [GB("bank%d" % i) for i in range(8)]

        ident = K.sb("ident", [128, 128], F32); b_ident = GB("ident")
        identb = K.sb("identb", [128, 128], BF16); b_identb = GB("identb")
        bcA = K.sb("bcA", [128, D], F32); b_bcA = GB("bcA")
        bcB = K.sb("bcB", [128, D], F32); b_bcB = GB("bcB")
        fgbc = K.sb("fgbc", [128, D], F32); b_fgbc = GB("fgbc")
        bcC = K.sb("bcC", [128, D], F32); b_bcC = GB("bcC")
        subg = K.sb("subg", [128, 128], F32); b_subg = GB("subg")
        lam_t = K.sb("lam_t", [128, 8], F32); b_lam = GB("lam")
        brt = K.sb("brt", [128, NR], F32); b_brt = GB("brt")
        wrt = K.sb("wrt", [128, DC, NR], BF16); b_wrt = GB("wrt")
        mod_scr = nc.dram_tensor("mod_scr", [NB, 6 * D], F32, kind="Internal").ap(); b_modscr = GB("mod_scr")
        cw_scr = nc.dram_tensor("cw_scr", [NE, S], F32, kind="Internal").ap(); b_cwscr = GB("cw_scr")

        HW_ = DC * S // 2
        VW = TC * NH * 130 // 2 + 2
        o_hT = 0
        o_ya = o_hT + HW_
        o_z = o_ya + HW_
        ZW = max(HW_, VW)
        o_y = o_z + ZW
        YW = max(HW_, 2 * S + 4096)
        o_w = o_y + YW
        WW = 12 * 1024
        ARENA = o_w + WW
        arena = K.sb("arena", [128, ARENA], F32)

        def view(off, words, dt, pattern=None, **kw):
            a = arena[:, off:off + words]
            if dt is BF16:
                a = a.bitcast(BF16)
            if pattern:
                a = a.rearrange(pattern, **kw)
            return a

        hT = view(o_hT, HW_, BF16, "p (c s) -> p c s", c=DC); b_hT = GB("hT")
        yaT = view(o_ya, HW_, BF16, "p (c s) -> p c s", c=NH) if NH == DC else None
        assert NH == DC
        b_yaT = GB("yaT")
        zT = view(o_z, HW_, BF16, "p (c s) -> p c s", c=G); b_zT = GB("zT")
        vaug = view(o_z, (TC * NH * 130) // 2, BF16, "p (t h e) -> p t h e", t=TC, h=NH); b_vaug = GB("vaug")
        yT = view(o_y, HW_, BF16, "p (c s) -> p c s", c=DC); b_yT = GB("yT")
        x1 = view(o_ya, TC * D, F32, "p (t d) -> p t d", t=TC); b_x1 = [GB("x1_%d" % i) for i in range(TC)]
        assert TC * D <= HW_ + ZW

        def mm(out, lhsT, rhs, start, stop, reads, writes, sig=None):
            if sig is None:
                sig = stop
            pe.op(lambda e: e.matmul(out, lhsT=lhsT, rhs=rhs, start=start, stop=stop), reads, writes, sig=sig)

        def tr(out, in_, idt, reads, writes, sig=True):
            pe.op(lambda e: e.transpose(out, in_, idt), reads, writes, sig=sig)

        def A(out, in_, func, reads, writes, **kw):
            act.op(lambda e: e.activation(out=out, in_=in_, func=func, **kw), reads, writes)

        def TT(eng, out, in0, in1, op, reads, writes):
            eng.op(lambda e: e.tensor_tensor(out=out, in0=in0, in1=in1, op=op), reads, writes)

        def TS(eng, out, in0, s1, s2, op0, op1, reads, writes, **kw):
            if op1 is None:
                eng.op(lambda e: e.tensor_scalar(out=out, in0=in0, scalar1=s1, scalar2=None, op0=op0, **kw), reads, writes)
            else:
                eng.op(lambda e: e.tensor_scalar(out=out, in0=in0, scalar1=s1, scalar2=s2, op0=op0, op1=op1, **kw), reads, writes)

        def STT(eng, out, in0, scalar, in1, op0, op1, reads, writes):
            eng.op(lambda e: e.scalar_tensor_tensor(out=out, in0=in0, scalar=scalar, in1=in1, op0=op0, op1=op1), reads, writes)

        def CP(eng, out, in_, reads, writes):
            if eng is act:
                act.op(lambda e: e.copy(out=out, in_=in_), reads, writes)
            else:
                eng.op(lambda e: e.tensor_copy(out=out, in_=in_), reads, writes)

        def LD(q, out, in_, anchor, reads=(), writes=None):
            q.dma(lambda e: e.dma_start(out=out, in_=in_), anchor, reads, [anchor] if writes is None else writes)

        dbg_outs = {}

        def dump(name, ap_, shape, dt, rb):
            if not getattr(cfg, "debug", False):
                return
            d = nc.dram_tensor("dbg_" + name, list(shape), dt, kind="ExternalOutput").ap()
            bb = GB("dbg_" + name)
            sp.dma(lambda e: e.dma_start(out=d, in_=ap_), bb, rb, [bb])
            dbg_outs[name] = d

        def rsqrt_col(dst, src, scale, tmp, rb, wb):
            A(tmp, src, AF.Sqrt, rb, wb, bias=epsc[:src.shape[0], 0:1], scale=scale)
            dve.op(lambda e: e.reciprocal(out=dst, in_=tmp), wb, wb)

        epsc = K.sb("epsc", [128, 1], F32); b_eps = GB("epsc")
        pool.op(lambda e: e.memset(epsc[:], EPS), (), [b_eps])
        pool.op(lambda e: e.memset(ident[:], 1.0), (), [b_ident])
        pool.op(lambda e: e.affine_select(out=ident[:], in_=ident[:], pattern=[[-1, 128]], compare_op=ALU.is_equal,
                                          fill=0.0, base=0, channel_multiplier=1), [b_ident], [b_ident])
        CP(dve, identb[:], ident[:], [b_ident], [b_identb])
        LD(sp, fgbc[:], final_g.broadcast_to([128, D]), b_fgbc)
        LD(sp, subg[:], subln_g.broadcast_to([128, 128]), b_subg)
        LD(sp, brt[:], b_rt.broadcast_to([128, NR]), b_brt)
        TS(dve, subg[:], subg[:], 1.0 - cfg.lam_init, None, ALU.mult, None, [b_subg], [b_subg])
        LD(pool, wrt[:], w_rt.rearrange("(c p) n -> p c n", p=128), b_wrt)
        lamw = K.sb("lamw", [128, 4, 64], F32); b_lamw = GB("lamw")
        LD(sp, lamw[:].rearrange("p a b -> p (a b)"), lam_d.rearrange("a b -> (a b)").rearrange("(o n) -> o n", o=1).broadcast_to([128, 256]), b_lamw)
        TT(dve, lamw[:, 0, :], lamw[:, 0, :], lamw[:, 1, :], ALU.mult, [b_lamw], [b_lamw])
        TT(dve, lamw[:, 2, :], lamw[:, 2, :], lamw[:, 3, :], ALU.mult, [b_lamw], [b_lamw])
        dve.op(lambda e: e.tensor_reduce(out=lam_t[:, 0:1], in_=lamw[:, 0, :], axis=AX.X, op=ALU.add), [b_lamw], [b_lam])
        dve.op(lambda e: e.tensor_reduce(out=lam_t[:, 1:2], in_=lamw[:, 2, :], axis=AX.X, op=ALU.add), [b_lamw], [b_lam])
        A(lam_t[:, 2:4], lam_t[:, 0:2], AF.Exp, [b_lam], [b_lam])
        TT(dve, lam_t[:, 4:5], lam_t[:, 2:3], lam_t[:, 3:4], ALU.subtract, [b_lam], [b_lam])
        TS(dve, lam_t[:, 5:6], lam_t[:, 4:5], cfg.lam_init, -1.0, ALU.add, ALU.mult, [b_lam], [b_lam])

        def wview(off_words, words, dt, pattern=None, rows=None, **kw):
            assert off_words + words <= WW, (off_words, words, WW)
            a_ = arena[:, o_w + off_words:o_w + off_words + words] if rows is None else arena[0:rows, o_w + off_words:o_w + off_words + words]
            if dt is BF16:
                a_ = a_.bitcast(BF16)
            if pattern:
                a_ = a_.rearrange(pattern, **kw)
            return a_

        def yview(off_words, words, dt, pattern=None, **kw):
            assert off_words + words <= YW, (off_words, words, YW)
            return view(o_y + off_words, words, dt, pattern, **kw)

        modv = view(o_y, 6 * D, F32)[0:NB, :]; b_modv = GB("modv")
        c_sb = view(o_y + 6 * D, D, F32)[0:NB, :]; b_c = GB("c_sb")
        sgc = view(o_y + 7 * D, D, F32)[0:NB, :]
        g2row = view(o_hT, 2 * D, F32, "p (a d) -> p a d", a=2)[0:NB]; b_g2row = GB("g2row")
        siluT = wview(0, DC * NB // 2 + 1, BF16)[:, 0:DC * NB].rearrange("p (c b) -> p c b", c=DC); b_siluT = GB("siluT")
        LD(sp, c_sb, c_d, b_c)
        A(sgc, c_sb, AF.Sigmoid, [b_c], [b_c])
        TT(dve, c_sb, c_sb, sgc, ALU.mult, [b_c], [b_c])
        pt = banks[0]
        for dc in range(DC):
            tr(pt[:, dc * NB:(dc + 1) * NB], c_sb[:, dc * 128:(dc + 1) * 128], ident[0:NB, 0:NB], [b_c, b_ident], [bbuf[0]])
        CP(dve, siluT.rearrange("p c b -> p (c b)"), pt[:, 0:DC * NB], [bbuf[0]], [b_siluT])
        LD(sp, modv, b_ada.broadcast_to([NB, 6 * D]), b_modv)
        NBLK = 6 * D // 512
        wa = [wview(64 + i * (DC * 256), DC * 256, BF16, "p (c n) -> p c n", c=DC) for i in range(2)]
        b_wa = [GB("wa0"), GB("wa1")]
        for blk in range(NBLK):
            i = blk % 2
            LD(pool, wa[i], w_ada[:, blk * 512:(blk + 1) * 512].rearrange("(c p) n -> p c n", p=128), b_wa[i])
            bk = 1 + (blk % 2)
            for dc in range(DC):
                mm(banks[bk][0:NB, :], siluT[:, dc, :], wa[i][:, dc, :], dc == 0, dc == DC - 1, [b_siluT, b_wa[i]], [bbuf[bk]])
            TT(dve, modv[:, blk * 512:(blk + 1) * 512], modv[:, blk * 512:(blk + 1) * 512], banks[bk][0:NB, :], ALU.add,
               [b_modv, bbuf[bk]], [b_modv])
        LD(sp, g2row[:, 0, :], norm1_g.broadcast_to([NB, D]), b_g2row)
        LD(sp, g2row[:, 1, :], norm2_g.broadcast_to([NB, D]), b_g2row)
        for (gi, off) in ((0, 1), (1, 4)):
            STT(dve, modv[:, off * D:(off + 1) * D], modv[:, off * D:(off + 1) * D], 1.0, g2row[:, gi, :], ALU.add, ALU.mult,
                [b_modv, b_g2row], [b_modv])
        sp.dma(lambda e: e.dma_start(out=mod_scr, in_=modv), b_modv, [b_modv], [b_modscr])
        dump("modv", modv, [NB, 6 * D], F32, [b_modv])
        K.barrier()

        def bcast_mod(b, idx, dst, b_dst):
            sp.dma(lambda e: e.dma_start(out=dst[:], in_=mod_scr[b:b + 1, idx * D:(idx + 1) * D].broadcast_to([128, D])),
                   b_dst, [b_modscr], [b_dst])

        def norm_to_hT(b, src_fn, b_src_fn, tag):
            xt = [wview(i * D, D, F32) for i in range(2)]; b_xt = [GB(tag + "xt0"), GB(tag + "xt1")]
            junk = wview(2 * D, D, F32); b_junk = GB(tag + "junk")
            hb = [wview(3 * D + i * (D // 2), D // 2, BF16) for i in range(2)]; b_hb = [GB(tag + "hb0"), GB(tag + "hb1")]
            st_ = wview(4 * D, 3 * TC, F32); b_st = GB(tag + "st")
            nj = [wview(4 * D + 3 * TC + i * D, D, F32) for i in range(2)]; b_nj = [GB(tag + "nj0"), GB(tag + "nj1")]
            for tc in range(TC):
                src, b_src = src_fn(tc, xt[tc % 2], b_xt[tc % 2])
                act.op((lambda src=src, tc=tc: (lambda e: e.activation(out=junk, in_=src, func=AF.Square, accum_out=st_[:, tc:tc + 1])))(),
                       [b_src], [b_junk, b_st])
            rsqrt_col(st_[:, 2 * TC:3 * TC], st_[:, 0:TC], 1.0 / D, st_[:, TC:2 * TC], [b_st, b_eps], [b_st])
            for tc in range(TC):
                i = tc % 2
                src, b_src = src_fn(tc, xt[i], b_xt[i])
                STT(dve, nj[i], src, st_[:, 2 * TC + tc:2 * TC + tc + 1], bcA[:], ALU.mult, ALU.mult, [b_src, b_st, b_bcA], [b_nj[i]])
                TT(pool, hb[i], nj[i], bcB[:], ALU.add, [b_nj[i], b_bcB], [b_hb[i]])
                bk = tc % 2
                ptb = banks[bk][:].bitcast(BF16)
                for dc in range(DC):
                    tr(ptb[:, dc * 128:(dc + 1) * 128], hb[i][:, dc * 128:(dc + 1) * 128], identb[:], [b_hb[i], b_identb], [bbuf[bk]],
                       sig=(dc == DC - 1))
                CP(act, hT[:, :, tc * 128:(tc + 1) * 128], ptb[:, 0:DC * 128].rearrange("p (c t) -> p c t", c=DC), [bbuf[bk]], [b_hT])

        for b in range(NB):
            bcast_mod(b, 1, bcA, b_bcA)
            bcast_mod(b, 0, bcB, b_bcB)

            def src_x(tc, xt_i, b_xt_i):
                LD(sp, xt_i, x_d[b, tc * 128:(tc + 1) * 128, :], b_xt_i)
                return xt_i, b_xt_i
            wv = wview(8192 if DC * AW // 2 + 8192 <= WW else 0, DC * AW // 2, BF16, "p (c n) -> p c n", c=DC); b_wv = GB("wv")
            LD(pool, wv, w_in[:, 2 * AW:3 * AW].rearrange("(c p) n -> p c n", p=128), b_wv)
            norm_to_hT(b, src_x, None, "n1")
            if b == 0:
                dump("hT", hT, [128, DC, S], BF16, [b_hT])
            K.barrier()

            pool.op(lambda e: e.memset(vaug[:, :, :, 128:130], 1.0), (), [b_vaug])
            VB = min(512, AW)
            HPB = VB // 128
            for tc in range(TC):
                for hb_ in range(AW // VB):
                    bk = (tc * (AW // VB) + hb_) % 4
                    for dc in range(DC):
                        mm(banks[bk][:, 0:VB], hT[:, dc, tc * 128:(tc + 1) * 128], wv[:, dc, hb_ * VB:(hb_ + 1) * VB], dc == 0, dc == DC - 1,
                           [b_hT, b_wv], [bbuf[bk]])
                    eng = act if (hb_ % 2 == 0) else dve
                    CP(eng, vaug[:, tc, hb_ * HPB:(hb_ + 1) * HPB, 0:128], banks[bk][:, 0:VB].rearrange("p (h e) -> p h e", h=HPB), [bbuf[bk]], [b_vaug])
            if b == 0:
                dump("vaug", vaug, [128, TC, NH, 130], BF16, [b_vaug])
            K.barrier()

            Ttab = yview(0, 2 * S, F32); b_T = GB("Ttab")
            pool.op(lambda e: e.iota(Ttab, pattern=[[1, 2 * S]], base=-S, channel_multiplier=-1,
                                     allow_small_or_imprecise_dtypes=True), (), [b_T])
            Ttab2 = yview(2 * S, 2 * S, F32)
            pool.op(lambda e: e.iota(Ttab2, pattern=[[-1, 2 * S]], base=S, channel_multiplier=1,
                                     allow_small_or_imprecise_dtypes=True), (), [b_T])
            TT(dve, Ttab, Ttab, Ttab2, ALU.max, [b_T], [b_T])
            wqk = [wview(i * (DC * 128), DC * 128, BF16, "p (c n) -> p c n", c=DC) for i in range(2)]
            b_wqk = [GB("wqk0"), GB("wqk1")]
            qo = 2 * DC * 128
            qT = [wview(qo + i * (S // 2), S // 2, BF16) for i in range(2)]; b_qT = [GB("qT0"), GB("qT1")]
            ko = qo + S
            kT = [wview(ko + i * (S // 2), S // 2, BF16) for i in range(2)]; b_kT = [GB("kT0"), GB("kT1")]
            to = ko + S
            NTB = 4
            tmpb = [wview(to + i * QT, QT, F32) for i in range(NTB)]; b_tmp = [GB("tmp%d" % i) for i in range(NTB)]
            po = to + NTB * QT
            pTb = [wview(po + i * (QT // 2), QT // 2, BF16) for i in range(NTB)]; b_pT = [GB("pT%d" % i) for i in range(NTB)]
            so = po + NTB * (QT // 2)
            NQC = QT // 128
            nab = (2 * NQC + 2) // 3
            accs = [wview(so, nab * 387, F32, "p (k c) -> p k c", k=nab)] * 2
            b_accs = [GB("accs0")] * 2
            so += nab * 387
            a_h = [yview(2 * S + i * (TC * 128), TC * 128, F32, "p (t e) -> p t e", t=TC) for i in range(2)]
            b_ah = [GB("a_h0"), GB("a_h1")]
            ssq = [wview(so + i * 3 * TC, TC, F32) for i in range(2)]
            c2e = [wview(so + i * 3 * TC + TC, TC, F32) for i in range(2)]
            rstd = [wview(so + i * 3 * TC + 2 * TC, TC, F32) for i in range(2)]
            b_st3 = [GB("st3_0"), GB("st3_1")]
            so += 6 * TC
            sm = wview(so, 8, F32); b_sm = GB("sm")
            a_j = [wview(so + 8 + 256 + i * 128, 128, F32) for i in range(2)]; b_aj = [GB("a_j0"), GB("a_j1")]
            a_k = wview(so + 8 + 512, 128, F32); b_ak = GB("a_k")
            yh2 = [wview(so + 8 + 128 + i * 64, 64, BF16) for i in range(2)]; b_yh2 = [GB("yh0"), GB("yh1")]
            assert so + 8 + 640 <= WW, so
            SB_ = [3, 4, 5, 6]

            def emit_proj(h):
                i = h % 2
                LD(pool, wqk[i][:, :, 0:128], w_in[:, h * 128:(h + 1) * 128].rearrange("(c p) n -> p c n", p=128), b_wqk[i])
                LD(pool, wqk[i][:, :, 128:256], w_in[:, AW + h * 128:AW + (h + 1) * 128].rearrange("(c p) n -> p c n", p=128), b_wqk[i])
                for t5 in range(S // QT):
                    for (which, dstT, b_dst) in ((0, qT[i], b_qT[i]), (1, kT[i], b_kT[i])):
                        for dc in range(DC):
                            mm(banks[7][:, 0:QT], wqk[i][:, dc, which * 128:(which + 1) * 128], hT[:, dc, t5 * QT:(t5 + 1) * QT],
                               dc == 0, dc == DC - 1, [b_wqk[i], b_hT], [bbuf[7]])
                        CP(act if which == 0 else dve, dstT[:, t5 * QT:(t5 + 1) * QT], banks[7][:, 0:QT], [bbuf[7]], [b_dst])

            def acc(m, qc):
                a_ = m * NQC + qc
                bk = a_ // 3
                return banks[bk][:, (a_ % 3) * 129:(a_ % 3) * 129 + 129], bbuf[bk], (a_ % 3 == 0)

            def emit_score_pair(h, p, qt, kc):
                i = h % 2
                for m in range(2):
                    sbk = SB_[(2 * p + m) % 4]
                    mm(banks[sbk][:, 0:QT], kT[i][m * 64:(m + 1) * 64, kc * 128:(kc + 1) * 128],
                       qT[i][m * 64:(m + 1) * 64, qt * QT:(qt + 1) * QT], True, True, [b_kT[i], b_qT[i]], [bbuf[sbk]])
                off = qt * QT - kc * 128 + S
                for m in range(2):
                    sbk = SB_[(2 * p + m) % 4]
                    ti = (2 * p + m) % NTB
                    STT(dve, tmpb[ti], Ttab[:, off:off + QT], -8.0 * cfg.slopes[h], banks[sbk][:, 0:QT], ALU.mult, ALU.add,
                        [b_T, bbuf[sbk]], [b_tmp[ti]])
                    A(pTb[ti], tmpb[ti], AF.Exp, [b_tmp[ti]], [b_pT[ti]], scale=0.125)

            def emit_av_pair(h, p, qt, kc):
                for m in range(2):
                    ti = (2 * p + m) % NTB
                    for qc in range(NQC):
                        a_ap, a_b, first = acc(m, qc)
                        pe.op((lambda a_ap=a_ap, ti=ti, qc=qc, kc=kc, h=h, first=first: (lambda e: e.matmul(
                            a_ap, lhsT=pTb[ti][:, qc * 128:(qc + 1) * 128], rhs=vaug[:, kc, h, 0:129],
                            start=(kc == 0 and first), stop=(kc == TC - 1), skip_group_check=True)))(),
                            [b_pT[ti], b_vaug], [a_b], sig=(qc == NQC - 1))
                if kc == TC - 1:
                    emit_post(h, qt)

            def emit_post(h, qt):
                i = h % 2
                par = qt % 2
                for bk in range(nab):
                    ncol = min(3, 2 * NQC - 3 * bk) * 129
                    CP(dve, accs[par][:, bk, 0:ncol], banks[bk][:, 0:ncol], [bbuf[bk]], [b_accs[par]])
                for qc in range(NQC):
                    slot = qt * NQC + qc
                    a0_, a1_ = qc, NQC + qc
                    c0 = (a0_ % 3) * 129; c1 = (a1_ % 3) * 129
                    o0 = accs[par][:, a0_ // 3, c0:c0 + 128]; l0 = accs[par][:, a0_ // 3, c0 + 128:c0 + 129]
                    o1 = accs[par][:, a1_ // 3, c1:c1 + 128]; l1 = accs[par][:, a1_ // 3, c1 + 128:c1 + 129]
                    at = a_h[i][:, slot, :]
                    TS(pool, at, o0, l1, 0.0, ALU.mult, ALU.add, [b_accs[par]], [b_ah[i]])
                    TT(pool, sm[:, 0:1], l0, lam_t[:, 5:6], ALU.mult, [b_accs[par], b_lam], [b_sm])
                    TS(pool, a_k, o1, sm[:, 0:1], 0.0, ALU.mult, ALU.add, [b_accs[par], b_sm], [b_ak])
                    TT(pool, at, at, a_k, ALU.add, [b_ak, b_ah[i]], [b_ah[i]])
                    TS(pool, sm[:, 1:2], l0, l1, 0.0, ALU.mult, ALU.add, [b_accs[par]], [b_sm])
                    TS(pool, c2e[i][:, slot:slot + 1], sm[:, 1:2], sm[:, 1:2], EPS, ALU.mult, ALU.mult, [b_sm], [b_st3[i]])

            def emit_norm(h):
                i = h % 2
                for slot in range(TC):
                    j = slot % 2
                    TT(pool, a_j[j], a_h[i][:, slot, :], a_h[i][:, slot, :], ALU.mult, [b_ah[i]], [b_aj[j]])
                    dve.op((lambda i=i, slot=slot, j=j: (lambda e: e.tensor_reduce(out=ssq[i][:, slot:slot + 1], in_=a_j[j], axis=AX.X, op=ALU.add)))(),
                           [b_aj[j]], [b_st3[i]])
                TS(pool, ssq[i], ssq[i], 1.0 / 128, 0.0, ALU.mult, ALU.add, [b_st3[i]], [b_st3[i]])
                TT(pool, ssq[i], ssq[i], c2e[i], ALU.add, [b_st3[i]], [b_st3[i]])
                A(c2e[i], ssq[i], AF.Sqrt, [b_st3[i]], [b_st3[i]])
                dve.op((lambda i=i: (lambda e: e.reciprocal(out=rstd[i], in_=c2e[i])))(), [b_st3[i]], [b_st3[i]])
                ptb = banks[7][:].bitcast(BF16)
                for slot in range(TC):
                    j = slot % 2
                    TS(pool, a_k, a_h[i][:, slot, :], rstd[i][:, slot:slot + 1], 0.0, ALU.mult, ALU.add, [b_ah[i], b_st3[i]], [b_ak])
                    TT(pool, yh2[j], a_k, subg[:], ALU.mult, [b_ak, b_subg], [b_yh2[j]])
                    tr(ptb[:, 0:128], yh2[j], identb[:], [b_yh2[j], b_identb], [bbuf[7]])
                    CP(dve, yaT[:, h, slot * 128:(slot + 1) * 128], ptb[:, 0:128], [bbuf[7]], [b_yaT])

            pairs = [(qt, kc) for qt in range(S // QT) for kc in range(TC)]
            npair = len(pairs)
            emit_proj(0)
            for h in range(NH):
                for p in range(npair + 1):
                    if p < npair:
                        emit_score_pair(h, p, *pairs[p])
                    if p == min(12, npair - 1) and h >= 1:
                        emit_norm(h - 1)
                    if p == npair // 2 and h + 1 < NH:
                        emit_proj(h + 1)
                    if p >= 1:
                        emit_av_pair(h, p - 1, *pairs[p - 1])
            emit_norm(NH - 1)
            if b == 0:
                dump("yaT", yaT, [128, NH, S], BF16, [b_yaT])
            K.barrier()

            NQC_ = QT // 128
            wu = wview(0, DC * D // 2, BF16, "p (c n) -> p c n", c=DC); b_wu = GB("wu")
            wsw = wview(DC * D // 2, DC * D // 2, BF16, "p (c n) -> p c n", c=DC); b_wsw = GB("wsw")
            LD(pool, wu, w_in[:, 3 * AW:3 * AW + D].rearrange("(c p) n -> p c n", p=128), b_wu)
            LD(pool, wsw, w_in[:, 3 * AW + D:3 * AW + 2 * D].rearrange("(c p) n -> p c n", p=128), b_wsw)
            o2 = DC * D
            uTt = wview(o2, G * QT // 2, BF16, "p (g t) -> p g t", g=G); b_uTt = GB("uTt")
            sfulls = [wview(o2 + G * QT // 2 + i * D, D, F32) for i in range(2)]; b_sfulls = [GB("sfull0"), GB("sfull1")]
            lnG = yview(0, D, F32); b_lnG = GB("lnG")
            lnB = yview(D, D, F32); b_lnB = GB("lnB")
            bsbc = yview(2 * D, G * 128, F32); b_bsbc = GB("bsbc")
            tmp2 = yview(2 * D + G * 128, G * 128, F32); b_tmp2 = GB("tmp2")
            g1s = [yview(2 * D + 2 * G * 128 + i * 512, 512, F32) for i in range(2)]; b_g1s = [GB("g1_0"), GB("g1_1")]
            g2s = [yview(YW - 1024 + i * 512, 512, F32) for i in range(2)]; b_g2s = [GB("g2_0"), GB("g2_1")]
            gcnt = [0]
            wsT = yview(2 * D + 2 * G * 128 + 1024, G * 64, BF16, "p (g t) -> p g t", g=G); b_wsT = GB("wsT")
            st4s = [yview(2 * D + 2 * G * 128 + 1024 + G * 64 + 136 + i * 8, 8, F32) for i in range(2)]; b_st4s = [GB("st4_0"), GB("st4_1")]
            vss = [yview(2 * D + 2 * G * 128 + 1024 + G * 64 + 160 + i * (D // 2), D // 2, BF16) for i in range(2)]; b_vss = [GB("vs0"), GB("vs1")]
            assert 2 * D + 2 * G * 128 + 1024 + G * 64 + 160 + D <= YW - 1024
            wst = yview(2 * D + 2 * G * 128 + 1024 + G * 64 + 8, 128, F32); b_wst = GB("wst")
            LD(sp, lnG, ln_g.broadcast_to([128, D]), b_lnG)
            LD(sp, lnB, ln_b.broadcast_to([128, D]), b_lnB)
            LD(sp, bsbc, b_s_d.broadcast_to([128, G * 128]), b_bsbc)
            for g in range(G):
                LD(sp, wst, w_s_d[g], b_wst)
                tr(banks[0][:, 0:128], wst, ident[:], [b_wst, b_ident], [bbuf[0]])
                CP(dve, wsT[:, g, :], banks[0][:, 0:128], [bbuf[0]], [b_wsT])

            C_G = 0.044715 ** 0.5

            def gelu_A(gi, src, n, rb):
                A(g1s[gi][:, 0:n], src, AF.Square, rb, [b_g1s[gi]], scale=C_G)
                STT(dve, g1s[gi][:, 0:n], g1s[gi][:, 0:n], 1.0, src, ALU.add, ALU.mult, [b_g1s[gi]] + rb, [b_g1s[gi]])

            def gelu_B(gi, dst, src, n, rb, wb):
                A(g2s[gi][:, 0:n], g1s[gi][:, 0:n], AF.Sigmoid, [b_g1s[gi]], [b_g2s[gi]], scale=1.5957691216057308)
                TT(dve, dst, g2s[gi][:, 0:n], src, ALU.mult, [b_g2s[gi]] + rb, wb)

            for t5 in range(S // QT):
                def uA(g):
                    bk = g % 2
                    for dc in range(DC):
                        mm(banks[bk][:, 0:QT], wu[:, dc, g * 128:(g + 1) * 128], hT[:, dc, t5 * QT:(t5 + 1) * QT], dc == 0, dc == DC - 1,
                           [b_wu, b_hT], [bbuf[bk]])
                    gelu_A(g % 2, banks[bk][:, 0:QT], QT, [bbuf[bk]])
                uA(0)
                for g in range(G):
                    if g + 1 < G:
                        uA(g + 1)
                    gelu_B(g % 2, uTt[:, g, :], banks[g % 2][:, 0:QT], QT, [bbuf[g % 2]], [b_uTt])
                NHB = D // DB

                def stA(tcl):
                    tc = t5 * NQC_ + tcl
                    sfull, b_sfull = sfulls[tc % 2], b_sfulls[tc % 2]

                    def sA(hb_):
                        bk = 2 + hb_ % 2
                        for dc in range(DC):
                            mm(banks[bk][:, 0:DB], hT[:, dc, tc * 128:(tc + 1) * 128], wsw[:, dc, hb_ * DB:(hb_ + 1) * DB], dc == 0, dc == DC - 1,
                               [b_hT, b_wsw], [bbuf[bk]])
                        gelu_A(hb_ % 2, banks[bk][:, 0:DB], DB, [bbuf[bk]])
                    sA(0)
                    for hb_ in range(NHB):
                        if hb_ + 1 < NHB:
                            sA(hb_ + 1)
                        gelu_B(hb_ % 2, sfull[:, hb_ * DB:(hb_ + 1) * DB], banks[2 + hb_ % 2][:, 0:DB], DB, [bbuf[2 + hb_ % 2]], [b_sfull])

                def stB(tcl):
                    tc = t5 * NQC_ + tcl
                    sfull, b_sfull = sfulls[tc % 2], b_sfulls[tc % 2]
                    vs, b_vs = vss[tc % 2], b_vss[tc % 2]
                    st4, b_st4 = st4s[tc % 2], b_st4s[tc % 2]
                    dve.op((lambda st4=st4, sfull=sfull: (lambda e: e.tensor_reduce(out=st4[:, 0:1], in_=sfull, axis=AX.X, op=ALU.add)))(), [b_sfull], [b_st4])
                    TS(dve, st4[:, 1:2], st4[:, 0:1], -1.0 / D, None, ALU.mult, None, [b_st4], [b_st4])
                    TS(dve, sfull, sfull, st4[:, 1:2], None, ALU.add, None, [b_sfull, b_st4], [b_sfull])
                    for hb_ in range(NHB):
                        act.op((lambda hb_=hb_, sfull=sfull, st4=st4: (lambda e: e.activation(
                            out=banks[6 + hb_][:, 0:DB], in_=sfull[:, hb_ * DB:(hb_ + 1) * DB], func=AF.Square, accum_out=st4[:, 5 + hb_:6 + hb_])))(),
                            [b_sfull], [bbuf[6 + hb_], b_st4])
                    if NHB == 2:
                        TT(dve, st4[:, 2:3], st4[:, 5:6], st4[:, 6:7], ALU.add, [b_st4], [b_st4])
                    else:
                        CP(dve, st4[:, 2:3], st4[:, 5:6], [b_st4], [b_st4])
                    rsqrt_col(st4[:, 4:5], st4[:, 2:3], 1.0 / D, st4[:, 3:4], [b_st4, b_eps], [b_st4])
                    STT(dve, sfull, sfull, st4[:, 4:5], lnG, ALU.mult, ALU.mult, [b_sfull, b_st4, b_lnG], [b_sfull])
                    TT(pool, vs, sfull, lnB, ALU.add, [b_sfull, b_lnB], [b_vs])

                def stC(tcl):
                    tc = t5 * NQC_ + tcl
                    vs, b_vs = vss[tc % 2], b_vss[tc % 2]
                    for g in range(G):
                        bk = 4 + g // 4
                        pe.op((lambda bk=bk, g=g, vs=vs: (lambda e: e.matmul(banks[bk][:, (g % 4) * 128:(g % 4) * 128 + 128], lhsT=vs[:, g * 128:(g + 1) * 128],
                                                                             rhs=wsT[:, g, :], start=True, stop=True, skip_group_check=True)))(),
                              [b_vs, b_wsT], [bbuf[bk]])
                    for gb in range((G + 3) // 4):
                        ng = min(4, G - gb * 4)
                        TT(dve, tmp2[:, gb * 512:gb * 512 + ng * 128], banks[4 + gb][:, 0:ng * 128], bsbc[:, gb * 512:gb * 512 + ng * 128], ALU.add,
                           [bbuf[4 + gb], b_bsbc], [b_tmp2])
                    TT(pool, zT[:, :, tc * 128:(tc + 1) * 128], tmp2.rearrange("p (g t) -> p g t", g=G), uTt[:, :, tcl * 128:(tcl + 1) * 128], ALU.mult,
                       [b_tmp2, b_uTt], [b_zT])

                for step in range(NQC_ + 2):
                    if step < NQC_:
                        stA(step)
                    if 1 <= step < NQC_ + 1:
                        stB(step - 1)
                    if step >= 2:
                        stC(step - 2)
            if b == 0:
                dump("zT", zT, [128, G, S], BF16, [b_zT])
            K.barrier()

            CW = DC * 64
            wsl = [[wview(i * 4 * CW + k * CW, CW, BF16, "p (c n) -> p c n", c=DC) for k in range(4)] for i in range(2)]
            b_wsl = [GB("wsl0"), GB("wsl1")]
            so5 = 8 * CW
            s12 = [[wview(so5 + (i * 2 + k) * QT, QT, F32) for k in range(2)] for i in range(2)]
            b_s12 = [[GB("s12_%d%d" % (i, k)) for k in range(2)] for i in range(2)]
            it = 0
            wo = wview(8192 if DC * D // 2 + 8192 <= WW else 6 * 1024, DC * D // 2, BF16, "p (c n) -> p c n", c=DC); b_wo = GB("wo")
            LD(pool, wo, w_out.rearrange("(c p) n -> p c n", p=128), b_wo)

            def load_s5(j):
                i = j % 2
                srcs = (w_ap[:, j * 128:(j + 1) * 128], w_in[:, 3 * AW + 2 * D + j * 128:3 * AW + 2 * D + (j + 1) * 128],
                        w_sp[:, j * 128:(j + 1) * 128], w_in[:, 3 * AW + 3 * D + j * 128:3 * AW + 3 * D + (j + 1) * 128])
                for k in range(4):
                    LD(pool, wsl[i][k], srcs[k].rearrange("(c p) n -> p c n", p=128), b_wsl[i])
            load_s5(0)
            for j in range(DC):
                i = j % 2
                if j + 1 < DC:
                    load_s5(j + 1)
                for t5 in range(S // QT):
                    p = (it % 2) * 4
                    ii = it % 2
                    it += 1
                    tok = slice(t5 * QT, (t5 + 1) * QT)
                    opnds = ((yaT, b_yaT, NH), (hT, b_hT, DC), (zT, b_zT, G), (hT, b_hT, DC))
                    for k in range(4):
                        src, b_src, nk = opnds[k]
                        for kc in range(nk):
                            mm(banks[p + k][:, 0:QT], wsl[i][k][:, kc, :], src[:, kc, tok], kc == 0, kc == nk - 1, [b_wsl[i], b_src], [bbuf[p + k]])
                    A(s12[ii][0], banks[p + 1][:, 0:QT], AF.Sigmoid, [bbuf[p + 1]], [b_s12[ii][0]])
                    A(s12[ii][1], banks[p + 3][:, 0:QT], AF.Sigmoid, [bbuf[p + 3]], [b_s12[ii][1]])
                    TT(dve, s12[ii][0], s12[ii][0], banks[p + 0][:, 0:QT], ALU.mult, [b_s12[ii][0], bbuf[p + 0]], [b_s12[ii][0]])
                    TT(dve, s12[ii][1], s12[ii][1], banks[p + 2][:, 0:QT], ALU.mult, [b_s12[ii][1], bbuf[p + 2]], [b_s12[ii][1]])
                    TT(pool, yT[:, j, tok], s12[ii][0], s12[ii][1], ALU.add, [b_s12[ii][0], b_s12[ii][1]], [b_yT])
            if b == 0:
                dump("yT", yT, [128, DC, S], BF16, [b_yT])
            K.barrier()

            bcast_mod(b, 2, bcA, b_bcA)
            xt6 = [wview(i * D, D, F32) for i in range(2)]; b_xt6 = [GB("xt6_0"), GB("xt6_1")]
            tm6 = wview(2 * D, D, F32); b_tm6 = GB("tm6")
            for tc in range(TC):
                i = tc % 2
                LD(sp, xt6[i], x_d[b, tc * 128:(tc + 1) * 128, :], b_xt6[i])
                for hb_ in range(D // DB):
                    bk = (tc % 2) * 2 + hb_ % 2
                    for kc in range(DC):
                        mm(banks[bk][:, 0:DB], yT[:, kc, tc * 128:(tc + 1) * 128], wo[:, kc, hb_ * DB:(hb_ + 1) * DB], kc == 0, kc == DC - 1,
                           [b_yT, b_wo], [bbuf[bk]])
                    TT(dve, tm6[:, hb_ * DB:(hb_ + 1) * DB], banks[bk][:, 0:DB], bcA[:, hb_ * DB:(hb_ + 1) * DB], ALU.mult, [bbuf[bk], b_bcA], [b_tm6])
                TT(pool, x1[:, tc, :], tm6, xt6[i], ALU.add, [b_tm6, b_xt6[i]], [b_x1[tc]])
            if b == 0:
                dump("x1", x1, [128, TC, D], F32, b_x1)
            K.barrier()

            bcast_mod(b, 5, bcC, b_bcC)
            MT = min(256, S)
            NMT = S // MT
            MC = MT // 128
            GUW = DC * DE
            DNW = FC * D // 2
            EW = GUW + DNW
            wslot = []
            for si in range(4):
                base_ = (o_y + si * EW) if si < 2 else (o_w + (si - 2) * EW)
                wslot.append((view(base_, GUW, BF16, "p (c n) -> p c n", c=DC), view(base_ + GUW, DNW, BF16, "p (f d) -> p f d", f=FC)))
            assert 2 * EW <= YW
            b_wslot = [GB("wslot%d" % si) for si in range(4)]
            def load_pair(ep):
                for e_ in range(2):
                    e = ep * 2 + e_
                    si = e % 4
                    LD(pool, wslot[si][0], w_gu[e].rearrange("(c p) n -> p c n", p=128), b_wslot[si])
                    LD(pool, wslot[si][1], w_dn[e].rearrange("(f p) d -> p f d", p=128), b_wslot[si])
                    for f in range(FC):
                        TT(pool, wslot[si][1][:, f, :], wslot[si][1][:, f, :], bcC[:], ALU.mult, [b_wslot[si], b_bcC], [b_wslot[si]])

            load_pair(0)

            bcast_mod(b, 4, bcA, b_bcA)
            bcast_mod(b, 3, bcB, b_bcB)
            norm_to_hT(b, lambda tc, xt_i, b_xt_i: (x1[:, tc, :], b_x1[tc]), None, "n2")
            if b == 0:
                dump("h2T", hT, [128, DC, S], BF16, [b_hT])
            K.barrier()

            lg = wview(0, NR + 4, F32)[:, 0:NR]; b_lg = GB("lg")
            r8 = wview(64, 16, F32); b_r8 = GB("r8")
            gm = wview(96, NG, F32); b_gm = GB("gm")
            els = wview(128, EPG, F32); b_els = GB("els")
            m8 = wview(160, 8, F32); b_m8 = GB("m8")
            cws = wview(192, EPG, F32); b_cws = GB("cws")
            cws2 = wview(224, EPG, F32)
            cw = wview(256, NE, F32); b_cw = GB("cw")
            cwT = [wview(512 + i * 128, 128, F32) for i in range(2)]; b_cwT = [GB("cwT0"), GB("cwT1")]
            for tc in range(TC):
                bk = tc % 2
                for dc in range(DC):
                    mm(banks[bk][:, 0:NR], hT[:, dc, tc * 128:(tc + 1) * 128], wrt[:, dc, :], dc == 0, dc == DC - 1, [b_hT, b_wrt], [bbuf[bk]])
                TT(dve, lg, banks[bk][:, 0:NR], brt[:], ALU.add, [bbuf[bk], b_brt], [b_lg])
                dve.op(lambda e: e.tensor_reduce(out=r8[:, 0:1], in_=lg[:, 0:NG], axis=AX.X, op=ALU.max), [b_lg], [b_r8])
                TS(dve, gm, lg[:, 0:NG], r8[:, 0:1], None, ALU.is_equal, None, [b_lg, b_r8], [b_gm])
                TS(dve, r8[:, 1:2], r8[:, 0:1], -1.0, None, ALU.mult, None, [b_r8], [b_r8])
                A(cws2[:, 0:NG], lg[:, 0:NG], AF.Exp, [b_lg, b_r8], [b_cws, b_r8], bias=r8[:, 1:2], scale=1.0, accum_out=r8[:, 2:3])
                dve.op(lambda e: e.reciprocal(out=r8[:, 3:4], in_=r8[:, 2:3]), [b_r8], [b_r8])
                TS(dve, els, lg[:, NG:NG + EPG], gm[:, 0:1], None, ALU.mult, None, [b_lg, b_gm], [b_els])
                for g in range(1, NG):
                    STT(dve, els, lg[:, NG + g * EPG:NG + (g + 1) * EPG], gm[:, g:g + 1], els, ALU.mult, ALU.add, [b_lg, b_gm, b_els], [b_els])
                dve.op(lambda e: e.max(out=m8, in_=els), [b_els], [b_m8])
                TT(dve, r8[:, 4:5], m8[:, 1:2], m8[:, 0:1], ALU.subtract, [b_m8], [b_r8])
                A(r8[:, 5:6], r8[:, 4:5], AF.Exp, [b_r8], [b_r8])
                TS(dve, r8[:, 6:7], r8[:, 5:6], 1.0, None, ALU.add, None, [b_r8], [b_r8])
                dve.op(lambda e: e.reciprocal(out=r8[:, 7:8], in_=r8[:, 6:7]), [b_r8], [b_r8])
                TT(dve, r8[:, 8:9], r8[:, 7:8], r8[:, 3:4], ALU.mult, [b_r8], [b_r8])
                TT(dve, r8[:, 9:10], r8[:, 3:4], r8[:, 8:9], ALU.subtract, [b_r8], [b_r8])
                TS(dve, cws, els, m8[:, 0:1], r8[:, 8:9], ALU.is_equal, ALU.mult, [b_els, b_m8, b_r8], [b_cws])
                TS(dve, cws2, els, m8[:, 1:2], r8[:, 9:10], ALU.is_equal, ALU.mult, [b_els, b_m8, b_r8, b_cws], [b_cws])
                TT(dve, cws, cws, cws2, ALU.add, [b_cws], [b_cws])
                for g in range(NG):
                    TS(dve, cw[:, g * EPG:(g + 1) * EPG], cws, gm[:, g:g + 1], None, ALU.mult, None, [b_cws, b_gm], [b_cw])
                tr(banks[2 + bk][0:NE, 0:128], cw, ident[:], [b_cw, b_ident], [bbuf[2 + bk]])
                CP(dve, cwT[bk][0:NE, :], banks[2 + bk][0:NE, 0:128], [bbuf[2 + bk]], [b_cwT[bk]])
                sp.dma((lambda bk=bk, tc=tc: (lambda e: e.dma_start(out=cw_scr[:, tc * 128:(tc + 1) * 128], in_=cwT[bk][0:NE, :])))(),
                       b_cwT[bk], [b_cwT[bk]], [b_cwscr])
            if b == 0:
                dump("cw", cw_scr, [NE, S], F32, [b_cwscr])
            K.barrier()

            o9 = 2 * EW
            cwbc = [[wview(o9 + (i * 2 + k) * MT, MT, F32) for k in range(2)] for i in range(2)]
            b_cwbc = [[GB("cwbc%d%d" % (i, k)) for k in range(2)] for i in range(2)]
            o9 += 4 * MT
            sg9 = [wview(o9 + i * FC * MT, FC * MT, F32) for i in range(2)]; b_sg9 = [GB("sg9_0"), GB("sg9_1")]
            o9 += 2 * FC * MT
            tm9 = [wview(o9 + i * 512, 512, F32) for i in range(2)]; b_tm9 = [GB("tm9_0"), GB("tm9_1")]
            o9 += 1024
            assert FC * MT <= 512
            actp2 = [[wview(o9 + (i * 2 + k) * (FC * MT // 2), FC * MT // 2, BF16, "p (f t) -> p f t", f=FC) for k in range(2)] for i in range(2)]
            b_actp2 = [[GB("actp%d%d" % (i, k)) for k in range(2)] for i in range(2)]
            o9 += 2 * FC * MT
            assert o9 <= WW, o9
            ycnt = [0]

            def emit_gu(k, ep, mt):
                tok = slice(mt * MT, (mt + 1) * MT)
                for e_ in range(2):
                    e = ep * 2 + e_
                    si = e % 4
                    wg = wslot[si][0]
                    LD(sp, cwbc[e_][mt % 2], cw_scr[e:e + 1, tok].broadcast_to([128, MT]), b_cwbc[e_][mt % 2], reads=[b_cwscr])
                    pb = e_ * 2
                    for n in range(2 * FC):
                        bk = pb + n // FC
                        col = (n % FC) * MT
                        for dc in range(DC):
                            pe.op((lambda bk=bk, col=col, wg=wg, dc=dc, n=n, tok=tok: (lambda e__: e__.matmul(
                                banks[bk][:, col:col + MT], lhsT=wg[:, dc, n * 128:(n + 1) * 128], rhs=hT[:, dc, tok],
                                start=(dc == 0), stop=(dc == DC - 1), skip_group_check=True)))(),
                                [b_wslot[si], b_hT], [bbuf[bk]], sig=(dc == DC - 1))
                    gps = banks[pb][:, 0:FC * MT]
                    ups = banks[pb + 1][:, 0:FC * MT]
                    A(sg9[e_], gps, AF.Sigmoid, [bbuf[pb]], [b_sg9[e_]])
                    TT(dve, sg9[e_], sg9[e_], gps, ALU.mult, [b_sg9[e_], bbuf[pb]], [b_sg9[e_]])
                    TT(dve, sg9[e_], sg9[e_], ups, ALU.mult, [b_sg9[e_], bbuf[pb + 1]], [b_sg9[e_]])
                    for f in range(FC):
                        TT(pool, actp2[e_][k % 2][:, f, :], sg9[e_][:, f * MT:(f + 1) * MT], cwbc[e_][mt % 2], ALU.mult,
                           [b_sg9[e_], b_cwbc[e_][mt % 2]], [b_actp2[e_][k % 2]])

            def emit_down(k, ep, mt):
                for mc in range(MC):
                    tc = mt * MC + mc
                    for hb_ in range(D // DB):
                        bk = 4 + ycnt[0] % 4
                        ti = ycnt[0] % 2
                        ycnt[0] += 1
                        nmm = 0
                        for e_ in range(2):
                            wd_ = wslot[(ep * 2 + e_) % 4][1]
                            for f in range(FC):
                                mm(banks[bk][:, 0:DB], actp2[e_][k % 2][:, f, mc * 128:(mc + 1) * 128], wd_[:, f, hb_ * DB:(hb_ + 1) * DB],
                                   nmm == 0, nmm == 2 * FC - 1, [b_actp2[e_][k % 2], b_wslot[(ep * 2 + e_) % 4]], [bbuf[bk]])
                                nmm += 1
                        TT(dve, x1[:, tc, hb_ * DB:(hb_ + 1) * DB], banks[bk][:, 0:DB], x1[:, tc, hb_ * DB:(hb_ + 1) * DB], ALU.add,
                           [bbuf[bk], b_x1[tc]], [b_x1[tc]])

            items = [(ep, mt) for ep in range(NE // 2) for mt in range(NMT)]
            for k, (ep, mt) in enumerate(items):
                emit_gu(k, ep, mt)
                if k >= 1:
                    emit_down(k - 1, *items[k - 1])
                if mt == 0 and ep + 1 < NE // 2:
                    load_pair(ep + 1)
            emit_down(len(items) - 1, *items[-1])
            if b == 0:
                dump("x2", x1, [128, TC, D], F32, b_x1)
            K.barrier()

            ot = [wview(i * D, D, F32) for i in range(2)]; b_ot = [GB("ot0"), GB("ot1")]
            jk = wview(2 * D, D, F32); b_jk = GB("jk10")
            st10 = wview(3 * D, 3 * TC, F32); b_st10 = GB("st10")
            for tc in range(TC):
                act.op((lambda tc=tc: (lambda e: e.activation(out=jk, in_=x1[:, tc, :], func=AF.Square, accum_out=st10[:, tc:tc + 1])))(),
                       [b_x1[tc]], [b_jk, b_st10])
            rsqrt_col(st10[:, 2 * TC:3 * TC], st10[:, 0:TC], 1.0 / D, st10[:, TC:2 * TC], [b_st10, b_eps], [b_st10])
            for tc in range(TC):
                i = tc % 2
                STT(dve, ot[i], x1[:, tc, :], st10[:, 2 * TC + tc:2 * TC + tc + 1], fgbc[:], ALU.mult, ALU.mult, [b_x1[tc], b_st10, b_fgbc], [b_ot[i]])
                sp.dma((lambda i=i, tc=tc, b=b: (lambda e: e.dma_start(out=y_d[b, tc * 128:(tc + 1) * 128, :], in_=ot[i])))(), b_ot[i], [b_ot[i]], [])
            K.barrier()
        K.barrier()
        K.emit()
    return nc


_NC_CACHE = {}


def _prep_shared(inp, cfg):
    f = lambda a: np.ascontiguousarray(np.asarray(a, dtype=np.float32))
    L = 0
    sh = {
        "w_ada": f(inp["w_ada"][L]), "b_ada": f(inp["b_ada"][L]).reshape(1, -1), "norm1_g": f(inp["norm1_g"][L]).reshape(1, -1),
        "w_in": f(inp["w_in"][L]),
        "lam4": f(np.stack([np.asarray(inp["lambda_q1"][L]), np.asarray(inp["lambda_k1"][L]),
                            np.asarray(inp["lambda_q2"][L]), np.asarray(inp["lambda_k2"][L])], axis=0)),
        "subln_g": f(inp["subln_g"][L]).reshape(1, -1), "w_attn_proj": f(inp["w_attn_proj"][L]),
        "sgu_ln_g": f(inp["sgu_ln_g"][L]).reshape(1, -1), "sgu_ln_b": f(inp["sgu_ln_b"][L]).reshape(1, -1),
        "sgu_w_s": f(inp["sgu_w_s"][L]), "sgu_b_s": f(inp["sgu_b_s"][L]).reshape(1, -1),
        "w_sgu_proj": f(inp["w_sgu_proj"][L]), "w_out": f(inp["w_out"][L]), "norm2_g": f(inp["norm2_g"][L]).reshape(1, -1),
        "w_router": f(np.concatenate([np.asarray(inp["w_router_group"][L]), np.asarray(inp["w_router_expert"][L])], axis=1)),
        "b_router": f(np.concatenate([np.asarray(inp["b_router_group"][L]), np.asarray(inp["b_router_expert"][L])], axis=0)).reshape(1, -1),
        "w_expert_gate_up": f(inp["w_expert_gate_up"][L]), "w_expert_down": f(inp["w_expert_down"][L]),
        "final_g": f(inp["final_g"]).reshape(1, -1),
    }
    return sh


def kernel(**inputs):
    cfg = Cfg()
    n_cores = 8
    x = np.asarray(inputs["x"], dtype=np.float32)
    c = np.asarray(inputs["c"], dtype=np.float32)
    sh = _prep_shared(inputs, cfg)
    if "nc" not in _NC_CACHE:
        _NC_CACHE["nc"] = build(cfg)
    nc = _NC_CACHE["nc"]
    in_maps = []
    for i in range(n_cores):
        m = dict(sh)
        m["x"] = np.ascontiguousarray(x[i * cfg.NB:(i + 1) * cfg.NB])
        m["c"] = np.ascontiguousarray(c[i * cfg.NB:(i + 1) * cfg.NB])
        in_maps.append(m)
    res = run_bass_kernel_spmd(nc, in_maps, core_ids=list(range(n_cores)))
    return np.concatenate([r["y"] for r in res.results], axis=0).astype(np.float32)
```

```python
import math
from contextlib import ExitStack
import numpy as np
import concourse.bass as bass
import concourse.mybir as mybir
from concourse.bass_utils import run_bass_kernel_spmd

F32 = mybir.dt.float32
BF16 = mybir.dt.bfloat16
AF = mybir.ActivationFunctionType
ALU = mybir.AluOpType
AX = mybir.AxisListType
EPS = 1e-6


class Buf:
    __slots__ = ("name", "w", "r", "dsem", "dcnt")

    def __init__(self, name):
        self.name = name
        self.w = None
        self.r = {}
        self.dsem = None
        self.dcnt = 0


class Eng:
    def __init__(self, K, name, sem, self_raw=True):
        self.K = K
        self.name = name
        self.sem = sem
        self.cnt = 0
        self.known = {}
        self.self_raw = self_raw
        self.prog = []

    def wait(self, ev):
        if ev is None:
            return
        sem, val = ev
        if self.known.get(id(sem), 0) >= val:
            return
        self.prog.append(("w", sem, val))
        self.known[id(sem)] = val

    def _deps(self, reads, writes):
        for b in reads:
            if b.w is not None:
                if b.w[0] is self.sem and not self.self_raw:
                    continue
                self.wait(b.w)
        for b in writes:
            if b.w is not None and (b.w[0] is not self.sem or self.self_raw):
                self.wait(b.w)
            for sem, v in b.r.values():
                if sem is not self.sem or self.self_raw:
                    self.wait((sem, v))

    def op(self, fn, reads=(), writes=(), sig=True):
        self._deps(reads, writes)
        if sig:
            self.cnt += 1
            self.prog.append(("o", fn, self.sem, 1))
            ev = (self.sem, self.cnt)
        else:
            self.prog.append(("o", fn, None, 0))
            ev = (self.sem, self.cnt + 1)
        for b in reads:
            b.r[id(self.sem)] = ev
        for b in writes:
            b.w = ev
            b.r = {}
        return ev

    def dma(self, fn, anchor, reads=(), writes=()):
        self._deps(reads, writes)
        if anchor.dsem is None:
            anchor.dsem = self.K.new_sem("d_" + anchor.name)
            self.K.dma_bufs.append(anchor)
        anchor.dcnt += 16
        self.prog.append(("o", fn, anchor.dsem, 16))
        ev = (anchor.dsem, anchor.dcnt)
        for b in reads:
            b.r[id(anchor.dsem)] = ev
        for b in writes:
            b.w = ev
            b.r = {}
        return ev

    def replay(self, eng):
        for it in self.prog:
            if it[0] == "w":
                eng.wait_ge(it[1], it[2])
            else:
                ins = it[1](eng)
                if it[2] is not None:
                    ins.then_inc(it[2], it[3])


class Kern:
    def __init__(self, nc, stack):
        self.nc = nc
        self.stack = stack
        self.dma_bufs = []
        self.pe = Eng(self, "pe", self.new_sem("s_pe"), self_raw=False)
        self.act = Eng(self, "act", self.new_sem("s_act"))
        self.dve = Eng(self, "dve", self.new_sem("s_dve"))
        self.pool = Eng(self, "pool", self.new_sem("s_pool"))
        self.sp = Eng(self, "sp", self.new_sem("s_sp"))
        self.engs = [self.pe, self.act, self.dve, self.pool, self.sp]

    def new_sem(self, name):
        return self.stack.enter_context(self.nc.semaphore(name))

    def sb(self, name, shape, dt):
        return self.stack.enter_context(self.nc.sbuf_tensor(name, shape, dt))

    def ps(self, name, shape, dt):
        return self.stack.enter_context(self.nc.psum_tensor(name, shape, dt))

    def barrier(self, engs=None):
        evs = [(e.sem, e.cnt) for e in self.engs if e.cnt > 0]
        evs += [(b.dsem, b.dcnt) for b in self.dma_bufs]
        for e in (engs or self.engs):
            for ev in evs:
                if ev[0] is not e.sem:
                    e.wait(ev)

    def emit(self):
        with self.nc.Block() as block:
            block.tensor(lambda e: self.pe.replay(e))
            block.scalar(lambda e: self.act.replay(e))
            block.vector(lambda e: self.dve.replay(e))
            block.gpsimd(lambda e: self.pool.replay(e))
            block.sync(lambda e: self.sp.replay(e))


class Cfg:
    def __init__(self, D=1024, S=2048, NB=2, NH=8, NG=4, EPG=8, DE=256, depth_l=0):
        self.D, self.S, self.NB, self.NH, self.NG, self.EPG, self.DE = D, S, NB, NH, NG, EPG, DE
        self.DC = D // 128
        self.TC = S // 128
        self.AW = NH * 128
        self.G = D // 128
        self.NE = NG * EPG
        self.FC = DE // 128
        self.INW = 3 * self.AW + 4 * D
        self.QT = min(512, S)
        self.lam_init = 0.8 - 0.6 * math.exp(-0.3 * depth_l)
        self.slopes = [2.0 ** (-8.0 * (h + 1) / NH) for h in range(NH)]


def build(cfg, stop_after=None):
    DB = min(512, cfg.D)
    D, S, NB, NH, NG, EPG, DE = cfg.D, cfg.S, cfg.NB, cfg.NH, cfg.NG, cfg.EPG, cfg.DE
    DC, TC, AW, G, NE, FC, INW, QT = cfg.DC, cfg.TC, cfg.AW, cfg.G, cfg.NE, cfg.FC, cfg.INW, cfg.QT
    NR = NG + NE
    nc = bass.Bass("TRN2", target_bir_lowering=False)
    _bufs = {}

    def GB(name):
        if name not in _bufs:
            _bufs[name] = Buf(name)
        return _bufs[name]

    def din(name, shape):
        return nc.dram_tensor(name, list(shape), F32, kind="ExternalInput").ap()

    x_d = din("x", [NB, S, D])
    c_d = din("c", [NB, D])
    w_ada = din("w_ada", [D, 6 * D])
    b_ada = din("b_ada", [1, 6 * D])
    norm1_g = din("norm1_g", [1, D])
    w_in = din("w_in", [D, INW])
    lam_d = din("lam4", [4, 64])
    subln_g = din("subln_g", [1, 128])
    w_ap = din("w_attn_proj", [AW, D])
    ln_g = din("sgu_ln_g", [1, D])
    ln_b = din("sgu_ln_b", [1, D])
    w_s_d = din("sgu_w_s", [G, 128, 128])
    b_s_d = din("sgu_b_s", [1, G * 128])
    w_sp = din("w_sgu_proj", [D, D])
    w_out = din("w_out", [D, D])
    norm2_g = din("norm2_g", [1, D])
    w_rt = din("w_router", [D, NR])
    b_rt = din("b_router", [1, NR])
    w_gu = din("w_expert_gate_up", [NE, D, 2 * DE])
    w_dn = din("w_expert_down", [NE, DE, D])
    final_g = din("final_g", [1, D])
    y_d = nc.dram_tensor("y", [NB, S, D], F32, kind="ExternalOutput").ap()

    with ExitStack() as st:
        K = Kern(nc, st)
        pe, act, dve, pool, sp = K.pe, K.act, K.dve, K.pool, K.sp

        banks = [K.ps("bank%d" % i, [128, 512], F32) for i in range(8)]
        bbuf = [GB("bank%d" % i) for i in range(8)]

        ident = K.sb("ident", [128, 128], F32); b_ident = GB("ident")
        identb = K.sb("identb", [128, 128], BF16); b_identb = GB("identb")
        bcA = K.sb("bcA", [128, D], F32); b_bcA = GB("bcA")
        bcB = K.sb("bcB", [128, D], F32); b_bcB = GB("bcB")
        fgbc = K.sb("fgbc", [128, D], F32); b_fgbc = GB("fgbc")
        bcC = K.sb("bcC", [128, D], F32); b_bcC = GB("bcC")
        subg = K.sb("subg", [128, 128], F32); b_subg = GB("subg")
        lam_t = K.sb("lam_t", [128, 8], F32); b_lam = GB("lam")
        brt = K.sb("brt", [128, NR], F32); b_brt = GB("brt")
        wrt = K.sb("wrt", [128, DC, NR], BF16); b_wrt = GB("wrt")
        mod_scr = nc.dram_tensor("mod_scr", [NB, 6 * D], F32, kind="Internal").ap(); b_modscr = GB("mod_scr")
        cw_scr = nc.dram_tensor("cw_scr", [NE, S], F32, kind="Internal").ap(); b_cwscr = GB("cw_scr")

        HW_ = DC * S // 2
        VW = TC * NH * 130 // 2 + 2
        o_hT = 0
        o_ya = o_hT + HW_
        o_z = o_ya + HW_
        ZW = max(HW_, VW)
        o_y = o_z + ZW
        YW = max(HW_, 2 * S + 4096)
        o_w = o_y + YW
        WW = 12 * 1024
        ARENA = o_w + WW
        arena = K.sb("arena", [128, ARENA], F32)

        def view(off, words, dt, pattern=None, **kw):
            a = arena[:, off:off + words]
            if dt is BF16:
                a = a.bitcast(BF16)
            if pattern:
                a = a.rearrange(pattern, **kw)
            return a

        hT = view(o_hT, HW_, BF16, "p (c s) -> p c s", c=DC); b_hT = GB("hT")
        yaT = view(o_ya, HW_, BF16, "p (c s) -> p c s", c=NH) if NH == DC else None
        assert NH == DC
        b_yaT = GB("yaT")
        zT = view(o_z, HW_, BF16, "p (c s) -> p c s", c=G); b_zT = GB("zT")
        vaug = view(o_z, (TC * NH * 130) // 2, BF16, "p (t h e) -> p t h e", t=TC, h=NH); b_vaug = GB("vaug")
        yT = view(o_y, HW_, BF16, "p (c s) -> p c s", c=DC); b_yT = GB("yT")
        x1 = view(o_ya, TC * D, F32, "p (t d) -> p t d", t=TC); b_x1 = [GB("x1_%d" % i) for i in range(TC)]
        assert TC * D <= HW_ + ZW

        def mm(out, lhsT, rhs, start, stop, reads, writes, sig=None):
            if sig is None:
                sig = stop
            pe.op(lambda e: e.matmul(out, lhsT=lhsT, rhs=rhs, start=start, stop=stop), reads, writes, sig=sig)

        def tr(out, in_, idt, reads, writes, sig=True):
            pe.op(lambda e: e.transpose(out, in_, idt), reads, writes, sig=sig)

        def A(out, in_, func, reads, writes, **kw):
            act.op(lambda e: e.activation(out=out, in_=in_, func=func, **kw), reads, writes)

        def TT(eng, out, in0, in1, op, reads, writes):
            eng.op(lambda e: e.tensor_tensor(out=out, in0=in0, in1=in1, op=op), reads, writes)

        def TS(eng, out, in0, s1, s2, op0, op1, reads, writes, **kw):
            if op1 is None:
                eng.op(lambda e: e.tensor_scalar(out=out, in0=in0, scalar1=s1, scalar2=None, op0=op0, **kw), reads, writes)
            else:
                eng.op(lambda e: e.tensor_scalar(out=out, in0=in0, scalar1=s1, scalar2=s2, op0=op0, op1=op1, **kw), reads, writes)

        def STT(eng, out, in0, scalar, in1, op0, op1, reads, writes):
            eng.op(lambda e: e.scalar_tensor_tensor(out=out, in0=in0, scalar=scalar, in1=in1, op0=op0, op1=op1), reads, writes)

        def CP(eng, out, in_, reads, writes):
            if eng is act:
                act.op(lambda e: e.copy(out=out, in_=in_), reads, writes)
            else:
                eng.op(lambda e: e.tensor_copy(out=out, in_=in_), reads, writes)

        def LD(q, out, in_, anchor, reads=(), writes=None):
            q.dma(lambda e: e.dma_start(out=out, in_=in_), anchor, reads, [anchor] if writes is None else writes)

        dbg_outs = {}

        def dump(name, ap_, shape, dt, rb):
            if not getattr(cfg, "debug", False):
                return
            d = nc.dram_tensor("dbg_" + name, list(shape), dt, kind="ExternalOutput").ap()
            bb = GB("dbg_" + name)
            sp.dma(lambda e: e.dma_start(out=d, in_=ap_), bb, rb, [bb])
            dbg_outs[name] = d

        def rsqrt_col(dst, src, scale, tmp, rb, wb):
            A(tmp, src, AF.Sqrt, rb, wb, bias=epsc[:src.shape[0], 0:1], scale=scale)
            dve.op(lambda e: e.reciprocal(out=dst, in_=tmp), wb, wb)

        epsc = K.sb("epsc", [128, 1], F32); b_eps = GB("epsc")
        pool.op(lambda e: e.memset(epsc[:], EPS), (), [b_eps])
        pool.op(lambda e: e.memset(ident[:], 1.0), (), [b_ident])
        pool.op(lambda e: e.affine_select(out=ident[:], in_=ident[:], pattern=[[-1, 128]], compare_op=ALU.is_equal,
                                          fill=0.0, base=0, channel_multiplier=1), [b_ident], [b_ident])
        CP(dve, identb[:], ident[:], [b_ident], [b_identb])
        LD(sp, fgbc[:], final_g.broadcast_to([128, D]), b_fgbc)
        LD(sp, subg[:], subln_g.broadcast_to([128, 128]), b_subg)
        LD(sp, brt[:], b_rt.broadcast_to([128, NR]), b_brt)
        TS(dve, subg[:], subg[:], 1.0 - cfg.lam_init, None, ALU.mult, None, [b_subg], [b_subg])
        LD(pool, wrt[:], w_rt.rearrange("(c p) n -> p c n", p=128), b_wrt)
        lamw = K.sb("lamw", [128, 4, 64], F32); b_lamw = GB("lamw")
        LD(sp, lamw[:].rearrange("p a b -> p (a b)"), lam_d.rearrange("a b -> (a b)").rearrange("(o n) -> o n", o=1).broadcast_to([128, 256]), b_lamw)
        TT(dve, lamw[:, 0, :], lamw[:, 0, :], lamw[:, 1, :], ALU.mult, [b_lamw], [b_lamw])
        TT(dve, lamw[:, 2, :], lamw[:, 2, :], lamw[:, 3, :], ALU.mult, [b_lamw], [b_lamw])
        dve.op(lambda e: e.tensor_reduce(out=lam_t[:, 0:1], in_=lamw[:, 0, :], axis=AX.X, op=ALU.add), [b_lamw], [b_lam])
        dve.op(lambda e: e.tensor_reduce(out=lam_t[:, 1:2], in_=lamw[:, 2, :], axis=AX.X, op=ALU.add), [b_lamw], [b_lam])
        A(lam_t[:, 2:4], lam_t[:, 0:2], AF.Exp, [b_lam], [b_lam])
        TT(dve, lam_t[:, 4:5], lam_t[:, 2:3], lam_t[:, 3:4], ALU.subtract, [b_lam], [b_lam])
        TS(dve, lam_t[:, 5:6], lam_t[:, 4:5], cfg.lam_init, -1.0, ALU.add, ALU.mult, [b_lam], [b_lam])

        def wview(off_words, words, dt, pattern=None, rows=None, **kw):
            assert off_words + words <= WW, (off_words, words, WW)
            a_ = arena[:, o_w + off_words:o_w + off_words + words] if rows is None else arena[0:rows, o_w + off_words:o_w + off_words + words]
            if dt is BF16:
                a_ = a_.bitcast(BF16)
            if pattern:
                a_ = a_.rearrange(pattern, **kw)
            return a_

        def yview(off_words, words, dt, pattern=None, **kw):
            assert off_words + words <= YW, (off_words, words, YW)
            return view(o_y + off_words, words, dt, pattern, **kw)

        modv = view(o_y, 6 * D, F32)[0:NB, :]; b_modv = GB("modv")
        c_sb = view(o_y + 6 * D, D, F32)[0:NB, :]; b_c = GB("c_sb")
        sgc = view(o_y + 7 * D, D, F32)[0:NB, :]
        g2row = view(o_hT, 2 * D, F32, "p (a d) -> p a d", a=2)[0:NB]; b_g2row = GB("g2row")
        siluT = wview(0, DC * NB // 2 + 1, BF16)[:, 0:DC * NB].rearrange("p (c b) -> p c b", c=DC); b_siluT = GB("siluT")
        LD(sp, c_sb, c_d, b_c)
        A(sgc, c_sb, AF.Sigmoid, [b_c], [b_c])
        TT(dve, c_sb, c_sb, sgc, ALU.mult, [b_c], [b_c])
        pt = banks[0]
        for dc in range(DC):
            tr(pt[:, dc * NB:(dc + 1) * NB], c_sb[:, dc * 128:(dc + 1) * 128], ident[0:NB, 0:NB], [b_c, b_ident], [bbuf[0]])
        CP(dve, siluT.rearrange("p c b -> p (c b)"), pt[:, 0:DC * NB], [bbuf[0]], [b_siluT])
        LD(sp, modv, b_ada.broadcast_to([NB, 6 * D]), b_modv)
        NBLK = 6 * D // 512
        wa = [wview(64 + i * (DC * 256), DC * 256, BF16, "p (c n) -> p c n", c=DC) for i in range(2)]
        b_wa = [GB("wa0"), GB("wa1")]
        for blk in range(NBLK):
            i = blk % 2
            LD(pool, wa[i], w_ada[:, blk * 512:(blk + 1) * 512].rearrange("(c p) n -> p c n", p=128), b_wa[i])
            bk = 1 + (blk % 2)
            for dc in range(DC):
                mm(banks[bk][0:NB, :], siluT[:, dc, :], wa[i][:, dc, :], dc == 0, dc == DC - 1, [b_siluT, b_wa[i]], [bbuf[bk]])
            TT(dve, modv[:, blk * 512:(blk + 1) * 512], modv[:, blk * 512:(blk + 1) * 512], banks[bk][0:NB, :], ALU.add,
               [b_modv, bbuf[bk]], [b_modv])
        LD(sp, g2row[:, 0, :], norm1_g.broadcast_to([NB, D]), b_g2row)
        LD(sp, g2row[:, 1, :], norm2_g.broadcast_to([NB, D]), b_g2row)
        for (gi, off) in ((0, 1), (1, 4)):
            STT(dve, modv[:, off * D:(off + 1) * D], modv[:, off * D:(off + 1) * D], 1.0, g2row[:, gi, :], ALU.add, ALU.mult,
                [b_modv, b_g2row], [b_modv])
        sp.dma(lambda e: e.dma_start(out=mod_scr, in_=modv), b_modv, [b_modv], [b_modscr])
        dump("modv", modv, [NB, 6 * D], F32, [b_modv])
        K.barrier()

        def bcast_mod(b, idx, dst, b_dst):
            sp.dma(lambda e: e.dma_start(out=dst[:], in_=mod_scr[b:b + 1, idx * D:(idx + 1) * D].broadcast_to([128, D])),
                   b_dst, [b_modscr], [b_dst])

        def norm_to_hT(b, src_fn, b_src_fn, tag):
            xt = [wview(i * D, D, F32) for i in range(2)]; b_xt = [GB(tag + "xt0"), GB(tag + "xt1")]
            junk = wview(2 * D, D, F32); b_junk = GB(tag + "junk")
            hb = [wview(3 * D + i * (D // 2), D // 2, BF16) for i in range(2)]; b_hb = [GB(tag + "hb0"), GB(tag + "hb1")]
            st_ = wview(4 * D, 3 * TC, F32); b_st = GB(tag + "st")
            nj = [wview(4 * D + 3 * TC + i * D, D, F32) for i in range(2)]; b_nj = [GB(tag + "nj0"), GB(tag + "nj1")]
            for tc in range(TC):
                src, b_src = src_fn(tc, xt[tc % 2], b_xt[tc % 2])
                act.op((lambda src=src, tc=tc: (lambda e: e.activation(out=junk, in_=src, func=AF.Square, accum_out=st_[:, tc:tc + 1])))(),
                       [b_src], [b_junk, b_st])
            rsqrt_col(st_[:, 2 * TC:3 * TC], st_[:, 0:TC], 1.0 / D, st_[:, TC:2 * TC], [b_st, b_eps], [b_st])
            for tc in range(TC):
                i = tc % 2
                src, b_src = src_fn(tc, xt[i], b_xt[i])
                STT(dve, nj[i], src, st_[:, 2 * TC + tc:2 * TC + tc + 1], bcA[:], ALU.mult, ALU.mult, [b_src, b_st, b_bcA], [b_nj[i]])
                TT(pool, hb[i], nj[i], bcB[:], ALU.add, [b_nj[i], b_bcB], [b_hb[i]])
                bk = tc % 2
                ptb = banks[bk][:].bitcast(BF16)
                for dc in range(DC):
                    tr(ptb[:, dc * 128:(dc + 1) * 128], hb[i][:, dc * 128:(dc + 1) * 128], identb[:], [b_hb[i], b_identb], [bbuf[bk]],
                       sig=(dc == DC - 1))
                CP(act, hT[:, :, tc * 128:(tc + 1) * 128], ptb[:, 0:DC * 128].rearrange("p (c t) -> p c t", c=DC), [bbuf[bk]], [b_hT])

        for b in range(NB):
            bcast_mod(b, 1, bcA, b_bcA)
            bcast_mod(b, 0, bcB, b_bcB)

            def src_x(tc, xt_i, b_xt_i):
                LD(sp, xt_i, x_d[b, tc * 128:(tc + 1) * 128, :], b_xt_i)
                return xt_i, b_xt_i
            wv = wview(8192 if DC * AW // 2 + 8192 <= WW else 0, DC * AW // 2, BF16, "p (c n) -> p c n", c=DC); b_wv = GB("wv")
            LD(pool, wv, w_in[:, 2 * AW:3 * AW].rearrange("(c p) n -> p c n", p=128), b_wv)
            norm_to_hT(b, src_x, None, "n1")
            if b == 0:
                dump("hT", hT, [128, DC, S], BF16, [b_hT])
            K.barrier()

            pool.op(lambda e: e.memset(vaug[:, :, :, 128:130], 1.0), (), [b_vaug])
            VB = min(512, AW)
            HPB = VB // 128
            for tc in range(TC):
                for hb_ in range(AW // VB):
                    bk = (tc * (AW // VB) + hb_) % 4
                    for dc in range(DC):
                        mm(banks[bk][:, 0:VB], hT[:, dc, tc * 128:(tc + 1) * 128], wv[:, dc, hb_ * VB:(hb_ + 1) * VB], dc == 0, dc == DC - 1,
                           [b_hT, b_wv], [bbuf[bk]])
                    eng = act if (hb_ % 2 == 0) else dve
                    CP(eng, vaug[:, tc, hb_ * HPB:(hb_ + 1) * HPB, 0:128], banks[bk][:, 0:VB].rearrange("p (h e) -> p h e", h=HPB), [bbuf[bk]], [b_vaug])
            if b == 0:
                dump("vaug", vaug, [128, TC, NH, 130], BF16, [b_vaug])
            K.barrier()

            Ttab = yview(0, 2 * S, F32); b_T = GB("Ttab")
            pool.op(lambda e: e.iota(Ttab, pattern=[[1, 2 * S]], base=-S, channel_multiplier=-1,
                                     allow_small_or_imprecise_dtypes=True), (), [b_T])
            Ttab2 = yview(2 * S, 2 * S, F32)
            pool.op(lambda e: e.iota(Ttab2, pattern=[[-1, 2 * S]], base=S, channel_multiplier=1,
                                     allow_small_or_imprecise_dtypes=True), (), [b_T])
            TT(dve, Ttab, Ttab, Ttab2, ALU.max, [b_T], [b_T])
            wqk = [wview(0, DC * 128, BF16, "p (c n) -> p c n", c=DC)] * 2
            b_wqk = [GB("wqk0")] * 2
            qo = DC * 128
            qT = [wview(qo + i * (S // 2), S // 2, BF16) for i in range(2)]; b_qT = [GB("qT0"), GB("qT1")]
            ko = qo + S
            kT = [wview(ko + i * (S // 2), S // 2, BF16) for i in range(2)]; b_kT = [GB("kT0"), GB("kT1")]
            to = ko + S
            NTB = 6
            LAP = 2
            tmpb = [wview(to + i * QT, QT, F32) for i in range(NTB)]; b_tmp = [GB("tmp%d" % i) for i in range(NTB)]
            po = to + NTB * QT
            pTb = [wview(po + i * (QT // 2), QT // 2, BF16) for i in range(NTB)]; b_pT = [GB("pT%d" % i) for i in range(NTB)]
            so = po + NTB * (QT // 2)
            NQC = QT // 128
            nab = (2 * NQC + 2) // 3
            accs = [wview(so, nab * 387, F32, "p (k c) -> p k c", k=nab)] * 2
            b_accs = [GB("accs0")] * 2
            so += nab * 387
            a_h = [yview(2 * S + i * (TC * 128), TC * 128, F32, "p (t e) -> p t e", t=TC) for i in range(2)]
            b_ah = [GB("a_h0"), GB("a_h1")]
            ssq = [wview(so + i * 3 * TC, TC, F32) for i in range(2)]
            c2e = [wview(so + i * 3 * TC + TC, TC, F32) for i in range(2)]
            rstd = [wview(so + i * 3 * TC + 2 * TC, TC, F32) for i in range(2)]
            b_st3 = [GB("st3_0"), GB("st3_1")]
            so += 6 * TC
            sm = wview(so, 8, F32); b_sm = GB("sm")
            a_j = [wview(so + 8 + 256 + i * 128, 128, F32) for i in range(2)]; b_aj = [GB("a_j0"), GB("a_j1")]
            a_k = wview(so + 8 + 512, 128, F32); b_ak = GB("a_k")
            yh2 = [wview(so + 8 + 128 + i * 64, 64, BF16) for i in range(2)]; b_yh2 = [GB("yh0"), GB("yh1")]
            assert so + 8 + 640 <= WW, so
            SB_ = [3, 4, 5, 6]

            def emit_proj(h):
                i = h % 2
                LD(pool, wqk[i][:, :, 0:128], w_in[:, h * 128:(h + 1) * 128].rearrange("(c p) n -> p c n", p=128), b_wqk[i])
                LD(pool, wqk[i][:, :, 128:256], w_in[:, AW + h * 128:AW + (h + 1) * 128].rearrange("(c p) n -> p c n", p=128), b_wqk[i])
                for t5 in range(S // QT):
                    for (which, dstT, b_dst) in ((0, qT[i], b_qT[i]), (1, kT[i], b_kT[i])):
                        for dc in range(DC):
                            mm(banks[7][:, 0:QT], wqk[i][:, dc, which * 128:(which + 1) * 128], hT[:, dc, t5 * QT:(t5 + 1) * QT],
                               dc == 0, dc == DC - 1, [b_wqk[i], b_hT], [bbuf[7]])
                        CP(act if which == 0 else dve, dstT[:, t5 * QT:(t5 + 1) * QT], banks[7][:, 0:QT], [bbuf[7]], [b_dst])

            def acc(m, qc):
                a_ = m * NQC + qc
                bk = a_ // 3
                return banks[bk][:, (a_ % 3) * 129:(a_ % 3) * 129 + 129], bbuf[bk], (a_ % 3 == 0)

            def emit_score_pair(h, p, qt, kc):
                i = h % 2
                for m in range(2):
                    sbk = SB_[(2 * p + m) % 4]
                    mm(banks[sbk][:, 0:QT], kT[i][m * 64:(m + 1) * 64, kc * 128:(kc + 1) * 128],
                       qT[i][m * 64:(m + 1) * 64, qt * QT:(qt + 1) * QT], True, True, [b_kT[i], b_qT[i]], [bbuf[sbk]])
                off = qt * QT - kc * 128 + S
                for m in range(2):
                    sbk = SB_[(2 * p + m) % 4]
                    ti = (2 * p + m) % NTB
                    STT(dve, tmpb[ti], Ttab[:, off:off + QT], -8.0 * cfg.slopes[h], banks[sbk][:, 0:QT], ALU.mult, ALU.add,
                        [b_T, bbuf[sbk]], [b_tmp[ti]])
                    A(pTb[ti], tmpb[ti], AF.Exp, [b_tmp[ti]], [b_pT[ti]], scale=0.125)

            def emit_av_pair(h, p, qt, kc):
                for m in range(2):
                    ti = (2 * p + m) % NTB
                    for qc in range(NQC):
                        a_ap, a_b, first = acc(m, qc)
                        pe.op((lambda a_ap=a_ap, ti=ti, qc=qc, kc=kc, h=h, first=first: (lambda e: e.matmul(
                            a_ap, lhsT=pTb[ti][:, qc * 128:(qc + 1) * 128], rhs=vaug[:, kc, h, 0:129],
                            start=(kc == 0 and first), stop=(kc == TC - 1), skip_group_check=True)))(),
                            [b_pT[ti], b_vaug], [a_b], sig=(qc == NQC - 1))
                if kc == TC - 1:
                    emit_post(h, qt)

            def emit_post(h, qt):
                i = h % 2
                par = qt % 2
                for bk in range(nab):
                    ncol = min(3, 2 * NQC - 3 * bk) * 129
                    CP(dve, accs[par][:, bk, 0:ncol], banks[bk][:, 0:ncol], [bbuf[bk]], [b_accs[par]])
                for qc in range(NQC):
                    slot = qt * NQC + qc
                    a0_, a1_ = qc, NQC + qc
                    c0 = (a0_ % 3) * 129; c1 = (a1_ % 3) * 129
                    o0 = accs[par][:, a0_ // 3, c0:c0 + 128]; l0 = accs[par][:, a0_ // 3, c0 + 128:c0 + 129]
                    o1 = accs[par][:, a1_ // 3, c1:c1 + 128]; l1 = accs[par][:, a1_ // 3, c1 + 128:c1 + 129]
                    at = a_h[i][:, slot, :]
                    TS(pool, at, o0, l1, 0.0, ALU.mult, ALU.add, [b_accs[par]], [b_ah[i]])
                    TT(pool, sm[:, 0:1], l0, lam_t[:, 5:6], ALU.mult, [b_accs[par], b_lam], [b_sm])
                    TS(pool, a_k, o1, sm[:, 0:1], 0.0, ALU.mult, ALU.add, [b_accs[par], b_sm], [b_ak])
                    TT(pool, at, at, a_k, ALU.add, [b_ak, b_ah[i]], [b_ah[i]])
                    TS(pool, sm[:, 1:2], l0, l1, 0.0, ALU.mult, ALU.add, [b_accs[par]], [b_sm])
                    TS(pool, c2e[i][:, slot:slot + 1], sm[:, 1:2], sm[:, 1:2], EPS, ALU.mult, ALU.mult, [b_sm], [b_st3[i]])

            def emit_norm(h):
                i = h % 2
                for slot in range(TC):
                    j = slot % 2
                    TT(pool, a_j[j], a_h[i][:, slot, :], a_h[i][:, slot, :], ALU.mult, [b_ah[i]], [b_aj[j]])
                    dve.op((lambda i=i, slot=slot, j=j: (lambda e: e.tensor_reduce(out=ssq[i][:, slot:slot + 1], in_=a_j[j], axis=AX.X, op=ALU.add)))(),
                           [b_aj[j]], [b_st3[i]])
                TS(pool, ssq[i], ssq[i], 1.0 / 128, 0.0, ALU.mult, ALU.add, [b_st3[i]], [b_st3[i]])
                TT(pool, ssq[i], ssq[i], c2e[i], ALU.add, [b_st3[i]], [b_st3[i]])
                A(c2e[i], ssq[i], AF.Sqrt, [b_st3[i]], [b_st3[i]])
                dve.op((lambda i=i: (lambda e: e.reciprocal(out=rstd[i], in_=c2e[i])))(), [b_st3[i]], [b_st3[i]])
                ptb = banks[7][:].bitcast(BF16)
                for slot in range(TC):
                    j = slot % 2
                    TS(pool, a_k, a_h[i][:, slot, :], rstd[i][:, slot:slot + 1], 0.0, ALU.mult, ALU.add, [b_ah[i], b_st3[i]], [b_ak])
                    TT(pool, yh2[j], a_k, subg[:], ALU.mult, [b_ak, b_subg], [b_yh2[j]])
                    tr(ptb[:, 0:128], yh2[j], identb[:], [b_yh2[j], b_identb], [bbuf[7]])
                    CP(dve, yaT[:, h, slot * 128:(slot + 1) * 128], ptb[:, 0:128], [bbuf[7]], [b_yaT])

            pairs = [(qt, kc) for qt in range(S // QT) for kc in range(TC)]
            npair = len(pairs)
            emit_proj(0)
            for h in range(NH):
                for p in range(npair + LAP):
                    if p < npair:
                        emit_score_pair(h, p, *pairs[p])
                    if p == min(12, npair - 1) and h >= 1:
                        emit_norm(h - 1)
                    if p == npair // 2 and h + 1 < NH:
                        emit_proj(h + 1)
                    if p >= LAP:
                        emit_av_pair(h, p - LAP, *pairs[p - LAP])
            emit_norm(NH - 1)
            if b == 0:
                dump("yaT", yaT, [128, NH, S], BF16, [b_yaT])
            K.barrier()

            NQC_ = QT // 128
            wu = wview(0, DC * D // 2, BF16, "p (c n) -> p c n", c=DC); b_wu = GB("wu")
            wsw = wview(DC * D // 2, DC * D // 2, BF16, "p (c n) -> p c n", c=DC); b_wsw = GB("wsw")
            LD(pool, wu, w_in[:, 3 * AW:3 * AW + D].rearrange("(c p) n -> p c n", p=128), b_wu)
            LD(pool, wsw, w_in[:, 3 * AW + D:3 * AW + 2 * D].rearrange("(c p) n -> p c n", p=128), b_wsw)
            o2 = DC * D
            uTt = wview(o2, G * QT // 2, BF16, "p (g t) -> p g t", g=G); b_uTt = GB("uTt")
            sfulls = [wview(o2 + G * QT // 2 + i * D, D, F32) for i in range(2)]; b_sfulls = [GB("sfull0"), GB("sfull1")]
            lnG = yview(0, D, F32); b_lnG = GB("lnG")
            lnB = yview(D, D, F32); b_lnB = GB("lnB")
            bsbc = yview(2 * D, G * 128, F32); b_bsbc = GB("bsbc")
            tmp2 = yview(2 * D + G * 128, G * 128, F32); b_tmp2 = GB("tmp2")
            g1s = [yview(2 * D + 2 * G * 128 + i * 512, 512, F32) for i in range(2)]; b_g1s = [GB("g1_0"), GB("g1_1")]
            g2s = [yview(YW - 1024 + i * 512, 512, F32) for i in range(2)]; b_g2s = [GB("g2_0"), GB("g2_1")]
            gcnt = [0]
            wsT = yview(2 * D + 2 * G * 128 + 1024, G * 64, BF16, "p (g t) -> p g t", g=G); b_wsT = GB("wsT")
            st4s = [yview(2 * D + 2 * G * 128 + 1024 + G * 64 + 136 + i * 8, 8, F32) for i in range(2)]; b_st4s = [GB("st4_0"), GB("st4_1")]
            vss = [yview(2 * D + 2 * G * 128 + 1024 + G * 64 + 160 + i * (D // 2), D // 2, BF16) for i in range(2)]; b_vss = [GB("vs0"), GB("vs1")]
            assert 2 * D + 2 * G * 128 + 1024 + G * 64 + 160 + D <= YW - 1024
            wst = yview(2 * D + 2 * G * 128 + 1024 + G * 64 + 8, 128, F32); b_wst = GB("wst")
            LD(sp, lnG, ln_g.broadcast_to([128, D]), b_lnG)
            LD(sp, lnB, ln_b.broadcast_to([128, D]), b_lnB)
            LD(sp, bsbc, b_s_d.broadcast_to([128, G * 128]), b_bsbc)
            for g in range(G):
                LD(sp, wst, w_s_d[g], b_wst)
                tr(banks[0][:, 0:128], wst, ident[:], [b_wst, b_ident], [bbuf[0]])
                CP(dve, wsT[:, g, :], banks[0][:, 0:128], [bbuf[0]], [b_wsT])

            C_G = 0.044715 ** 0.5

            def gelu_A(gi, src, n, rb):
                A(g1s[gi][:, 0:n], src, AF.Square, rb, [b_g1s[gi]], scale=C_G)
                STT(dve, g1s[gi][:, 0:n], g1s[gi][:, 0:n], 1.0, src, ALU.add, ALU.mult, [b_g1s[gi]] + rb, [b_g1s[gi]])

            def gelu_B(gi, dst, src, n, rb, wb):
                A(g2s[gi][:, 0:n], g1s[gi][:, 0:n], AF.Sigmoid, [b_g1s[gi]], [b_g2s[gi]], scale=1.5957691216057308)
                TT(dve, dst, g2s[gi][:, 0:n], src, ALU.mult, [b_g2s[gi]] + rb, wb)

            for t5 in range(S // QT):
                def uA(g):
                    bk = g % 2
                    for dc in range(DC):
                        mm(banks[bk][:, 0:QT], wu[:, dc, g * 128:(g + 1) * 128], hT[:, dc, t5 * QT:(t5 + 1) * QT], dc == 0, dc == DC - 1,
                           [b_wu, b_hT], [bbuf[bk]])
                    gelu_A(g % 2, banks[bk][:, 0:QT], QT, [bbuf[bk]])
                uA(0)
                for g in range(G):
                    if g + 1 < G:
                        uA(g + 1)
                    gelu_B(g % 2, uTt[:, g, :], banks[g % 2][:, 0:QT], QT, [bbuf[g % 2]], [b_uTt])
                NHB = D // DB

                def stA(tcl):
                    tc = t5 * NQC_ + tcl
                    sfull, b_sfull = sfulls[tc % 2], b_sfulls[tc % 2]

                    def sA(hb_):
                        bk = 2 + hb_ % 2
                        for dc in range(DC):
                            mm(banks[bk][:, 0:DB], hT[:, dc, tc * 128:(tc + 1) * 128], wsw[:, dc, hb_ * DB:(hb_ + 1) * DB], dc == 0, dc == DC - 1,
                               [b_hT, b_wsw], [bbuf[bk]])
                        gelu_A(hb_ % 2, banks[bk][:, 0:DB], DB, [bbuf[bk]])
                    sA(0)
                    for hb_ in range(NHB):
                        if hb_ + 1 < NHB:
                            sA(hb_ + 1)
                        gelu_B(hb_ % 2, sfull[:, hb_ * DB:(hb_ + 1) * DB], banks[2 + hb_ % 2][:, 0:DB], DB, [bbuf[2 + hb_ % 2]], [b_sfull])

                def stB(tcl):
                    tc = t5 * NQC_ + tcl
                    sfull, b_sfull = sfulls[tc % 2], b_sfulls[tc % 2]
                    vs, b_vs = vss[tc % 2], b_vss[tc % 2]
                    st4, b_st4 = st4s[tc % 2], b_st4s[tc % 2]
                    dve.op((lambda st4=st4, sfull=sfull: (lambda e: e.tensor_reduce(out=st4[:, 0:1], in_=sfull, axis=AX.X, op=ALU.add)))(), [b_sfull], [b_st4])
                    TS(dve, st4[:, 1:2], st4[:, 0:1], -1.0 / D, None, ALU.mult, None, [b_st4], [b_st4])
                    TS(dve, sfull, sfull, st4[:, 1:2], None, ALU.add, None, [b_sfull, b_st4], [b_sfull])
                    for hb_ in range(NHB):
                        act.op((lambda hb_=hb_, sfull=sfull, st4=st4: (lambda e: e.activation(
                            out=banks[6 + hb_][:, 0:DB], in_=sfull[:, hb_ * DB:(hb_ + 1) * DB], func=AF.Square, accum_out=st4[:, 5 + hb_:6 + hb_])))(),
                            [b_sfull], [bbuf[6 + hb_], b_st4])
                    if NHB == 2:
                        TT(dve, st4[:, 2:3], st4[:, 5:6], st4[:, 6:7], ALU.add, [b_st4], [b_st4])
                    else:
                        CP(dve, st4[:, 2:3], st4[:, 5:6], [b_st4], [b_st4])
                    rsqrt_col(st4[:, 4:5], st4[:, 2:3], 1.0 / D, st4[:, 3:4], [b_st4, b_eps], [b_st4])
                    STT(dve, sfull, sfull, st4[:, 4:5], lnG, ALU.mult, ALU.mult, [b_sfull, b_st4, b_lnG], [b_sfull])
                    TT(pool, vs, sfull, lnB, ALU.add, [b_sfull, b_lnB], [b_vs])

                def stC(tcl):
                    tc = t5 * NQC_ + tcl
                    vs, b_vs = vss[tc % 2], b_vss[tc % 2]
                    for g in range(G):
                        bk = 4 + g // 4
                        pe.op((lambda bk=bk, g=g, vs=vs: (lambda e: e.matmul(banks[bk][:, (g % 4) * 128:(g % 4) * 128 + 128], lhsT=vs[:, g * 128:(g + 1) * 128],
                                                                             rhs=wsT[:, g, :], start=True, stop=True, skip_group_check=True)))(),
                              [b_vs, b_wsT], [bbuf[bk]])
                    for gb in range((G + 3) // 4):
                        ng = min(4, G - gb * 4)
                        TT(dve, tmp2[:, gb * 512:gb * 512 + ng * 128], banks[4 + gb][:, 0:ng * 128], bsbc[:, gb * 512:gb * 512 + ng * 128], ALU.add,
                           [bbuf[4 + gb], b_bsbc], [b_tmp2])
                    TT(pool, zT[:, :, tc * 128:(tc + 1) * 128], tmp2.rearrange("p (g t) -> p g t", g=G), uTt[:, :, tcl * 128:(tcl + 1) * 128], ALU.mult,
                       [b_tmp2, b_uTt], [b_zT])

                for step in range(NQC_ + 2):
                    if step < NQC_:
                        stA(step)
                    if 1 <= step < NQC_ + 1:
                        stB(step - 1)
                    if step >= 2:
                        stC(step - 2)
            if b == 0:
                dump("zT", zT, [128, G, S], BF16, [b_zT])
            K.barrier()

            CW = DC * 64
            wsl = [[wview(i * 4 * CW + k * CW, CW, BF16, "p (c n) -> p c n", c=DC) for k in range(4)] for i in range(2)]
            b_wsl = [GB("wsl0"), GB("wsl1")]
            so5 = 8 * CW
            s12 = [[wview(so5 + (i * 2 + k) * QT, QT, F32) for k in range(2)] for i in range(2)]
            b_s12 = [[GB("s12_%d%d" % (i, k)) for k in range(2)] for i in range(2)]
            it = 0
            wo = wview(8192 if DC * D // 2 + 8192 <= WW else 6 * 1024, DC * D // 2, BF16, "p (c n) -> p c n", c=DC); b_wo = GB("wo")
            LD(pool, wo, w_out.rearrange("(c p) n -> p c n", p=128), b_wo)

            def load_s5(j):
                i = j % 2
                srcs = (w_ap[:, j * 128:(j + 1) * 128], w_in[:, 3 * AW + 2 * D + j * 128:3 * AW + 2 * D + (j + 1) * 128],
                        w_sp[:, j * 128:(j + 1) * 128], w_in[:, 3 * AW + 3 * D + j * 128:3 * AW + 3 * D + (j + 1) * 128])
                for k in range(4):
                    LD(pool, wsl[i][k], srcs[k].rearrange("(c p) n -> p c n", p=128), b_wsl[i])
            load_s5(0)
            for j in range(DC):
                i = j % 2
                if j + 1 < DC:
                    load_s5(j + 1)
                for t5 in range(S // QT):
                    p = (it % 2) * 4
                    ii = it % 2
                    it += 1
                    tok = slice(t5 * QT, (t5 + 1) * QT)
                    opnds = ((yaT, b_yaT, NH), (hT, b_hT, DC), (zT, b_zT, G), (hT, b_hT, DC))
                    for k in range(4):
                        src, b_src, nk = opnds[k]
                        for kc in range(nk):
                            mm(banks[p + k][:, 0:QT], wsl[i][k][:, kc, :], src[:, kc, tok], kc == 0, kc == nk - 1, [b_wsl[i], b_src], [bbuf[p + k]])
                    A(s12[ii][0], banks[p + 1][:, 0:QT], AF.Sigmoid, [bbuf[p + 1]], [b_s12[ii][0]])
                    A(s12[ii][1], banks[p + 3][:, 0:QT], AF.Sigmoid, [bbuf[p + 3]], [b_s12[ii][1]])
                    TT(dve, s12[ii][0], s12[ii][0], banks[p + 0][:, 0:QT], ALU.mult, [b_s12[ii][0], bbuf[p + 0]], [b_s12[ii][0]])
                    TT(dve, s12[ii][1], s12[ii][1], banks[p + 2][:, 0:QT], ALU.mult, [b_s12[ii][1], bbuf[p + 2]], [b_s12[ii][1]])
                    TT(pool, yT[:, j, tok], s12[ii][0], s12[ii][1], ALU.add, [b_s12[ii][0], b_s12[ii][1]], [b_yT])
            if b == 0:
                dump("yT", yT, [128, DC, S], BF16, [b_yT])
            K.barrier()

            bcast_mod(b, 2, bcA, b_bcA)
            xt6 = [wview(i * D, D, F32) for i in range(2)]; b_xt6 = [GB("xt6_0"), GB("xt6_1")]
            tm6 = wview(2 * D, D, F32); b_tm6 = GB("tm6")
            for tc in range(TC):
                i = tc % 2
                LD(sp, xt6[i], x_d[b, tc * 128:(tc + 1) * 128, :], b_xt6[i])
                for hb_ in range(D // DB):
                    bk = (tc % 2) * 2 + hb_ % 2
                    for kc in range(DC):
                        mm(banks[bk][:, 0:DB], yT[:, kc, tc * 128:(tc + 1) * 128], wo[:, kc, hb_ * DB:(hb_ + 1) * DB], kc == 0, kc == DC - 1,
                           [b_yT, b_wo], [bbuf[bk]])
                    TT(dve, tm6[:, hb_ * DB:(hb_ + 1) * DB], banks[bk][:, 0:DB], bcA[:, hb_ * DB:(hb_ + 1) * DB], ALU.mult, [bbuf[bk], b_bcA], [b_tm6])
                TT(pool, x1[:, tc, :], tm6, xt6[i], ALU.add, [b_tm6, b_xt6[i]], [b_x1[tc]])
            if b == 0:
                dump("x1", x1, [128, TC, D], F32, b_x1)
            K.barrier()

            bcast_mod(b, 5, bcC, b_bcC)
            MT = min(256, S)
            NMT = S // MT
            MC = MT // 128
            GUW = DC * DE
            DNW = FC * D // 2
            EW = GUW + DNW
            wslot = []
            for si in range(4):
                base_ = (o_y + si * EW) if si < 2 else (o_w + (si - 2) * EW)
                wslot.append((view(base_, GUW, BF16, "p (c n) -> p c n", c=DC), view(base_ + GUW, DNW, BF16, "p (f d) -> p f d", f=FC)))
            assert 2 * EW <= YW
            b_wslot = [GB("wslot%d" % si) for si in range(4)]
            def load_pair(ep):
                for e_ in range(2):
                    e = ep * 2 + e_
                    si = e % 4
                    LD(pool, wslot[si][0], w_gu[e].rearrange("(c p) n -> p c n", p=128), b_wslot[si])
                    LD(pool, wslot[si][1], w_dn[e].rearrange("(f p) d -> p f d", p=128), b_wslot[si])
                    for f in range(FC):
                        TT(pool, wslot[si][1][:, f, :], wslot[si][1][:, f, :], bcC[:], ALU.mult, [b_wslot[si], b_bcC], [b_wslot[si]])

            load_pair(0)

            bcast_mod(b, 4, bcA, b_bcA)
            bcast_mod(b, 3, bcB, b_bcB)
            norm_to_hT(b, lambda tc, xt_i, b_xt_i: (x1[:, tc, :], b_x1[tc]), None, "n2")
            if b == 0:
                dump("h2T", hT, [128, DC, S], BF16, [b_hT])
            K.barrier()

            lg = wview(0, NR + 4, F32)[:, 0:NR]; b_lg = GB("lg")
            r8 = wview(64, 16, F32); b_r8 = GB("r8")
            gm = wview(96, NG, F32); b_gm = GB("gm")
            els = wview(128, EPG, F32); b_els = GB("els")
            m8 = wview(160, 8, F32); b_m8 = GB("m8")
            cws = wview(192, EPG, F32); b_cws = GB("cws")
            cws2 = wview(224, EPG, F32)
            cw = wview(256, NE, F32); b_cw = GB("cw")
            cwT = [wview(512 + i * 128, 128, F32) for i in range(2)]; b_cwT = [GB("cwT0"), GB("cwT1")]
            for tc in range(TC):
                bk = tc % 2
                for dc in range(DC):
                    mm(banks[bk][:, 0:NR], hT[:, dc, tc * 128:(tc + 1) * 128], wrt[:, dc, :], dc == 0, dc == DC - 1, [b_hT, b_wrt], [bbuf[bk]])
                TT(dve, lg, banks[bk][:, 0:NR], brt[:], ALU.add, [bbuf[bk], b_brt], [b_lg])
                dve.op(lambda e: e.tensor_reduce(out=r8[:, 0:1], in_=lg[:, 0:NG], axis=AX.X, op=ALU.max), [b_lg], [b_r8])
                TS(dve, gm, lg[:, 0:NG], r8[:, 0:1], None, ALU.is_equal, None, [b_lg, b_r8], [b_gm])
                TS(dve, r8[:, 1:2], r8[:, 0:1], -1.0, None, ALU.mult, None, [b_r8], [b_r8])
                A(cws2[:, 0:NG], lg[:, 0:NG], AF.Exp, [b_lg, b_r8], [b_cws, b_r8], bias=r8[:, 1:2], scale=1.0, accum_out=r8[:, 2:3])
                dve.op(lambda e: e.reciprocal(out=r8[:, 3:4], in_=r8[:, 2:3]), [b_r8], [b_r8])
                TS(dve, els, lg[:, NG:NG + EPG], gm[:, 0:1], None, ALU.mult, None, [b_lg, b_gm], [b_els])
                for g in range(1, NG):
                    STT(dve, els, lg[:, NG + g * EPG:NG + (g + 1) * EPG], gm[:, g:g + 1], els, ALU.mult, ALU.add, [b_lg, b_gm, b_els], [b_els])
                dve.op(lambda e: e.max(out=m8, in_=els), [b_els], [b_m8])
                TT(dve, r8[:, 4:5], m8[:, 1:2], m8[:, 0:1], ALU.subtract, [b_m8], [b_r8])
                A(r8[:, 5:6], r8[:, 4:5], AF.Exp, [b_r8], [b_r8])
                TS(dve, r8[:, 6:7], r8[:, 5:6], 1.0, None, ALU.add, None, [b_r8], [b_r8])
                dve.op(lambda e: e.reciprocal(out=r8[:, 7:8], in_=r8[:, 6:7]), [b_r8], [b_r8])
                TT(dve, r8[:, 8:9], r8[:, 7:8], r8[:, 3:4], ALU.mult, [b_r8], [b_r8])
                TT(dve, r8[:, 9:10], r8[:, 3:4], r8[:, 8:9], ALU.subtract, [b_r8], [b_r8])
                TS(dve, cws, els, m8[:, 0:1], r8[:, 8:9], ALU.is_equal, ALU.mult, [b_els, b_m8, b_r8], [b_cws])
                TS(dve, cws2, els, m8[:, 1:2], r8[:, 9:10], ALU.is_equal, ALU.mult, [b_els, b_m8, b_r8, b_cws], [b_cws])
                TT(dve, cws, cws, cws2, ALU.add, [b_cws], [b_cws])
                for g in range(NG):
                    TS(dve, cw[:, g * EPG:(g + 1) * EPG], cws, gm[:, g:g + 1], None, ALU.mult, None, [b_cws, b_gm], [b_cw])
                tr(banks[2 + bk][0:NE, 0:128], cw, ident[:], [b_cw, b_ident], [bbuf[2 + bk]])
                CP(dve, cwT[bk][0:NE, :], banks[2 + bk][0:NE, 0:128], [bbuf[2 + bk]], [b_cwT[bk]])
                sp.dma((lambda bk=bk, tc=tc: (lambda e: e.dma_start(out=cw_scr[:, tc * 128:(tc + 1) * 128], in_=cwT[bk][0:NE, :])))(),
                       b_cwT[bk], [b_cwT[bk]], [b_cwscr])
            if b == 0:
                dump("cw", cw_scr, [NE, S], F32, [b_cwscr])
            K.barrier()

            o9 = 2 * EW
            cwbc = [[wview(o9 + (i * 2 + k) * MT, MT, F32) for k in range(2)] for i in range(2)]
            b_cwbc = [[GB("cwbc%d%d" % (i, k)) for k in range(2)] for i in range(2)]
            o9 += 4 * MT
            sg9 = [wview(o9 + i * FC * MT, FC * MT, F32) for i in range(2)]; b_sg9 = [GB("sg9_0"), GB("sg9_1")]
            o9 += 2 * FC * MT
            tm9 = [wview(o9 + i * 512, 512, F32) for i in range(2)]; b_tm9 = [GB("tm9_0"), GB("tm9_1")]
            o9 += 1024
            assert FC * MT <= 512
            actp2 = [[wview(o9 + (i * 2 + k) * (FC * MT // 2), FC * MT // 2, BF16, "p (f t) -> p f t", f=FC) for k in range(2)] for i in range(2)]
            b_actp2 = [[GB("actp%d%d" % (i, k)) for k in range(2)] for i in range(2)]
            o9 += 2 * FC * MT
            assert o9 <= WW, o9
            ycnt = [0]

            def emit_gu(k, ep, mt):
                tok = slice(mt * MT, (mt + 1) * MT)
                for e_ in range(2):
                    e = ep * 2 + e_
                    si = e % 4
                    wg = wslot[si][0]
                    LD(sp, cwbc[e_][mt % 2], cw_scr[e:e + 1, tok].broadcast_to([128, MT]), b_cwbc[e_][mt % 2], reads=[b_cwscr])
                    pb = e_ * 2
                    for n in range(2 * FC):
                        bk = pb + n // FC
                        col = (n % FC) * MT
                        for dc in range(DC):
                            pe.op((lambda bk=bk, col=col, wg=wg, dc=dc, n=n, tok=tok: (lambda e__: e__.matmul(
                                banks[bk][:, col:col + MT], lhsT=wg[:, dc, n * 128:(n + 1) * 128], rhs=hT[:, dc, tok],
                                start=(dc == 0), stop=(dc == DC - 1), skip_group_check=True)))(),
                                [b_wslot[si], b_hT], [bbuf[bk]], sig=(dc == DC - 1))
                    gps = banks[pb][:, 0:FC * MT]
                    ups = banks[pb + 1][:, 0:FC * MT]
                    A(sg9[e_], gps, AF.Sigmoid, [bbuf[pb]], [b_sg9[e_]])
                    TT(dve, sg9[e_], sg9[e_], gps, ALU.mult, [b_sg9[e_], bbuf[pb]], [b_sg9[e_]])
                    TT(dve, sg9[e_], sg9[e_], ups, ALU.mult, [b_sg9[e_], bbuf[pb + 1]], [b_sg9[e_]])
                    for f in range(FC):
                        TT(pool, actp2[e_][k % 2][:, f, :], sg9[e_][:, f * MT:(f + 1) * MT], cwbc[e_][mt % 2], ALU.mult,
                           [b_sg9[e_], b_cwbc[e_][mt % 2]], [b_actp2[e_][k % 2]])

            def emit_down(k, ep, mt):
                for mc in range(MC):
                    tc = mt * MC + mc
                    for hb_ in range(D // DB):
                        bk = 4 + ycnt[0] % 4
                        ti = ycnt[0] % 2
                        ycnt[0] += 1
                        nmm = 0
                        for e_ in range(2):
                            wd_ = wslot[(ep * 2 + e_) % 4][1]
                            for f in range(FC):
                                mm(banks[bk][:, 0:DB], actp2[e_][k % 2][:, f, mc * 128:(mc + 1) * 128], wd_[:, f, hb_ * DB:(hb_ + 1) * DB],
                                   nmm == 0, nmm == 2 * FC - 1, [b_actp2[e_][k % 2], b_wslot[(ep * 2 + e_) % 4]], [bbuf[bk]])
                                nmm += 1
                        TT(dve, x1[:, tc, hb_ * DB:(hb_ + 1) * DB], banks[bk][:, 0:DB], x1[:, tc, hb_ * DB:(hb_ + 1) * DB], ALU.add,
                           [bbuf[bk], b_x1[tc]], [b_x1[tc]])

            items = [(ep, mt) for ep in range(NE // 2) for mt in range(NMT)]
            for k, (ep, mt) in enumerate(items):
                emit_gu(k, ep, mt)
                if k >= 1:
                    emit_down(k - 1, *items[k - 1])
                if mt == 0 and ep + 1 < NE // 2:
                    load_pair(ep + 1)
            emit_down(len(items) - 1, *items[-1])
            if b == 0:
                dump("x2", x1, [128, TC, D], F32, b_x1)
            K.barrier()

            ot = [wview(i * D, D, F32) for i in range(2)]; b_ot = [GB("ot0"), GB("ot1")]
            jk = wview(2 * D, D, F32); b_jk = GB("jk10")
            st10 = wview(3 * D, 3 * TC, F32); b_st10 = GB("st10")
            for tc in range(TC):
                act.op((lambda tc=tc: (lambda e: e.activation(out=jk, in_=x1[:, tc, :], func=AF.Square, accum_out=st10[:, tc:tc + 1])))(),
                       [b_x1[tc]], [b_jk, b_st10])
            rsqrt_col(st10[:, 2 * TC:3 * TC], st10[:, 0:TC], 1.0 / D, st10[:, TC:2 * TC], [b_st10, b_eps], [b_st10])
            for tc in range(TC):
                i = tc % 2
                STT(dve, ot[i], x1[:, tc, :], st10[:, 2 * TC + tc:2 * TC + tc + 1], fgbc[:], ALU.mult, ALU.mult, [b_x1[tc], b_st10, b_fgbc], [b_ot[i]])
                sp.dma((lambda i=i, tc=tc, b=b: (lambda e: e.dma_start(out=y_d[b, tc * 128:(tc + 1) * 128, :], in_=ot[i])))(), b_ot[i], [b_ot[i]], [])
            K.barrier()
        K.barrier()
        K.emit()
    return nc


_NC_CACHE = {}


def _prep_shared(inp, cfg):
    f = lambda a: np.ascontiguousarray(np.asarray(a, dtype=np.float32))
    L = 0
    sh = {
        "w_ada": f(inp["w_ada"][L]), "b_ada": f(inp["b_ada"][L]).reshape(1, -1), "norm1_g": f(inp["norm1_g"][L]).reshape(1, -1),
        "w_in": f(inp["w_in"][L]),
        "lam4": f(np.stack([np.asarray(inp["lambda_q1"][L]), np.asarray(inp["lambda_k1"][L]),
                            np.asarray(inp["lambda_q2"][L]), np.asarray(inp["lambda_k2"][L])], axis=0)),
        "subln_g": f(inp["subln_g"][L]).reshape(1, -1), "w_attn_proj": f(inp["w_attn_proj"][L]),
        "sgu_ln_g": f(inp["sgu_ln_g"][L]).reshape(1, -1), "sgu_ln_b": f(inp["sgu_ln_b"][L]).reshape(1, -1),
        "sgu_w_s": f(inp["sgu_w_s"][L]), "sgu_b_s": f(inp["sgu_b_s"][L]).reshape(1, -1),
        "w_sgu_proj": f(inp["w_sgu_proj"][L]), "w_out": f(inp["w_out"][L]), "norm2_g": f(inp["norm2_g"][L]).reshape(1, -1),
        "w_router": f(np.concatenate([np.asarray(inp["w_router_group"][L]), np.asarray(inp["w_router_expert"][L])], axis=1)),
        "b_router": f(np.concatenate([np.asarray(inp["b_router_group"][L]), np.asarray(inp["b_router_expert"][L])], axis=0)).reshape(1, -1),
        "w_expert_gate_up": f(inp["w_expert_gate_up"][L]), "w_expert_down": f(inp["w_expert_down"][L]),
        "final_g": f(inp["final_g"]).reshape(1, -1),
    }
    return sh


def kernel(**inputs):
    cfg = Cfg()
    n_cores = 8
    x = np.asarray(inputs["x"], dtype=np.float32)
    c = np.asarray(inputs["c"], dtype=np.float32)
    sh = _prep_shared(inputs, cfg)
    if "nc" not in _NC_CACHE:
        _NC_CACHE["nc"] = build(cfg)
    nc = _NC_CACHE["nc"]
    in_maps = []
    for i in range(n_cores):
        m = dict(sh)
        m["x"] = np.ascontiguousarray(x[i * cfg.NB:(i + 1) * cfg.NB])
        m["c"] = np.ascontiguousarray(c[i * cfg.NB:(i + 1) * cfg.NB])
        in_maps.append(m)
    res = run_bass_kernel_spmd(nc, in_maps, core_ids=list(range(n_cores)))
    return np.concatenate([r["y"] for r in res.results], axis=0).astype(np.float32)
```

```python
import math
from contextlib import ExitStack
import numpy as np
import concourse.bass as bass
import concourse.mybir as mybir
from concourse.bass_utils import run_bass_kernel_spmd

F32 = mybir.dt.float32
BF16 = mybir.dt.bfloat16
AF = mybir.ActivationFunctionType
ALU = mybir.AluOpType
AX = mybir.AxisListType
EPS = 1e-6


class Buf:
    __slots__ = ("name", "w", "r", "dsem", "dcnt")

    def __init__(self, name):
        self.name = name
        self.w = None
        self.r = {}
        self.dsem = None
        self.dcnt = 0


class Eng:
    def __init__(self, K, name, sem, self_raw=True):
        self.K = K
        self.name = name
        self.sem = sem
        self.cnt = 0
        self.known = {}
        self.self_raw = self_raw
        self.prog = []

    def wait(self, ev):
        if ev is None:
            return
        sem, val = ev
        if self.known.get(id(sem), 0) >= val:
            return
        self.prog.append(("w", sem, val))
        self.known[id(sem)] = val

    def _deps(self, reads, writes):
        for b in reads:
            if b.w is not None:
                if b.w[0] is self.sem and not self.self_raw:
                    continue
                self.wait(b.w)
        for b in writes:
            if b.w is not None and (b.w[0] is not self.sem or self.self_raw):
                self.wait(b.w)
            for sem, v in b.r.values():
                if sem is not self.sem or self.self_raw:
                    self.wait((sem, v))

    def op(self, fn, reads=(), writes=(), sig=True):
        self._deps(reads, writes)
        if sig:
            self.cnt += 1
            self.prog.append(("o", fn, self.sem, 1))
            ev = (self.sem, self.cnt)
        else:
            self.prog.append(("o", fn, None, 0))
            ev = (self.sem, self.cnt + 1)
        for b in reads:
            b.r[id(self.sem)] = ev
        for b in writes:
            b.w = ev
            b.r = {}
        return ev

    def dma(self, fn, anchor, reads=(), writes=()):
        self._deps(reads, writes)
        if anchor.dsem is None:
            anchor.dsem = self.K.new_sem("d_" + anchor.name)
            self.K.dma_bufs.append(anchor)
        anchor.dcnt += 16
        self.prog.append(("o", fn, anchor.dsem, 16))
        ev = (anchor.dsem, anchor.dcnt)
        for b in reads:
            b.r[id(anchor.dsem)] = ev
        for b in writes:
            b.w = ev
            b.r = {}
        return ev

    def replay(self, eng):
        for it in self.prog:
            if it[0] == "w":
                eng.wait_ge(it[1], it[2])
            else:
                ins = it[1](eng)
                if it[2] is not None:
                    ins.then_inc(it[2], it[3])


class Kern:
    def __init__(self, nc, stack):
        self.nc = nc
        self.stack = stack
        self.dma_bufs = []
        self.pe = Eng(self, "pe", self.new_sem("s_pe"), self_raw=False)
        self.act = Eng(self, "act", self.new_sem("s_act"))
        self.dve = Eng(self, "dve", self.new_sem("s_dve"))
        self.pool = Eng(self, "pool", self.new_sem("s_pool"))
        self.sp = Eng(self, "sp", self.new_sem("s_sp"))
        self.engs = [self.pe, self.act, self.dve, self.pool, self.sp]

    def new_sem(self, name):
        return self.stack.enter_context(self.nc.semaphore(name))

    def sb(self, name, shape, dt):
        return self.stack.enter_context(self.nc.sbuf_tensor(name, shape, dt))

    def ps(self, name, shape, dt):
        return self.stack.enter_context(self.nc.psum_tensor(name, shape, dt))

    def barrier(self, engs=None):
        evs = [(e.sem, e.cnt) for e in self.engs if e.cnt > 0]
        evs += [(b.dsem, b.dcnt) for b in self.dma_bufs]
        for e in (engs or self.engs):
            for ev in evs:
                if ev[0] is not e.sem:
                    e.wait(ev)

    def emit(self):
        with self.nc.Block() as block:
            block.tensor(lambda e: self.pe.replay(e))
            block.scalar(lambda e: self.act.replay(e))
            block.vector(lambda e: self.dve.replay(e))
            block.gpsimd(lambda e: self.pool.replay(e))
            block.sync(lambda e: self.sp.replay(e))


class Cfg:
    def __init__(self, D=1024, S=2048, NB=2, NH=8, NG=4, EPG=8, DE=256, depth_l=0):
        self.D, self.S, self.NB, self.NH, self.NG, self.EPG, self.DE = D, S, NB, NH, NG, EPG, DE
        self.DC = D // 128
        self.TC = S // 128
        self.AW = NH * 128
        self.G = D // 128
        self.NE = NG * EPG
        self.FC = DE // 128
        self.INW = 3 * self.AW + 4 * D
        self.QT = min(512, S)
        self.lam_init = 0.8 - 0.6 * math.exp(-0.3 * depth_l)
        self.slopes = [2.0 ** (-8.0 * (h + 1) / NH) for h in range(NH)]


def build(cfg, stop_after=None):
    DB = min(512, cfg.D)
    D, S, NB, NH, NG, EPG, DE = cfg.D, cfg.S, cfg.NB, cfg.NH, cfg.NG, cfg.EPG, cfg.DE
    DC, TC, AW, G, NE, FC, INW, QT = cfg.DC, cfg.TC, cfg.AW, cfg.G, cfg.NE, cfg.FC, cfg.INW, cfg.QT
    NR = NG + NE
    nc = bass.Bass("TRN2", target_bir_lowering=False)
    _bufs = {}

    def GB(name):
        if name not in _bufs:
            _bufs[name] = Buf(name)
        return _bufs[name]

    def din(name, shape):
        return nc.dram_tensor(name, list(shape), F32, kind="ExternalInput").ap()

    x_d = din("x", [NB, S, D])
    c_d = din("c", [NB, D])
    w_ada = din("w_ada", [D, 6 * D])
    b_ada = din("b_ada", [1, 6 * D])
    norm1_g = din("norm1_g", [1, D])
    w_in = din("w_in", [D, INW])
    lam_d = din("lam4", [4, 64])
    subln_g = din("subln_g", [1, 128])
    w_ap = din("w_attn_proj", [AW, D])
    ln_g = din("sgu_ln_g", [1, D])
    ln_b = din("sgu_ln_b", [1, D])
    w_s_d = din("sgu_w_s", [G, 128, 128])
    b_s_d = din("sgu_b_s", [1, G * 128])
    w_sp = din("w_sgu_proj", [D, D])
    w_out = din("w_out", [D, D])
    norm2_g = din("norm2_g", [1, D])
    w_rt = din("w_router", [D, NR])
    b_rt = din("b_router", [1, NR])
    w_gu = din("w_expert_gate_up", [NE, D, 2 * DE])
    w_dn = din("w_expert_down", [NE, DE, D])
    final_g = din("final_g", [1, D])
    y_d = nc.dram_tensor("y", [NB, S, D], F32, kind="ExternalOutput").ap()

    with ExitStack() as st:
        K = Kern(nc, st)
        pe, act, dve, pool, sp = K.pe, K.act, K.dve, K.pool, K.sp

        banks = [K.ps("bank%d" % i, [128, 512], F32) for i in range(8)]
        bbuf = [GB("bank%d" % i) for i in range(8)]

        ident = K.sb("ident", [128, 128], F32); b_ident = GB("ident")
        identb = K.sb("identb", [128, 128], BF16); b_identb = GB("identb")
        bcA = K.sb("bcA", [128, D], F32); b_bcA = GB("bcA")
        bcB = K.sb("bcB", [128, D], F32); b_bcB = GB("bcB")
        fgbc = K.sb("fgbc", [128, D], F32); b_fgbc = GB("fgbc")
        bcC = K.sb("bcC", [128, D], F32); b_bcC = GB("bcC")
        subg = K.sb("subg", [128, 128], F32); b_subg = GB("subg")
        lam_t = K.sb("lam_t", [128, 8], F32); b_lam = GB("lam")
        brt = K.sb("brt", [128, NR], F32); b_brt = GB("brt")
        wrt = K.sb("wrt", [128, DC, NR], BF16); b_wrt = GB("wrt")
        mod_scr = nc.dram_tensor("mod_scr", [NB, 6 * D], F32, kind="Internal").ap(); b_modscr = GB("mod_scr")
        cw_scr = nc.dram_tensor("cw_scr", [NE, S], F32, kind="Internal").ap(); b_cwscr = GB("cw_scr")

        HW_ = DC * S // 2
        VW = TC * NH * 130 // 2 + 2
        o_hT = 0
        o_ya = o_hT + HW_
        o_z = o_ya + HW_
        ZW = max(HW_, VW)
        o_y = o_z + ZW
        YW = max(HW_, 2 * S + 4096)
        o_w = o_y + YW
        WW = 12 * 1024
        ARENA = o_w + WW
        arena = K.sb("arena", [128, ARENA], F32)

        def view(off, words, dt, pattern=None, **kw):
            a = arena[:, off:off + words]
            if dt is BF16:
                a = a.bitcast(BF16)
            if pattern:
                a = a.rearrange(pattern, **kw)
            return a

        hT = view(o_hT, HW_, BF16, "p (c s) -> p c s", c=DC); b_hT = GB("hT")
        yaT = view(o_ya, HW_, BF16, "p (c s) -> p c s", c=NH) if NH == DC else None
        assert NH == DC
        b_yaT = GB("yaT")
        zT = view(o_z, HW_, BF16, "p (c s) -> p c s", c=G); b_zT = GB("zT")
        vaug = view(o_z, (TC * NH * 130) // 2, BF16, "p (t h e) -> p t h e", t=TC, h=NH); b_vaug = GB("vaug")
        yT = view(o_y, HW_, BF16, "p (c s) -> p c s", c=DC); b_yT = GB("yT")
        x1 = view(o_ya, TC * D, F32, "p (t d) -> p t d", t=TC); b_x1 = [GB("x1_%d" % i) for i in range(TC)]
        assert TC * D <= HW_ + ZW

        def mm(out, lhsT, rhs, start, stop, reads, writes, sig=None):
            if sig is None:
                sig = stop
            pe.op(lambda e: e.matmul(out, lhsT=lhsT, rhs=rhs, start=start, stop=stop), reads, writes, sig=sig)

        def tr(out, in_, idt, reads, writes, sig=True):
            pe.op(lambda e: e.transpose(out, in_, idt), reads, writes, sig=sig)

        def A(out, in_, func, reads, writes, **kw):
            act.op(lambda e: e.activation(out=out, in_=in_, func=func, **kw), reads, writes)

        def TT(eng, out, in0, in1, op, reads, writes):
            eng.op(lambda e: e.tensor_tensor(out=out, in0=in0, in1=in1, op=op), reads, writes)

        def TS(eng, out, in0, s1, s2, op0, op1, reads, writes, **kw):
            if op1 is None:
                eng.op(lambda e: e.tensor_scalar(out=out, in0=in0, scalar1=s1, scalar2=None, op0=op0, **kw), reads, writes)
            else:
                eng.op(lambda e: e.tensor_scalar(out=out, in0=in0, scalar1=s1, scalar2=s2, op0=op0, op1=op1, **kw), reads, writes)

        def STT(eng, out, in0, scalar, in1, op0, op1, reads, writes):
            eng.op(lambda e: e.scalar_tensor_tensor(out=out, in0=in0, scalar=scalar, in1=in1, op0=op0, op1=op1), reads, writes)

        def CP(eng, out, in_, reads, writes):
            if eng is act:
                act.op(lambda e: e.copy(out=out, in_=in_), reads, writes)
            else:
                eng.op(lambda e: e.tensor_copy(out=out, in_=in_), reads, writes)

        def LD(q, out, in_, anchor, reads=(), writes=None):
            q.dma(lambda e: e.dma_start(out=out, in_=in_), anchor, reads, [anchor] if writes is None else writes)

        dbg_outs = {}

        def dump(name, ap_, shape, dt, rb):
            if not getattr(cfg, "debug", False):
                return
            d = nc.dram_tensor("dbg_" + name, list(shape), dt, kind="ExternalOutput").ap()
            bb = GB("dbg_" + name)
            sp.dma(lambda e: e.dma_start(out=d, in_=ap_), bb, rb, [bb])
            dbg_outs[name] = d

        def rsqrt_col(dst, src, scale, tmp, rb, wb):
            A(tmp, src, AF.Sqrt, rb, wb, bias=epsc[:src.shape[0], 0:1], scale=scale)
            dve.op(lambda e: e.reciprocal(out=dst, in_=tmp), wb, wb)

        epsc = K.sb("epsc", [128, 1], F32); b_eps = GB("epsc")
        pool.op(lambda e: e.memset(epsc[:], EPS), (), [b_eps])
        pool.op(lambda e: e.memset(ident[:], 1.0), (), [b_ident])
        pool.op(lambda e: e.affine_select(out=ident[:], in_=ident[:], pattern=[[-1, 128]], compare_op=ALU.is_equal,
                                          fill=0.0, base=0, channel_multiplier=1), [b_ident], [b_ident])
        CP(dve, identb[:], ident[:], [b_ident], [b_identb])
        LD(sp, fgbc[:], final_g.broadcast_to([128, D]), b_fgbc)
        LD(sp, subg[:], subln_g.broadcast_to([128, 128]), b_subg)
        LD(sp, brt[:], b_rt.broadcast_to([128, NR]), b_brt)
        TS(dve, subg[:], subg[:], 1.0 - cfg.lam_init, None, ALU.mult, None, [b_subg], [b_subg])
        LD(pool, wrt[:], w_rt.rearrange("(c p) n -> p c n", p=128), b_wrt)
        lamw = K.sb("lamw", [128, 4, 64], F32); b_lamw = GB("lamw")
        LD(sp, lamw[:].rearrange("p a b -> p (a b)"), lam_d.rearrange("a b -> (a b)").rearrange("(o n) -> o n", o=1).broadcast_to([128, 256]), b_lamw)
        TT(dve, lamw[:, 0, :], lamw[:, 0, :], lamw[:, 1, :], ALU.mult, [b_lamw], [b_lamw])
        TT(dve, lamw[:, 2, :], lamw[:, 2, :], lamw[:, 3, :], ALU.mult, [b_lamw], [b_lamw])
        dve.op(lambda e: e.tensor_reduce(out=lam_t[:, 0:1], in_=lamw[:, 0, :], axis=AX.X, op=ALU.add), [b_lamw], [b_lam])
        dve.op(lambda e: e.tensor_reduce(out=lam_t[:, 1:2], in_=lamw[:, 2, :], axis=AX.X, op=ALU.add), [b_lamw], [b_lam])
        A(lam_t[:, 2:4], lam_t[:, 0:2], AF.Exp, [b_lam], [b_lam])
        TT(dve, lam_t[:, 4:5], lam_t[:, 2:3], lam_t[:, 3:4], ALU.subtract, [b_lam], [b_lam])
        TS(dve, lam_t[:, 5:6], lam_t[:, 4:5], cfg.lam_init, -1.0, ALU.add, ALU.mult, [b_lam], [b_lam])

        def wview(off_words, words, dt, pattern=None, rows=None, **kw):
            assert off_words + words <= WW, (off_words, words, WW)
            a_ = arena[:, o_w + off_words:o_w + off_words + words] if rows is None else arena[0:rows, o_w + off_words:o_w + off_words + words]
            if dt is BF16:
                a_ = a_.bitcast(BF16)
            if pattern:
                a_ = a_.rearrange(pattern, **kw)
            return a_

        def yview(off_words, words, dt, pattern=None, **kw):
            assert off_words + words <= YW, (off_words, words, YW)
            return view(o_y + off_words, words, dt, pattern, **kw)

        modv = view(o_y, 6 * D, F32)[0:NB, :]; b_modv = GB("modv")
        c_sb = view(o_y + 6 * D, D, F32)[0:NB, :]; b_c = GB("c_sb")
        sgc = view(o_y + 7 * D, D, F32)[0:NB, :]
        g2row = view(o_hT, 2 * D, F32, "p (a d) -> p a d", a=2)[0:NB]; b_g2row = GB("g2row")
        siluT = wview(0, DC * NB // 2 + 1, BF16)[:, 0:DC * NB].rearrange("p (c b) -> p c b", c=DC); b_siluT = GB("siluT")
        LD(sp, c_sb, c_d, b_c)
        A(sgc, c_sb, AF.Sigmoid, [b_c], [b_c])
        TT(dve, c_sb, c_sb, sgc, ALU.mult, [b_c], [b_c])
        pt = banks[0]
        for dc in range(DC):
            tr(pt[:, dc * NB:(dc + 1) * NB], c_sb[:, dc * 128:(dc + 1) * 128], ident[0:NB, 0:NB], [b_c, b_ident], [bbuf[0]])
        CP(dve, siluT.rearrange("p c b -> p (c b)"), pt[:, 0:DC * NB], [bbuf[0]], [b_siluT])
        LD(sp, modv, b_ada.broadcast_to([NB, 6 * D]), b_modv)
        NBLK = 6 * D // 512
        wa = [wview(64 + i * (DC * 256), DC * 256, BF16, "p (c n) -> p c n", c=DC) for i in range(2)]
        b_wa = [GB("wa0"), GB("wa1")]
        for blk in range(NBLK):
            i = blk % 2
            LD(pool, wa[i], w_ada[:, blk * 512:(blk + 1) * 512].rearrange("(c p) n -> p c n", p=128), b_wa[i])
            bk = 1 + (blk % 2)
            for dc in range(DC):
                mm(banks[bk][0:NB, :], siluT[:, dc, :], wa[i][:, dc, :], dc == 0, dc == DC - 1, [b_siluT, b_wa[i]], [bbuf[bk]])
            TT(dve, modv[:, blk * 512:(blk + 1) * 512], modv[:, blk * 512:(blk + 1) * 512], banks[bk][0:NB, :], ALU.add,
               [b_modv, bbuf[bk]], [b_modv])
        LD(sp, g2row[:, 0, :], norm1_g.broadcast_to([NB, D]), b_g2row)
        LD(sp, g2row[:, 1, :], norm2_g.broadcast_to([NB, D]), b_g2row)
        for (gi, off) in ((0, 1), (1, 4)):
            STT(dve, modv[:, off * D:(off + 1) * D], modv[:, off * D:(off + 1) * D], 1.0, g2row[:, gi, :], ALU.add, ALU.mult,
                [b_modv, b_g2row], [b_modv])
        sp.dma(lambda e: e.dma_start(out=mod_scr, in_=modv), b_modv, [b_modv], [b_modscr])
        dump("modv", modv, [NB, 6 * D], F32, [b_modv])
        K.barrier()

        def bcast_mod(b, idx, dst, b_dst):
            sp.dma(lambda e: e.dma_start(out=dst[:], in_=mod_scr[b:b + 1, idx * D:(idx + 1) * D].broadcast_to([128, D])),
                   b_dst, [b_modscr], [b_dst])

        def norm_to_hT(b, src_fn, b_src_fn, tag):
            xt = [wview(i * D, D, F32) for i in range(2)]; b_xt = [GB(tag + "xt0"), GB(tag + "xt1")]
            junk = wview(2 * D, D, F32); b_junk = GB(tag + "junk")
            hb = [wview(3 * D + i * (D // 2), D // 2, BF16) for i in range(2)]; b_hb = [GB(tag + "hb0"), GB(tag + "hb1")]
            st_ = wview(4 * D, 3 * TC, F32); b_st = GB(tag + "st")
            nj = [wview(4 * D + 3 * TC + i * D, D, F32) for i in range(2)]; b_nj = [GB(tag + "nj0"), GB(tag + "nj1")]
            for tc in range(TC):
                src, b_src = src_fn(tc, xt[tc % 2], b_xt[tc % 2])
                act.op((lambda src=src, tc=tc: (lambda e: e.activation(out=junk, in_=src, func=AF.Square, accum_out=st_[:, tc:tc + 1])))(),
                       [b_src], [b_junk, b_st])
            rsqrt_col(st_[:, 2 * TC:3 * TC], st_[:, 0:TC], 1.0 / D, st_[:, TC:2 * TC], [b_st, b_eps], [b_st])
            for tc in range(TC):
                i = tc % 2
                src, b_src = src_fn(tc, xt[i], b_xt[i])
                STT(dve, nj[i], src, st_[:, 2 * TC + tc:2 * TC + tc + 1], bcA[:], ALU.mult, ALU.mult, [b_src, b_st, b_bcA], [b_nj[i]])
                TT(pool, hb[i], nj[i], bcB[:], ALU.add, [b_nj[i], b_bcB], [b_hb[i]])
                bk = tc % 2
                ptb = banks[bk][:].bitcast(BF16)
                for dc in range(DC):
                    tr(ptb[:, dc * 128:(dc + 1) * 128], hb[i][:, dc * 128:(dc + 1) * 128], identb[:], [b_hb[i], b_identb], [bbuf[bk]],
                       sig=(dc == DC - 1))
                CP(act, hT[:, :, tc * 128:(tc + 1) * 128], ptb[:, 0:DC * 128].rearrange("p (c t) -> p c t", c=DC), [bbuf[bk]], [b_hT])

        for b in range(NB):
            bcast_mod(b, 1, bcA, b_bcA)
            bcast_mod(b, 0, bcB, b_bcB)

            def src_x(tc, xt_i, b_xt_i):
                LD(sp, xt_i, x_d[b, tc * 128:(tc + 1) * 128, :], b_xt_i)
                return xt_i, b_xt_i
            wv = wview(8192 if DC * AW // 2 + 8192 <= WW else 0, DC * AW // 2, BF16, "p (c n) -> p c n", c=DC); b_wv = GB("wv")
            LD(pool, wv, w_in[:, 2 * AW:3 * AW].rearrange("(c p) n -> p c n", p=128), b_wv)
            norm_to_hT(b, src_x, None, "n1")
            if b == 0:
                dump("hT", hT, [128, DC, S], BF16, [b_hT])
            K.barrier()

            pool.op(lambda e: e.memset(vaug[:, :, :, 128:130], 1.0), (), [b_vaug])
            VB = min(512, AW)
            HPB = VB // 128
            for tc in range(TC):
                for hb_ in range(AW // VB):
                    bk = (tc * (AW // VB) + hb_) % 4
                    for dc in range(DC):
                        mm(banks[bk][:, 0:VB], hT[:, dc, tc * 128:(tc + 1) * 128], wv[:, dc, hb_ * VB:(hb_ + 1) * VB], dc == 0, dc == DC - 1,
                           [b_hT, b_wv], [bbuf[bk]])
                    eng = act if (hb_ % 2 == 0) else dve
                    CP(eng, vaug[:, tc, hb_ * HPB:(hb_ + 1) * HPB, 0:128], banks[bk][:, 0:VB].rearrange("p (h e) -> p h e", h=HPB), [bbuf[bk]], [b_vaug])
            if b == 0:
                dump("vaug", vaug, [128, TC, NH, 130], BF16, [b_vaug])
            K.barrier()

            Ttab = yview(0, 2 * S, F32); b_T = GB("Ttab")
            pool.op(lambda e: e.iota(Ttab, pattern=[[1, 2 * S]], base=-S, channel_multiplier=-1,
                                     allow_small_or_imprecise_dtypes=True), (), [b_T])
            Ttab2 = yview(2 * S, 2 * S, F32)
            pool.op(lambda e: e.iota(Ttab2, pattern=[[-1, 2 * S]], base=S, channel_multiplier=1,
                                     allow_small_or_imprecise_dtypes=True), (), [b_T])
            TT(dve, Ttab, Ttab, Ttab2, ALU.max, [b_T], [b_T])
            wqk = [wview(0, DC * 128, BF16, "p (c n) -> p c n", c=DC)] * 2
            b_wqk = [GB("wqk0")] * 2
            qo = DC * 128
            qT = [wview(qo + i * (S // 2), S // 2, BF16) for i in range(2)]; b_qT = [GB("qT0"), GB("qT1")]
            ko = qo + S
            kT = [wview(ko + i * (S // 2), S // 2, BF16) for i in range(2)]; b_kT = [GB("kT0"), GB("kT1")]
            to = ko + S
            NTB = 6
            LAP = 2
            tmpb = [wview(to + i * QT, QT, F32) for i in range(NTB)]; b_tmp = [GB("tmp%d" % i) for i in range(NTB)]
            po = to + NTB * QT
            pTb = [wview(po + i * (QT // 2), QT // 2, BF16) for i in range(NTB)]; b_pT = [GB("pT%d" % i) for i in range(NTB)]
            so = po + NTB * (QT // 2)
            NQC = QT // 128
            nab = (2 * NQC + 2) // 3
            accs = [wview(so, nab * 387, F32, "p (k c) -> p k c", k=nab)] * 2
            b_accs = [GB("accs0")] * 2
            so += nab * 387
            a_h = [yview(2 * S + i * (TC * 128), TC * 128, F32, "p (t e) -> p t e", t=TC) for i in range(2)]
            b_ah = [GB("a_h0"), GB("a_h1")]
            ssq = [wview(so + i * 3 * TC, TC, F32) for i in range(2)]
            c2e = [wview(so + i * 3 * TC + TC, TC, F32) for i in range(2)]
            rstd = [wview(so + i * 3 * TC + 2 * TC, TC, F32) for i in range(2)]
            b_st3 = [GB("st3_0"), GB("st3_1")]
            so += 6 * TC
            sm = wview(so, 8, F32); b_sm = GB("sm")
            a_j = [wview(so + 8 + 256 + i * 128, 128, F32) for i in range(2)]; b_aj = [GB("a_j0"), GB("a_j1")]
            a_k = wview(so + 8 + 512, 128, F32); b_ak = GB("a_k")
            yh2 = [wview(so + 8 + 128 + i * 64, 64, BF16) for i in range(2)]; b_yh2 = [GB("yh0"), GB("yh1")]
            assert so + 8 + 640 <= WW, so
            SB_ = [3, 4, 5, 6]

            def emit_proj(h):
                i = h % 2
                LD(pool, wqk[i][:, :, 0:128], w_in[:, h * 128:(h + 1) * 128].rearrange("(c p) n -> p c n", p=128), b_wqk[i])
                LD(pool, wqk[i][:, :, 128:256], w_in[:, AW + h * 128:AW + (h + 1) * 128].rearrange("(c p) n -> p c n", p=128), b_wqk[i])
                for t5 in range(S // QT):
                    for (which, dstT, b_dst) in ((0, qT[i], b_qT[i]), (1, kT[i], b_kT[i])):
                        for dc in range(DC):
                            mm(banks[7][:, 0:QT], wqk[i][:, dc, which * 128:(which + 1) * 128], hT[:, dc, t5 * QT:(t5 + 1) * QT],
                               dc == 0, dc == DC - 1, [b_wqk[i], b_hT], [bbuf[7]])
                        CP(act if which == 0 else dve, dstT[:, t5 * QT:(t5 + 1) * QT], banks[7][:, 0:QT], [bbuf[7]], [b_dst])

            def acc(m, qc):
                a_ = m * NQC + qc
                bk = a_ // 3
                return banks[bk][:, (a_ % 3) * 129:(a_ % 3) * 129 + 129], bbuf[bk], (a_ % 3 == 0)

            def emit_score_pair(h, p, qt, kc):
                i = h % 2
                for m in range(2):
                    sbk = SB_[(2 * p + m) % 4]
                    mm(banks[sbk][:, 0:QT], kT[i][m * 64:(m + 1) * 64, kc * 128:(kc + 1) * 128],
                       qT[i][m * 64:(m + 1) * 64, qt * QT:(qt + 1) * QT], True, True, [b_kT[i], b_qT[i]], [bbuf[sbk]])
                off = qt * QT - kc * 128 + S
                for m in range(2):
                    sbk = SB_[(2 * p + m) % 4]
                    ti = (2 * p + m) % NTB
                    STT(dve, tmpb[ti], Ttab[:, off:off + QT], -8.0 * cfg.slopes[h], banks[sbk][:, 0:QT], ALU.mult, ALU.add,
                        [b_T, bbuf[sbk]], [b_tmp[ti]])
                    A(pTb[ti], tmpb[ti], AF.Exp, [b_tmp[ti]], [b_pT[ti]], scale=0.125)

            def emit_av_pair(h, p, qt, kc):
                for m in range(2):
                    ti = (2 * p + m) % NTB
                    for qc in range(NQC):
                        a_ap, a_b, first = acc(m, qc)
                        pe.op((lambda a_ap=a_ap, ti=ti, qc=qc, kc=kc, h=h, first=first: (lambda e: e.matmul(
                            a_ap, lhsT=pTb[ti][:, qc * 128:(qc + 1) * 128], rhs=vaug[:, kc, h, 0:129],
                            start=(kc == 0 and first), stop=(kc == TC - 1), skip_group_check=True)))(),
                            [b_pT[ti], b_vaug], [a_b], sig=(qc == NQC - 1))
                if kc == TC - 1:
                    emit_post(h, qt)

            def emit_post(h, qt):
                i = h % 2
                par = qt % 2
                for bk in range(nab):
                    ncol = min(3, 2 * NQC - 3 * bk) * 129
                    CP(dve, accs[par][:, bk, 0:ncol], banks[bk][:, 0:ncol], [bbuf[bk]], [b_accs[par]])
                for qc in range(NQC):
                    slot = qt * NQC + qc
                    a0_, a1_ = qc, NQC + qc
                    c0 = (a0_ % 3) * 129; c1 = (a1_ % 3) * 129
                    o0 = accs[par][:, a0_ // 3, c0:c0 + 128]; l0 = accs[par][:, a0_ // 3, c0 + 128:c0 + 129]
                    o1 = accs[par][:, a1_ // 3, c1:c1 + 128]; l1 = accs[par][:, a1_ // 3, c1 + 128:c1 + 129]
                    at = a_h[i][:, slot, :]
                    TS(pool, at, o0, l1, 0.0, ALU.mult, ALU.add, [b_accs[par]], [b_ah[i]])
                    TT(pool, sm[:, 0:1], l0, lam_t[:, 5:6], ALU.mult, [b_accs[par], b_lam], [b_sm])
                    TS(pool, a_k, o1, sm[:, 0:1], 0.0, ALU.mult, ALU.add, [b_accs[par], b_sm], [b_ak])
                    TT(pool, at, at, a_k, ALU.add, [b_ak, b_ah[i]], [b_ah[i]])
                    TS(pool, sm[:, 1:2], l0, l1, 0.0, ALU.mult, ALU.add, [b_accs[par]], [b_sm])
                    TS(pool, c2e[i][:, slot:slot + 1], sm[:, 1:2], sm[:, 1:2], EPS, ALU.mult, ALU.mult, [b_sm], [b_st3[i]])

            def emit_norm(h):
                i = h % 2
                for slot in range(TC):
                    j = slot % 2
                    TT(pool, a_j[j], a_h[i][:, slot, :], a_h[i][:, slot, :], ALU.mult, [b_ah[i]], [b_aj[j]])
                    dve.op((lambda i=i, slot=slot, j=j: (lambda e: e.tensor_reduce(out=ssq[i][:, slot:slot + 1], in_=a_j[j], axis=AX.X, op=ALU.add)))(),
                           [b_aj[j]], [b_st3[i]])
                TS(pool, ssq[i], ssq[i], 1.0 / 128, 0.0, ALU.mult, ALU.add, [b_st3[i]], [b_st3[i]])
                TT(pool, ssq[i], ssq[i], c2e[i], ALU.add, [b_st3[i]], [b_st3[i]])
                A(c2e[i], ssq[i], AF.Sqrt, [b_st3[i]], [b_st3[i]])
                dve.op((lambda i=i: (lambda e: e.reciprocal(out=rstd[i], in_=c2e[i])))(), [b_st3[i]], [b_st3[i]])
                ptb = banks[7][:].bitcast(BF16)
                for slot in range(TC):
                    j = slot % 2
                    TS(pool, a_k, a_h[i][:, slot, :], rstd[i][:, slot:slot + 1], 0.0, ALU.mult, ALU.add, [b_ah[i], b_st3[i]], [b_ak])
                    TT(pool, yh2[j], a_k, subg[:], ALU.mult, [b_ak, b_subg], [b_yh2[j]])
                    tr(ptb[:, 0:128], yh2[j], identb[:], [b_yh2[j], b_identb], [bbuf[7]])
                    CP(dve, yaT[:, h, slot * 128:(slot + 1) * 128], ptb[:, 0:128], [bbuf[7]], [b_yaT])

            pairs = [(qt, kc) for qt in range(S // QT) for kc in range(TC)]
            npair = len(pairs)
            emit_proj(0)
            for h in range(NH):
                for p in range(npair + LAP):
                    if p < npair:
                        emit_score_pair(h, p, *pairs[p])
                    if p == min(12, npair - 1) and h >= 1:
                        emit_norm(h - 1)
                    if p == npair // 2 and h + 1 < NH:
                        emit_proj(h + 1)
                    if p >= LAP:
                        emit_av_pair(h, p - LAP, *pairs[p - LAP])
            emit_norm(NH - 1)
            if b == 0:
                dump("yaT", yaT, [128, NH, S], BF16, [b_yaT])
            K.barrier()

            NQC_ = QT // 128
            wu = wview(0, DC * D // 2, BF16, "p (c n) -> p c n", c=DC); b_wu = GB("wu")
            wsw = wview(DC * D // 2, DC * D // 2, BF16, "p (c n) -> p c n", c=DC); b_wsw = GB("wsw")
            LD(pool, wu, w_in[:, 3 * AW:3 * AW + D].rearrange("(c p) n -> p c n", p=128), b_wu)
            LD(pool, wsw, w_in[:, 3 * AW + D:3 * AW + 2 * D].rearrange("(c p) n -> p c n", p=128), b_wsw)
            o2 = DC * D
            uTt = wview(o2, G * QT // 2, BF16, "p (g t) -> p g t", g=G); b_uTt = GB("uTt")
            sfulls = [wview(o2 + G * QT // 2 + i * D, D, F32) for i in range(2)]; b_sfulls = [GB("sfull0"), GB("sfull1")]
            lnG = yview(0, D, F32); b_lnG = GB("lnG")
            lnB = yview(D, D, F32); b_lnB = GB("lnB")
            bsbc = yview(2 * D, G * 128, F32); b_bsbc = GB("bsbc")
            tmp2 = yview(2 * D + G * 128, G * 128, F32); b_tmp2 = GB("tmp2")
            g1s = [yview(2 * D + 2 * G * 128 + i * 512, 512, F32) for i in range(2)]; b_g1s = [GB("g1_0"), GB("g1_1")]
            g2s = [yview(YW - 1024 + i * 512, 512, F32) for i in range(2)]; b_g2s = [GB("g2_0"), GB("g2_1")]
            gcnt = [0]
            wsT = yview(2 * D + 2 * G * 128 + 1024, G * 64, BF16, "p (g t) -> p g t", g=G); b_wsT = GB("wsT")
            st4s = [yview(2 * D + 2 * G * 128 + 1024 + G * 64 + 136 + i * 8, 8, F32) for i in range(2)]; b_st4s = [GB("st4_0"), GB("st4_1")]
            vss = [yview(2 * D + 2 * G * 128 + 1024 + G * 64 + 160 + i * (D // 2), D // 2, BF16) for i in range(2)]; b_vss = [GB("vs0"), GB("vs1")]
            assert 2 * D + 2 * G * 128 + 1024 + G * 64 + 160 + D <= YW - 1024
            wst = yview(2 * D + 2 * G * 128 + 1024 + G * 64 + 8, 128, F32); b_wst = GB("wst")
            LD(sp, lnG, ln_g.broadcast_to([128, D]), b_lnG)
            LD(sp, lnB, ln_b.broadcast_to([128, D]), b_lnB)
            LD(sp, bsbc, b_s_d.broadcast_to([128, G * 128]), b_bsbc)
            for g in range(G):
                LD(sp, wst, w_s_d[g], b_wst)
                tr(banks[0][:, 0:128], wst, ident[:], [b_wst, b_ident], [bbuf[0]])
                CP(dve, wsT[:, g, :], banks[0][:, 0:128], [bbuf[0]], [b_wsT])

            C_G = 0.044715 ** 0.5

            def gelu_A(gi, src, n, rb):
                A(g1s[gi][:, 0:n], src, AF.Square, rb, [b_g1s[gi]], scale=C_G)
                STT(dve, g1s[gi][:, 0:n], g1s[gi][:, 0:n], 1.0, src, ALU.add, ALU.mult, [b_g1s[gi]] + rb, [b_g1s[gi]])

            def gelu_B(gi, dst, src, n, rb, wb):
                A(g2s[gi][:, 0:n], g1s[gi][:, 0:n], AF.Sigmoid, [b_g1s[gi]], [b_g2s[gi]], scale=1.5957691216057308)
                TT(dve, dst, g2s[gi][:, 0:n], src, ALU.mult, [b_g2s[gi]] + rb, wb)

            for t5 in range(S // QT):
                def uA(g):
                    bk = g % 2
                    for dc in range(DC):
                        mm(banks[bk][:, 0:QT], wu[:, dc, g * 128:(g + 1) * 128], hT[:, dc, t5 * QT:(t5 + 1) * QT], dc == 0, dc == DC - 1,
                           [b_wu, b_hT], [bbuf[bk]])
                    gelu_A(g % 2, banks[bk][:, 0:QT], QT, [bbuf[bk]])
                uA(0)
                for g in range(G):
                    if g + 1 < G:
                        uA(g + 1)
                    gelu_B(g % 2, uTt[:, g, :], banks[g % 2][:, 0:QT], QT, [bbuf[g % 2]], [b_uTt])
                NHB = D // DB

                def stA(tcl):
                    tc = t5 * NQC_ + tcl
                    sfull, b_sfull = sfulls[tc % 2], b_sfulls[tc % 2]

                    def sA(hb_):
                        bk = 2 + hb_ % 2
                        for dc in range(DC):
                            mm(banks[bk][:, 0:DB], hT[:, dc, tc * 128:(tc + 1) * 128], wsw[:, dc, hb_ * DB:(hb_ + 1) * DB], dc == 0, dc == DC - 1,
                               [b_hT, b_wsw], [bbuf[bk]])
                        gelu_A(hb_ % 2, banks[bk][:, 0:DB], DB, [bbuf[bk]])
                    sA(0)
                    for hb_ in range(NHB):
                        if hb_ + 1 < NHB:
                            sA(hb_ + 1)
                        gelu_B(hb_ % 2, sfull[:, hb_ * DB:(hb_ + 1) * DB], banks[2 + hb_ % 2][:, 0:DB], DB, [bbuf[2 + hb_ % 2]], [b_sfull])

                def stB(tcl):
                    tc = t5 * NQC_ + tcl
                    sfull, b_sfull = sfulls[tc % 2], b_sfulls[tc % 2]
                    vs, b_vs = vss[tc % 2], b_vss[tc % 2]
                    st4, b_st4 = st4s[tc % 2], b_st4s[tc % 2]
                    dve.op((lambda st4=st4, sfull=sfull: (lambda e: e.tensor_reduce(out=st4[:, 0:1], in_=sfull, axis=AX.X, op=ALU.add)))(), [b_sfull], [b_st4])
                    TS(dve, st4[:, 1:2], st4[:, 0:1], -1.0 / D, None, ALU.mult, None, [b_st4], [b_st4])
                    TS(dve, sfull, sfull, st4[:, 1:2], None, ALU.add, None, [b_sfull, b_st4], [b_sfull])
                    for hb_ in range(NHB):
                        act.op((lambda hb_=hb_, sfull=sfull, st4=st4: (lambda e: e.activation(
                            out=banks[6 + hb_][:, 0:DB], in_=sfull[:, hb_ * DB:(hb_ + 1) * DB], func=AF.Square, accum_out=st4[:, 5 + hb_:6 + hb_])))(),
                            [b_sfull], [bbuf[6 + hb_], b_st4])
                    if NHB == 2:
                        TT(dve, st4[:, 2:3], st4[:, 5:6], st4[:, 6:7], ALU.add, [b_st4], [b_st4])
                    else:
                        CP(dve, st4[:, 2:3], st4[:, 5:6], [b_st4], [b_st4])
                    rsqrt_col(st4[:, 4:5], st4[:, 2:3], 1.0 / D, st4[:, 3:4], [b_st4, b_eps], [b_st4])
                    STT(dve, sfull, sfull, st4[:, 4:5], lnG, ALU.mult, ALU.mult, [b_sfull, b_st4, b_lnG], [b_sfull])
                    TT(pool, vs, sfull, lnB, ALU.add, [b_sfull, b_lnB], [b_vs])

                def stC(tcl):
                    tc = t5 * NQC_ + tcl
                    vs, b_vs = vss[tc % 2], b_vss[tc % 2]
                    for g in range(G):
                        bk = 4 + g // 4
                        pe.op((lambda bk=bk, g=g, vs=vs: (lambda e: e.matmul(banks[bk][:, (g % 4) * 128:(g % 4) * 128 + 128], lhsT=vs[:, g * 128:(g + 1) * 128],
                                                                             rhs=wsT[:, g, :], start=True, stop=True, skip_group_check=True)))(),
                              [b_vs, b_wsT], [bbuf[bk]])
                    for gb in range((G + 3) // 4):
                        ng = min(4, G - gb * 4)
                        TT(dve, tmp2[:, gb * 512:gb * 512 + ng * 128], banks[4 + gb][:, 0:ng * 128], bsbc[:, gb * 512:gb * 512 + ng * 128], ALU.add,
                           [bbuf[4 + gb], b_bsbc], [b_tmp2])
                    TT(pool, zT[:, :, tc * 128:(tc + 1) * 128], tmp2.rearrange("p (g t) -> p g t", g=G), uTt[:, :, tcl * 128:(tcl + 1) * 128], ALU.mult,
                       [b_tmp2, b_uTt], [b_zT])

                for step in range(NQC_ + 2):
                    if step < NQC_:
                        stA(step)
                    if 1 <= step < NQC_ + 1:
                        stB(step - 1)
                    if step >= 2:
                        stC(step - 2)
            if b == 0:
                dump("zT", zT, [128, G, S], BF16, [b_zT])
            K.barrier()

            CW = DC * 64
            wsl = [[wview(i * 4 * CW + k * CW, CW, BF16, "p (c n) -> p c n", c=DC) for k in range(4)] for i in range(2)]
            b_wsl = [GB("wsl0"), GB("wsl1")]
            so5 = 8 * CW
            s12 = [[wview(so5 + (i * 2 + k) * QT, QT, F32) for k in range(2)] for i in range(2)]
            b_s12 = [[GB("s12_%d%d" % (i, k)) for k in range(2)] for i in range(2)]
            it = 0
            wo = wview(8192 if DC * D // 2 + 8192 <= WW else 6 * 1024, DC * D // 2, BF16, "p (c n) -> p c n", c=DC); b_wo = GB("wo")
            LD(pool, wo, w_out.rearrange("(c p) n -> p c n", p=128), b_wo)

            def load_s5(j):
                i = j % 2
                srcs = (w_ap[:, j * 128:(j + 1) * 128], w_in[:, 3 * AW + 2 * D + j * 128:3 * AW + 2 * D + (j + 1) * 128],
                        w_sp[:, j * 128:(j + 1) * 128], w_in[:, 3 * AW + 3 * D + j * 128:3 * AW + 3 * D + (j + 1) * 128])
                for k in range(4):
                    LD(pool, wsl[i][k], srcs[k].rearrange("(c p) n -> p c n", p=128), b_wsl[i])
            load_s5(0)
            for j in range(DC):
                i = j % 2
                if j + 1 < DC:
                    load_s5(j + 1)
                for t5 in range(S // QT):
                    p = (it % 2) * 4
                    ii = it % 2
                    it += 1
                    tok = slice(t5 * QT, (t5 + 1) * QT)
                    opnds = ((yaT, b_yaT, NH), (hT, b_hT, DC), (zT, b_zT, G), (hT, b_hT, DC))
                    for k in range(4):
                        src, b_src, nk = opnds[k]
                        for kc in range(nk):
                            mm(banks[p + k][:, 0:QT], wsl[i][k][:, kc, :], src[:, kc, tok], kc == 0, kc == nk - 1, [b_wsl[i], b_src], [bbuf[p + k]])
                    A(s12[ii][0], banks[p + 1][:, 0:QT], AF.Sigmoid, [bbuf[p + 1]], [b_s12[ii][0]])
                    A(s12[ii][1], banks[p + 3][:, 0:QT], AF.Sigmoid, [bbuf[p + 3]], [b_s12[ii][1]])
                    TT(dve, s12[ii][0], s12[ii][0], banks[p + 0][:, 0:QT], ALU.mult, [b_s12[ii][0], bbuf[p + 0]], [b_s12[ii][0]])
                    TT(dve, s12[ii][1], s12[ii][1], banks[p + 2][:, 0:QT], ALU.mult, [b_s12[ii][1], bbuf[p + 2]], [b_s12[ii][1]])
                    TT(pool, yT[:, j, tok], s12[ii][0], s12[ii][1], ALU.add, [b_s12[ii][0], b_s12[ii][1]], [b_yT])
            if b == 0:
                dump("yT", yT, [128, DC, S], BF16, [b_yT])
            K.barrier()

            bcast_mod(b, 2, bcA, b_bcA)
            xt6 = [wview(i * D, D, F32) for i in range(2)]; b_xt6 = [GB("xt6_0"), GB("xt6_1")]
            tm6 = wview(2 * D, D, F32); b_tm6 = GB("tm6")
            for tc in range(TC):
                i = tc % 2
                LD(sp, xt6[i], x_d[b, tc * 128:(tc + 1) * 128, :], b_xt6[i])
                for hb_ in range(D // DB):
                    bk = (tc % 2) * 2 + hb_ % 2
                    for kc in range(DC):
                        mm(banks[bk][:, 0:DB], yT[:, kc, tc * 128:(tc + 1) * 128], wo[:, kc, hb_ * DB:(hb_ + 1) * DB], kc == 0, kc == DC - 1,
                           [b_yT, b_wo], [bbuf[bk]])
                    TT(dve, tm6[:, hb_ * DB:(hb_ + 1) * DB], banks[bk][:, 0:DB], bcA[:, hb_ * DB:(hb_ + 1) * DB], ALU.mult, [bbuf[bk], b_bcA], [b_tm6])
                TT(pool, x1[:, tc, :], tm6, xt6[i], ALU.add, [b_tm6, b_xt6[i]], [b_x1[tc]])
            if b == 0:
                dump("x1", x1, [128, TC, D], F32, b_x1)
            K.barrier()

            bcast_mod(b, 5, bcC, b_bcC)
            MT = min(256, S)
            NMT = S // MT
            MC = MT // 128
            GUW = DC * DE
            DNW = FC * D // 2
            EW = GUW + DNW
            wslot = []
            for si in range(4):
                base_ = (o_y + si * EW) if si < 2 else (o_w + (si - 2) * EW)
                wslot.append((view(base_, GUW, BF16, "p (c n) -> p c n", c=DC), view(base_ + GUW, DNW, BF16, "p (f d) -> p f d", f=FC)))
            assert 2 * EW <= YW
            b_wslot = [GB("wslot%d" % si) for si in range(4)]
            def load_pair(ep):
                for e_ in range(2):
                    e = ep * 2 + e_
                    si = e % 4
                    LD(pool, wslot[si][0], w_gu[e].rearrange("(c p) n -> p c n", p=128), b_wslot[si])
                    LD(pool, wslot[si][1], w_dn[e].rearrange("(f p) d -> p f d", p=128), b_wslot[si])
                    for f in range(FC):
                        TT(pool, wslot[si][1][:, f, :], wslot[si][1][:, f, :], bcC[:], ALU.mult, [b_wslot[si], b_bcC], [b_wslot[si]])

            load_pair(0)

            bcast_mod(b, 4, bcA, b_bcA)
            bcast_mod(b, 3, bcB, b_bcB)
            norm_to_hT(b, lambda tc, xt_i, b_xt_i: (x1[:, tc, :], b_x1[tc]), None, "n2")
            if b == 0:
                dump("h2T", hT, [128, DC, S], BF16, [b_hT])
            K.barrier()

            NPAR = 4

            def router_chunk(tc, par):
                base = par * 768
                lg = wview(base + 0, NR + 4, F32)[:, 0:NR]; b_lg = GB("lg%d" % par)
                r8 = wview(base + 64, 16, F32); b_r8 = GB("r8_%d" % par)
                gm = wview(base + 96, NG, F32); b_gm = GB("gm%d" % par)
                els = wview(base + 128, EPG, F32); b_els = GB("els%d" % par)
                m8 = wview(base + 160, 8, F32); b_m8 = GB("m8_%d" % par)
                cws = wview(base + 192, EPG, F32); b_cws = GB("cws%d" % par)
                cws2 = wview(base + 224, EPG, F32)
                cw = wview(base + 256, NE, F32); b_cw = GB("cw%d" % par)
                cwT = wview(base + 512, 128, F32); b_cwT = GB("cwT%d" % par)
                bk = par
                for dc in range(DC):
                    mm(banks[bk][:, 0:NR], hT[:, dc, tc * 128:(tc + 1) * 128], wrt[:, dc, :], dc == 0, dc == DC - 1, [b_hT, b_wrt], [bbuf[bk]])
                TT(dve, lg, banks[bk][:, 0:NR], brt[:], ALU.add, [bbuf[bk], b_brt], [b_lg]); yield
                dve.op(lambda e: e.tensor_reduce(out=r8[:, 0:1], in_=lg[:, 0:NG], axis=AX.X, op=ALU.max), [b_lg], [b_r8]); yield
                TS(dve, gm, lg[:, 0:NG], r8[:, 0:1], None, ALU.is_equal, None, [b_lg, b_r8], [b_gm]); yield
                TS(dve, r8[:, 1:2], r8[:, 0:1], -1.0, None, ALU.mult, None, [b_r8], [b_r8]); yield
                A(cws2[:, 0:NG], lg[:, 0:NG], AF.Exp, [b_lg, b_r8], [b_cws, b_r8], bias=r8[:, 1:2], scale=1.0, accum_out=r8[:, 2:3])
                TS(dve, els, lg[:, NG:NG + EPG], gm[:, 0:1], None, ALU.mult, None, [b_lg, b_gm], [b_els]); yield
                for g in range(1, NG):
                    STT(dve, els, lg[:, NG + g * EPG:NG + (g + 1) * EPG], gm[:, g:g + 1], els, ALU.mult, ALU.add, [b_lg, b_gm, b_els], [b_els]); yield
                dve.op(lambda e: e.max(out=m8, in_=els), [b_els], [b_m8]); yield
                TT(dve, r8[:, 4:5], m8[:, 1:2], m8[:, 0:1], ALU.subtract, [b_m8], [b_r8]); yield
                A(r8[:, 5:6], r8[:, 4:5], AF.Exp, [b_r8], [b_r8])
                dve.op(lambda e: e.reciprocal(out=r8[:, 3:4], in_=r8[:, 2:3]), [b_r8], [b_r8]); yield
                TS(dve, r8[:, 6:7], r8[:, 5:6], 1.0, None, ALU.add, None, [b_r8], [b_r8]); yield
                dve.op(lambda e: e.reciprocal(out=r8[:, 7:8], in_=r8[:, 6:7]), [b_r8], [b_r8]); yield
                TT(dve, r8[:, 8:9], r8[:, 7:8], r8[:, 3:4], ALU.mult, [b_r8], [b_r8]); yield
                TT(dve, r8[:, 9:10], r8[:, 3:4], r8[:, 8:9], ALU.subtract, [b_r8], [b_r8]); yield
                TS(dve, cws, els, m8[:, 0:1], r8[:, 8:9], ALU.is_equal, ALU.mult, [b_els, b_m8, b_r8], [b_cws]); yield
                TS(dve, cws2, els, m8[:, 1:2], r8[:, 9:10], ALU.is_equal, ALU.mult, [b_els, b_m8, b_r8, b_cws], [b_cws]); yield
                TT(dve, cws, cws, cws2, ALU.add, [b_cws], [b_cws]); yield
                for g in range(NG):
                    TS(dve, cw[:, g * EPG:(g + 1) * EPG], cws, gm[:, g:g + 1], None, ALU.mult, None, [b_cws, b_gm], [b_cw]); yield
                tr(banks[4 + bk][0:NE, 0:128], cw, ident[:], [b_cw, b_ident], [bbuf[4 + bk]])
                CP(dve, cwT[0:NE, :], banks[4 + bk][0:NE, 0:128], [bbuf[4 + bk]], [b_cwT]); yield
                sp.dma(lambda e: e.dma_start(out=cw_scr[:, tc * 128:(tc + 1) * 128], in_=cwT[0:NE, :]), b_cwT, [b_cwT], [b_cwscr])

            for t0_ in range(0, TC, NPAR):
                gens = [router_chunk(tc, tc - t0_) for tc in range(t0_, min(TC, t0_ + NPAR))]
                while gens:
                    nxt = []
                    for g_ in gens:
                        try:
                            next(g_)
                            nxt.append(g_)
                        except StopIteration:
                            pass
                    gens = nxt
            if b == 0:
                dump("cw", cw_scr, [NE, S], F32, [b_cwscr])
            K.barrier()

            o9 = 2 * EW
            cwbc = [[wview(o9 + (i * 2 + k) * MT, MT, F32) for k in range(2)] for i in range(2)]
            b_cwbc = [[GB("cwbc%d%d" % (i, k)) for k in range(2)] for i in range(2)]
            o9 += 4 * MT
            sg9 = [wview(o9 + i * FC * MT, FC * MT, F32) for i in range(2)]; b_sg9 = [GB("sg9_0"), GB("sg9_1")]
            o9 += 2 * FC * MT
            tm9 = [wview(o9 + i * 512, 512, F32) for i in range(2)]; b_tm9 = [GB("tm9_0"), GB("tm9_1")]
            o9 += 1024
            assert FC * MT <= 512
            actp2 = [[wview(o9 + (i * 2 + k) * (FC * MT // 2), FC * MT // 2, BF16, "p (f t) -> p f t", f=FC) for k in range(2)] for i in range(2)]
            b_actp2 = [[GB("actp%d%d" % (i, k)) for k in range(2)] for i in range(2)]
            o9 += 2 * FC * MT
            assert o9 <= WW, o9
            ycnt = [0]

            def emit_gu(k, ep, mt):
                tok = slice(mt * MT, (mt + 1) * MT)
                for e_ in range(2):
                    e = ep * 2 + e_
                    si = e % 4
                    wg = wslot[si][0]
                    LD(sp, cwbc[e_][mt % 2], cw_scr[e:e + 1, tok].broadcast_to([128, MT]), b_cwbc[e_][mt % 2], reads=[b_cwscr])
                    pb = e_ * 2
                    for n in range(2 * FC):
                        bk = pb + n // FC
                        col = (n % FC) * MT
                        for dc in range(DC):
                            pe.op((lambda bk=bk, col=col, wg=wg, dc=dc, n=n, tok=tok: (lambda e__: e__.matmul(
                                banks[bk][:, col:col + MT], lhsT=wg[:, dc, n * 128:(n + 1) * 128], rhs=hT[:, dc, tok],
                                start=(dc == 0), stop=(dc == DC - 1), skip_group_check=True)))(),
                                [b_wslot[si], b_hT], [bbuf[bk]], sig=(dc == DC - 1))
                    gps = banks[pb][:, 0:FC * MT]
                    ups = banks[pb + 1][:, 0:FC * MT]
                    A(sg9[e_], gps, AF.Sigmoid, [bbuf[pb]], [b_sg9[e_]])
                    TT(dve, sg9[e_], sg9[e_], gps, ALU.mult, [b_sg9[e_], bbuf[pb]], [b_sg9[e_]])
                    TT(dve, sg9[e_], sg9[e_], ups, ALU.mult, [b_sg9[e_], bbuf[pb + 1]], [b_sg9[e_]])
                    for f in range(FC):
                        TT(pool, actp2[e_][k % 2][:, f, :], sg9[e_][:, f * MT:(f + 1) * MT], cwbc[e_][mt % 2], ALU.mult,
                           [b_sg9[e_], b_cwbc[e_][mt % 2]], [b_actp2[e_][k % 2]])

            def emit_down(k, ep, mt):
                for mc in range(MC):
                    tc = mt * MC + mc
                    for hb_ in range(D // DB):
                        bk = 4 + ycnt[0] % 4
                        ti = ycnt[0] % 2
                        ycnt[0] += 1
                        nmm = 0
                        for e_ in range(2):
                            wd_ = wslot[(ep * 2 + e_) % 4][1]
                            for f in range(FC):
                                mm(banks[bk][:, 0:DB], actp2[e_][k % 2][:, f, mc * 128:(mc + 1) * 128], wd_[:, f, hb_ * DB:(hb_ + 1) * DB],
                                   nmm == 0, nmm == 2 * FC - 1, [b_actp2[e_][k % 2], b_wslot[(ep * 2 + e_) % 4]], [bbuf[bk]])
                                nmm += 1
                        TT(dve, x1[:, tc, hb_ * DB:(hb_ + 1) * DB], banks[bk][:, 0:DB], x1[:, tc, hb_ * DB:(hb_ + 1) * DB], ALU.add,
                           [bbuf[bk], b_x1[tc]], [b_x1[tc]])

            items = [(ep, mt) for ep in range(NE // 2) for mt in range(NMT)]
            for k, (ep, mt) in enumerate(items):
                emit_gu(k, ep, mt)
                if k >= 1:
                    emit_down(k - 1, *items[k - 1])
                if mt == 0 and ep + 1 < NE // 2:
                    load_pair(ep + 1)
            emit_down(len(items) - 1, *items[-1])
            if b == 0:
                dump("x2", x1, [128, TC, D], F32, b_x1)
            K.barrier()

            ot = [wview(i * D, D, F32) for i in range(2)]; b_ot = [GB("ot0"), GB("ot1")]
            jk = wview(2 * D, D, F32); b_jk = GB("jk10")
            st10 = wview(3 * D, 3 * TC, F32); b_st10 = GB("st10")
            for tc in range(TC):
                act.op((lambda tc=tc: (lambda e: e.activation(out=jk, in_=x1[:, tc, :], func=AF.Square, accum_out=st10[:, tc:tc + 1])))(),
                       [b_x1[tc]], [b_jk, b_st10])
            rsqrt_col(st10[:, 2 * TC:3 * TC], st10[:, 0:TC], 1.0 / D, st10[:, TC:2 * TC], [b_st10, b_eps], [b_st10])
            for tc in range(TC):
                i = tc % 2
                STT(dve, ot[i], x1[:, tc, :], st10[:, 2 * TC + tc:2 * TC + tc + 1], fgbc[:], ALU.mult, ALU.mult, [b_x1[tc], b_st10, b_fgbc], [b_ot[i]])
                sp.dma((lambda i=i, tc=tc, b=b: (lambda e: e.dma_start(out=y_d[b, tc * 128:(tc + 1) * 128, :], in_=ot[i])))(), b_ot[i], [b_ot[i]], [])
            K.barrier()
        K.barrier()
        K.emit()
    return nc


_NC_CACHE = {}


def _prep_shared(inp, cfg):
    f = lambda a: np.ascontiguousarray(np.asarray(a, dtype=np.float32))
    L = 0
    sh = {
        "w_ada": f(inp["w_ada"][L]), "b_ada": f(inp["b_ada"][L]).reshape(1, -1), "norm1_g": f(inp["norm1_g"][L]).reshape(1, -1),
        "w_in": f(inp["w_in"][L]),
        "lam4": f(np.stack([np.asarray(inp["lambda_q1"][L]), np.asarray(inp["lambda_k1"][L]),
                            np.asarray(inp["lambda_q2"][L]), np.asarray(inp["lambda_k2"][L])], axis=0)),
        "subln_g": f(inp["subln_g"][L]).reshape(1, -1), "w_attn_proj": f(inp["w_attn_proj"][L]),
        "sgu_ln_g": f(inp["sgu_ln_g"][L]).reshape(1, -1), "sgu_ln_b": f(inp["sgu_ln_b"][L]).reshape(1, -1),
        "sgu_w_s": f(inp["sgu_w_s"][L]), "sgu_b_s": f(inp["sgu_b_s"][L]).reshape(1, -1),
        "w_sgu_proj": f(inp["w_sgu_proj"][L]), "w_out": f(inp["w_out"][L]), "norm2_g": f(inp["norm2_g"][L]).reshape(1, -1),
        "w_router": f(np.concatenate([np.asarray(inp["w_router_group"][L]), np.asarray(inp["w_router_expert"][L])], axis=1)),
        "b_router": f(np.concatenate([np.asarray(inp["b_router_group"][L]), np.asarray(inp["b_router_expert"][L])], axis=0)).reshape(1, -1),
        "w_expert_gate_up": f(inp["w_expert_gate_up"][L]), "w_expert_down": f(inp["w_expert_down"][L]),
        "final_g": f(inp["final_g"]).reshape(1, -1),
    }
    return sh


def kernel(**inputs):
    cfg = Cfg()
    n_cores = 8
    x = np.asarray(inputs["x"], dtype=np.float32)
    c = np.asarray(inputs["c"], dtype=np.float32)
    sh = _prep_shared(inputs, cfg)
    if "nc" not in _NC_CACHE:
        _NC_CACHE["nc"] = build(cfg)
    nc = _NC_CACHE["nc"]
    in_maps = []
    for i in range(n_cores):
        m = dict(sh)
        m["x"] = np.ascontiguousarray(x[i * cfg.NB:(i + 1) * cfg.NB])
        m["c"] = np.ascontiguousarray(c[i * cfg.NB:(i + 1) * cfg.NB])
        in_maps.append(m)
    res = run_bass_kernel_spmd(nc, in_maps, core_ids=list(range(n_cores)))
    return np.concatenate([r["y"] for r in res.results], axis=0).astype(np.float32)
```
